# Optimizing a Trainium2 kernel written in Bass

```python
import math
import jax
import jax.numpy as jnp
from jax import lax
import numpy as np

D_MODEL = 1024
BATCH = 4
SEQ = 8192
DEPTH = 4

GRID_W = 64
CTX_LEN = 256
NORM_EPS = 1e-6
F32 = jnp.float32

HY_W = 512
HY_ORDER = 2
HY_SHORT = 3
HY_BANDS = 16
HY_EMB = 2 * HY_BANDS + 1
HY_FFN = 64
HY_TARGET = 1e-2
HY_FAST_RATE = math.log(HY_TARGET) / 0.3
HY_SLOW_RATE = math.log(HY_TARGET) / 1.5

RG_W = 512
RG_BLOCKS = 8
RG_BS = RG_W // RG_BLOCKS
RG_CONV = 4
RG_C = 8.0

GLA_HEADS = 4
GLA_DK = 64
GLA_DV = 128
GLA_QK = GLA_HEADS * GLA_DK
GLA_V = GLA_HEADS * GLA_DV
GLA_RANK = 16
GLA_TAU = 16.0
GLA_CHUNK = 64

N_BRANCH = 3
IN_GROUPS = ((HY_ORDER + 1) * HY_W, RG_W, RG_W, 2 * GLA_QK + GLA_V + 2 * GLA_RANK, GLA_V, N_BRANCH * D_MODEL)
IN_DIM = sum(IN_GROUPS)

PEER_HEADS = 8
PEER_NKEYS = 128
PEER_EXPERTS = PEER_NKEYS * PEER_NKEYS
PEER_DKEY = 256
PEER_TOPK = 16
PEER_BLOCK = 128

kernel_name = 'hybrid_hyena_rglru_gla_peer_dit'


def rmsnorm(x, g):
    xf = x.astype(F32)
    y = xf * lax.rsqrt(jnp.mean(xf * xf, axis=-1, keepdims=True) + NORM_EPS)
    return (y * g.astype(F32)).astype(x.dtype)


def modulate(h, shift, scale):
    return h * (1.0 + scale) + shift


def flip_seq(t, d):
    return t[:, ::-1] if d else t


def raster_to_column(t):
    b_, n = t.shape[:2]
    rows = n // GRID_W
    return t.reshape(b_, rows, GRID_W, *t.shape[2:]).swapaxes(1, 2).reshape(t.shape)


def column_to_raster(t):
    b_, n = t.shape[:2]
    rows = n // GRID_W
    return t.reshape(b_, GRID_W, rows, *t.shape[2:]).swapaxes(1, 2).reshape(t.shape)


def depthwise_conv(x, w, b, left):
    width = w.shape[0]
    n = x.shape[1]
    xp = jnp.pad(x, ((0, 0), (left, width - 1 - left), (0, 0)))
    y = b
    for j in range(width):
        y = y + w[j] * xp[:, j:j + n]
    return y


def linear_scan(a, b, h0):
    def combine(lft, rgt):
        return lft[0] * rgt[0], rgt[0] * lft[1] + rgt[1]
    a_cum, b_cum = lax.associative_scan(combine, (a, b), axis=1)
    h = a_cum * h0[:, None] + b_cum
    return h, h[:, -1]


def hyena_filter_spectra(n, w1, b1, w2, b2, w3, freq):
    idx = jnp.arange(n, dtype=F32)
    tn = idx / (n - 1)
    bands = jnp.linspace(1e-4, HY_BANDS - 1, HY_BANDS, dtype=F32)
    ang = (2.0 * math.pi / n) * idx[:, None] * bands[None, :]
    feats = jnp.concatenate([tn[:, None], jnp.cos(ang), -jnp.sin(ang)], axis=-1)
    fr = freq.astype(F32)
    h = jnp.sin(fr * (feats @ w1.astype(F32) + b1.astype(F32)))
    h = jnp.sin(fr * (h @ w2.astype(F32) + b2.astype(F32)))
    h = (h @ w3.astype(F32)).reshape(n, HY_ORDER, 2, HY_W)
    deltas = jnp.abs(jnp.linspace(HY_FAST_RATE, HY_SLOW_RATE, HY_W, dtype=F32))
    window = jnp.exp(-tn[:, None] * deltas[None, :])
    h = h * window[:, None, None, :]
    h = h / (jnp.sum(jnp.abs(h), axis=(0, 2), keepdims=True) + 1e-6)
    h_fwd, h_bwd = h[:, :, 0], h[:, :, 1]
    taps = jnp.concatenate([h_fwd[:1] + h_bwd[:1], h_fwd[1:], jnp.zeros_like(h_fwd[:1]), h_bwd[:0:-1]], axis=0)
    return jnp.fft.rfft(taps, axis=0)


def fft_long_conv(u, spec, skip):
    n = u.shape[1]
    uf = jnp.fft.rfft(u, n=2 * n, axis=1)
    y = jnp.fft.irfft(uf * spec[None], n=2 * n, axis=1)[:, :n]
    return y + u * skip


def hyena_mix(u, conv_w, conv_b, w1, b1, w2, b2, w3, freq, skip):
    dt = u.dtype
    n = u.shape[1]
    u = depthwise_conv(u, conv_w, conv_b, left=(HY_SHORT - 1) // 2).astype(F32)
    v, x1, x2 = jnp.split(u, 3, axis=-1)
    spec = hyena_filter_spectra(n, w1, b1, w2, b2, w3, freq)
    skip = skip.astype(F32)
    z = x1 * fft_long_conv(v, spec[:, 0], skip[0])
    z = x2 * fft_long_conv(z, spec[:, 1], skip[1])
    return z.astype(dt)


def rglru_scan(u, h0, conv_w, conv_b, wa, ba, wx, bx, lam):
    b_, n, _ = u.shape
    xc = depthwise_conv(u, conv_w, conv_b, left=RG_CONV - 1)
    xb = xc.reshape(b_, n, RG_BLOCKS, RG_BS)
    gate_r = jax.nn.sigmoid((jnp.einsum('blgi,gij->blgj', xb, wa).reshape(b_, n, RG_W) + ba).astype(F32))
    gate_i = jax.nn.sigmoid((jnp.einsum('blgi,gij->blgj', xb, wx).reshape(b_, n, RG_W) + bx).astype(F32))
    log_a = -RG_C * gate_r * jax.nn.softplus(-lam.astype(F32))
    a = jnp.exp(log_a)
    b = jnp.sqrt(-jnp.expm1(2.0 * log_a)) * (gate_i * xc.astype(F32))
    return linear_scan(a, b, h0)


def rglru_mix(u_ctx, g_ctx, u_lat, g_lat, conv_w, conv_b, wa, ba, wx, bx, lam, need_ctx):
    h0 = jnp.zeros((u_lat.shape[0], RG_W), F32)
    h_ctx = 0.0
    h_lat = 0.0
    for d in range(2):
        args = (conv_w[d], conv_b[d], wa[d], ba[d], wx[d], bx[d], lam[d])
        hc, hc_last = rglru_scan(flip_seq(u_ctx, d), h0, *args)
        hl, _ = rglru_scan(flip_seq(u_lat, d), hc_last, *args)
        h_ctx = h_ctx + flip_seq(hc, d)
        h_lat = h_lat + flip_seq(hl, d)
    y_lat = h_lat.astype(u_lat.dtype) * jax.nn.gelu(g_lat)
    y_ctx = h_ctx.astype(u_ctx.dtype) * jax.nn.gelu(g_ctx) if need_ctx else None
    return y_ctx, y_lat


def gla_chunked(q, k, v, log_a, s0):
    b_, n, nh, _ = q.shape
    nc = n // GLA_CHUNK

    def blocks(t):
        return t.reshape(b_, nc, GLA_CHUNK, nh, t.shape[-1]).transpose(0, 3, 1, 2, 4)

    q, k, v, log_a = blocks(q), blocks(k), blocks(v), blocks(log_a)
    cum = jnp.cumsum(log_a, axis=3)
    cum_last = cum[:, :, :, -1:]
    qg = q * jnp.exp(cum)
    scores = jnp.einsum('bhncd,bhnsd->bhncs', qg, k * jnp.exp(-cum))
    lower = jnp.tril(jnp.ones((GLA_CHUNK, GLA_CHUNK), dtype=bool))
    scores = jnp.where(lower, scores, 0.0)
    o = jnp.einsum('bhncs,bhnse->bhnce', scores, v)
    ds = jnp.einsum('bhncd,bhnce->bhnde', k * jnp.exp(cum_last - cum), v)
    decay = jnp.exp(cum_last[:, :, :, 0])

    def combine(lft, rgt):
        return lft[0] * rgt[0], rgt[0][..., None] * lft[1] + rgt[1]

    d_cum, s_cum = lax.associative_scan(combine, (decay, ds), axis=2)
    s_end = d_cum[..., None] * s0[:, :, None] + s_cum
    s_start = jnp.concatenate([s0[:, :, None], s_end[:, :, :-1]], axis=2)
    o = o + jnp.einsum('bhncd,bhnde->bhnce', qg, s_start)
    o = o.transpose(0, 2, 3, 1, 4).reshape(b_, n, nh, GLA_DV)
    return o, s_end[:, :, -1]


def gla_prepare(t, w_lr, b_lr):
    b_, n, _ = t.shape
    t = t.astype(F32)
    q, k, v, lr = jnp.split(t, [GLA_QK, 2 * GLA_QK, 2 * GLA_QK + GLA_V], axis=-1)
    q = q.reshape(b_, n, GLA_HEADS, GLA_DK) * (GLA_DK ** -0.5)
    k = k.reshape(b_, n, GLA_HEADS, GLA_DK)
    v = v.reshape(b_, n, GLA_HEADS, GLA_DV)
    lr = lr.reshape(b_, n, 2, GLA_RANK)
    logits = jnp.einsum('bldr,drk->bldk', lr, w_lr.astype(F32)) + b_lr.astype(F32)
    log_a = (jax.nn.log_sigmoid(logits) / GLA_TAU).reshape(b_, n, 2, GLA_HEADS, GLA_DK)
    return q, k, v, log_a


def gla_mix(t_ctx, t_lat, w_lr, b_lr):
    qc, kc, vc, lac = gla_prepare(t_ctx, w_lr, b_lr)
    ql, kl, vl, lal = gla_prepare(t_lat, w_lr, b_lr)
    s0 = jnp.zeros((qc.shape[0], GLA_HEADS, GLA_DK, GLA_DV), F32)
    o_ctx = -jnp.sum(qc * kc, axis=-1, keepdims=True) * vc
    o_lat = -jnp.sum(ql * kl, axis=-1, keepdims=True) * vl
    for d in range(2):
        oc, sc = gla_chunked(flip_seq(qc, d), flip_seq(kc, d), flip_seq(vc, d), flip_seq(lac[:, :, d], d), s0)
        ol, _ = gla_chunked(flip_seq(ql, d), flip_seq(kl, d), flip_seq(vl, d), flip_seq(lal[:, :, d], d), sc)
        o_ctx = o_ctx + flip_seq(oc, d)
        o_lat = o_lat + flip_seq(ol, d)
    return o_ctx, o_lat


def gla_finish(o, g, norm_g):
    on = o * lax.rsqrt(jnp.mean(o * o, axis=-1, keepdims=True) + NORM_EPS) * norm_g.astype(F32)
    return on.reshape(g.shape).astype(g.dtype) * jax.nn.silu(g)


def merge_branches(gate_logits, y_hy, y_rg, y_gla, lp):
    g = jax.nn.sigmoid(gate_logits + lp['b_merge'])
    g_hy, g_rg, g_gla = jnp.split(g, N_BRANCH, axis=-1)
    m = g_hy * (y_hy @ lp['w_hy_o']) + g_rg * (y_rg @ lp['w_rg_o']) + g_gla * (y_gla @ lp['w_gla_o'])
    return m @ lp['w_out']


def token_mixer(h_ctx, h_lat, lp, need_ctx):
    pts = [int(p) for p in np.cumsum(IN_GROUPS)[:-1]]
    hy_c, rgx_c, rgg_c, gla_c, glag_c, mg_c = jnp.split(h_ctx @ lp['w_in'], pts, axis=-1)
    hy_l, rgx_l, rgg_l, gla_l, glag_l, mg_l = jnp.split(h_lat @ lp['w_in'], pts, axis=-1)
    hy_args = (lp['hy_conv_w'], lp['hy_conv_b'], lp['hy_w1'], lp['hy_b1'], lp['hy_w2'], lp['hy_b2'],
               lp['hy_w3'], lp['hy_freq'], lp['hy_skip'])
    y_hy_l = hyena_mix(hy_l, *hy_args)
    y_rg_c, y_rg_l = rglru_mix(rgx_c, rgg_c, rgx_l, rgg_l, lp['rg_conv_w'], lp['rg_conv_b'], lp['rg_wa'],
                               lp['rg_ba'], lp['rg_wx'], lp['rg_bx'], lp['rg_lambda'], need_ctx)
    o_gla_c, o_gla_l = gla_mix(gla_c, raster_to_column(gla_l), lp['gla_w_lr'], lp['gla_b_lr'])
    y_gla_l = gla_finish(column_to_raster(o_gla_l), glag_l, lp['gla_norm_g'])
    out_lat = merge_branches(mg_l, y_hy_l, y_rg_l, y_gla_l, lp)
    if not need_ctx:
        return None, out_lat
    y_hy_c = hyena_mix(hy_c, *hy_args)
    y_gla_c = gla_finish(o_gla_c, glag_c, lp['gla_norm_g'])
    out_ctx = merge_branches(mg_c, y_hy_c, y_rg_c, y_gla_c, lp)
    return out_ctx, out_lat


def peer_ffn(h, wq, keys, u_tab, v_tab):
    b_, n, dm = h.shape
    token_blocks = h.reshape(b_ * n // PEER_BLOCK, PEER_BLOCK, dm)
    keys = keys.astype(F32)

    def block(xb):
        q = (xb @ wq).astype(F32).reshape(PEER_BLOCK, PEER_HEADS, 2, PEER_DKEY // 2)
        s = jnp.einsum('thpc,hpnc->thpn', q, keys)
        s_top, i_top = lax.top_k(s, PEER_TOPK)
        cand = (s_top[:, :, 0, :, None] + s_top[:, :, 1, None, :]).reshape(PEER_BLOCK, PEER_HEADS, -1)
        cand_idx = (i_top[:, :, 0, :, None] * PEER_NKEYS + i_top[:, :, 1, None, :]).reshape(PEER_BLOCK, PEER_HEADS, -1)
        best, pos = lax.top_k(cand, PEER_TOPK)
        expert = jnp.take_along_axis(cand_idx, pos, axis=-1).reshape(PEER_BLOCK, -1)
        weight = jax.nn.softmax(best, axis=-1).reshape(PEER_BLOCK, -1)
        u = jnp.take(u_tab, expert, axis=0)
        v = jnp.take(v_tab, expert, axis=0)
        act = jax.nn.gelu(jnp.einsum('td,ted->te', xb, u).astype(F32))
        return jnp.einsum('te,ted->td', (weight * act).astype(v.dtype), v)

    return lax.map(block, token_blocks).reshape(b_, n, dm)


def setup_inputs(seed: int = 0) -> dict:
    key = jax.random.key(seed)
    keys = jax.random.split(key, 48)
    counter = [0]

    def nrm(shape, scale):
        k = keys[counter[0]]
        counter[0] += 1
        return jax.random.normal(k, shape, F32) * scale

    def gain(shape):
        return 1.0 + nrm(shape, 0.05)

    d = D_MODEL
    lam_u = jax.random.uniform(keys[47], (DEPTH, 2, RG_W), F32, 0.9, 0.999)
    lam_a = lam_u ** (1.0 / RG_C)
    rg_lambda = jnp.log(lam_a) - jnp.log1p(-lam_a)
    return {
        'x': nrm((BATCH, SEQ, d), 1.0),
        'c': nrm((BATCH, d), 1.0),
        'ctx': nrm((BATCH, CTX_LEN, d), 1.0),
        'c_ctx': nrm((d,), 1.0),
        'w_mod': nrm((DEPTH, d, 6 * d), 0.5 * d ** -0.5),
        'b_mod': nrm((DEPTH, 6 * d), 0.02),
        'g_norm_mix': gain((DEPTH, d)),
        'g_norm_ffn': gain((DEPTH, d)),
        'w_in': nrm((DEPTH, d, IN_DIM), d ** -0.5),
        'hy_conv_w': nrm((DEPTH, HY_SHORT, (HY_ORDER + 1) * HY_W), 0.5),
        'hy_conv_b': nrm((DEPTH, (HY_ORDER + 1) * HY_W), 0.02),
        'hy_w1': nrm((DEPTH, HY_EMB, HY_FFN), HY_EMB ** -0.5),
        'hy_b1': nrm((DEPTH, HY_FFN), 0.1),
        'hy_w2': nrm((DEPTH, HY_FFN, HY_FFN), HY_FFN ** -0.5),
        'hy_b2': nrm((DEPTH, HY_FFN), 0.1),
        'hy_w3': nrm((DEPTH, HY_FFN, HY_ORDER * 2 * HY_W), HY_FFN ** -0.5),
        'hy_freq': gain((DEPTH, HY_FFN)),
        'hy_skip': nrm((DEPTH, HY_ORDER, HY_W), 0.1),
        'rg_conv_w': nrm((DEPTH, 2, RG_CONV, RG_W), 0.5),
        'rg_conv_b': nrm((DEPTH, 2, RG_W), 0.02),
        'rg_wa': nrm((DEPTH, 2, RG_BLOCKS, RG_BS, RG_BS), RG_BS ** -0.5),
        'rg_ba': nrm((DEPTH, 2, RG_W), 0.1),
        'rg_wx': nrm((DEPTH, 2, RG_BLOCKS, RG_BS, RG_BS), RG_BS ** -0.5),
        'rg_bx': nrm((DEPTH, 2, RG_W), 0.1),
        'rg_lambda': rg_lambda,
        'gla_w_lr': nrm((DEPTH, 2, GLA_RANK, GLA_QK), GLA_RANK ** -0.5),
        'gla_b_lr': nrm((DEPTH, 2, GLA_QK), 0.1),
        'gla_norm_g': gain((DEPTH, GLA_DV)),
        'w_hy_o': nrm((DEPTH, HY_W, d), HY_W ** -0.5),
        'w_rg_o': nrm((DEPTH, RG_W, d), RG_W ** -0.5),
        'w_gla_o': nrm((DEPTH, GLA_V, d), GLA_V ** -0.5),
        'b_merge': nrm((DEPTH, N_BRANCH * d), 0.02),
        'w_out': nrm((DEPTH, d, d), d ** -0.5),
        'peer_wq': nrm((DEPTH, d, PEER_HEADS * PEER_DKEY), d ** -0.5),
        'peer_keys': nrm((DEPTH, PEER_HEADS, 2, PEER_NKEYS, PEER_DKEY // 2), (PEER_DKEY // 2) ** -0.5),
        'peer_u': nrm((DEPTH, PEER_EXPERTS, d), d ** -0.5),
        'peer_v': nrm((DEPTH, PEER_EXPERTS, d), 0.5),
        'g_final': gain((d,)),
    }


def reference(x, c, ctx, c_ctx, w_mod, b_mod, g_norm_mix, g_norm_ffn, w_in, hy_conv_w, hy_conv_b,
              hy_w1, hy_b1, hy_w2, hy_b2, hy_w3, hy_freq, hy_skip, rg_conv_w, rg_conv_b, rg_wa, rg_ba,
              rg_wx, rg_bx, rg_lambda, gla_w_lr, gla_b_lr, gla_norm_g, w_hy_o, w_rg_o, w_gla_o, b_merge,
              w_out, peer_wq, peer_keys, peer_u, peer_v, g_final):
    cond_lat = jax.nn.silu(c)[:, None, :]
    cond_ctx = jax.nn.silu(c_ctx)[None, None, :]
    x_lat, x_ctx = x, ctx
    for l in range(DEPTH):
        need_ctx = l < DEPTH - 1
        sh1, sc1, gt1, sh2, sc2, gt2 = jnp.split(cond_lat @ w_mod[l] + b_mod[l], 6, axis=-1)
        csh1, csc1, cgt1, csh2, csc2, cgt2 = jnp.split(cond_ctx @ w_mod[l] + b_mod[l], 6, axis=-1)
        lp = {
            'w_in': w_in[l], 'hy_conv_w': hy_conv_w[l], 'hy_conv_b': hy_conv_b[l],
            'hy_w1': hy_w1[l], 'hy_b1': hy_b1[l], 'hy_w2': hy_w2[l], 'hy_b2': hy_b2[l],
            'hy_w3': hy_w3[l], 'hy_freq': hy_freq[l], 'hy_skip': hy_skip[l],
            'rg_conv_w': rg_conv_w[l], 'rg_conv_b': rg_conv_b[l], 'rg_wa': rg_wa[l], 'rg_ba': rg_ba[l],
            'rg_wx': rg_wx[l], 'rg_bx': rg_bx[l], 'rg_lambda': rg_lambda[l],
            'gla_w_lr': gla_w_lr[l], 'gla_b_lr': gla_b_lr[l], 'gla_norm_g': gla_norm_g[l],
            'w_hy_o': w_hy_o[l], 'w_rg_o': w_rg_o[l], 'w_gla_o': w_gla_o[l],
            'b_merge': b_merge[l], 'w_out': w_out[l],
        }
        h_lat = modulate(rmsnorm(x_lat, g_norm_mix[l]), sh1, sc1)
        h_ctx = modulate(rmsnorm(x_ctx, g_norm_mix[l]), csh1, csc1)
        y_ctx, y_lat = token_mixer(h_ctx, h_lat, lp, need_ctx)
        x_lat = x_lat + gt1 * y_lat
        h_lat = modulate(rmsnorm(x_lat, g_norm_ffn[l]), sh2, sc2)
        x_lat = x_lat + gt2 * peer_ffn(h_lat, peer_wq[l], peer_keys[l], peer_u[l], peer_v[l])
        if need_ctx:
            x_ctx = x_ctx + cgt1 * y_ctx
            h_ctx = modulate(rmsnorm(x_ctx, g_norm_ffn[l]), csh2, csc2)
            x_ctx = x_ctx + cgt2 * peer_ffn(h_ctx, peer_wq[l], peer_keys[l], peer_u[l], peer_v[l])
    return rmsnorm(x_lat, g_final)
```

```python
import math
from contextlib import ExitStack

import numpy as np
import ml_dtypes
import concourse.bass as bass
import concourse.mybir as mybir
from concourse.bass_utils import run_bass_kernel_spmd

F32 = mybir.dt.float32
BF16 = mybir.dt.bfloat16
I32 = mybir.dt.int32
U32 = mybir.dt.uint32
AF = mybir.ActivationFunctionType
ALU = mybir.AluOpType
AX = mybir.AxisListType

D = 1024
NCTX = 256
NLAT = 8192
T = NCTX + NLAT
DEPTH = 4
IN_DIM = 7200
EPS = 1e-6
NDMASEM = 8
TILES = [(0, 256)] + [(256 + i * 512, 512) for i in range(16)]


class KB:
    def __init__(self, nc, self_wait=True):
        self.nc = nc
        self.es = ExitStack()
        self.engs = {"pe": nc.tensor, "dve": nc.vector, "act": nc.scalar, "pool": nc.gpsimd, "sp": nc.sync}
        self.sems = {}
        self.cnt = {}
        for e in self.engs:
            self._mksem("c_" + e)
            for i in range(NDMASEM):
                self._mksem(f"d_{e}{i}")
        self.dma_rr = {e: 0 for e in self.engs}
        self.waited = {e: {} for e in self.engs}
        self.last_w = {}
        self.readers = {}
        self.self_wait = self_wait
        self.n_ins = 0
        self.n_wait = 0
        self.phase = None

    def _mksem(self, name):
        self.sems[name] = self.es.enter_context(self.nc.semaphore(name))
        self.cnt[name] = 0

    def begin_phase(self):
        self.pstack = getattr(self, "pstack", [])
        self.pstack.append(self.phase)
        self.phase = ExitStack()

    def end_phase(self):
        self.barrier()
        self.phase.close()
        self.phase = self.pstack.pop()

    def sb(self, name, shape, dt=F32, perm=False):
        st = self.es if (perm or self.phase is None) else self.phase
        self.uid = getattr(self, "uid", 0) + 1
        return st.enter_context(self.nc.sbuf_tensor(f"{name}_{self.uid}", list(shape), dt))

    def ps(self, name, shape, dt=F32, perm=False):
        st = self.es if (perm or self.phase is None) else self.phase
        self.uid = getattr(self, "uid", 0) + 1
        return st.enter_context(self.nc.psum_tensor(f"{name}_{self.uid}", list(shape), dt))

    def dram(self, name, shape, dt=F32, kind="Internal"):
        return self.nc.dram_tensor(name, list(shape), dt, kind=kind).ap()

    def _wait(self, eng, s, v):
        if self.waited[eng].get(s, 0) >= v:
            return
        self.engs[eng].wait_ge(self.sems[s], v)
        self.waited[eng][s] = v
        self.n_wait += 1

    def _need(self, eng, reads, writes):
        need = {}

        def add(sv):
            s, v = sv
            if need.get(s, 0) < v:
                need[s] = v

        for r in reads:
            if r in self.last_w:
                add(self.last_w[r])
        for w in writes:
            if w in self.last_w:
                add(self.last_w[w])
            for sv in self.readers.get(w, ()):
                add(sv)
        own = "c_" + eng
        for s, v in need.items():
            if s == own and (eng == "pe" or not self.self_wait):
                continue
            self._wait(eng, s, v)

    def _record(self, sem, val, reads, writes):
        for r in reads:
            self.readers.setdefault(r, []).append((sem, val))
        for w in writes:
            self.last_w[w] = (sem, val)
            self.readers[w] = []

    def op(self, eng, fn, reads=(), writes=()):
        self._need(eng, reads, writes)
        ins = fn()
        sem = "c_" + eng
        self.cnt[sem] += 1
        ins.then_inc(self.sems[sem], 1)
        self._record(sem, self.cnt[sem], reads, writes)
        self.n_ins += 1
        return ins

    def _dma_sem(self, eng):
        i = self.dma_rr[eng]
        self.dma_rr[eng] = (i + 1) % NDMASEM
        return f"d_{eng}{i}"

    def dma(self, eng, out, in_, reads=(), writes=(), **kw):
        self._need(eng, reads, writes)
        sem = self._dma_sem(eng)
        ins = self.engs[eng].dma_start(out=out, in_=in_, **kw)
        self.cnt[sem] += 16
        ins.then_inc(self.sems[sem], 16)
        self._record(sem, self.cnt[sem], reads, writes)
        self.n_ins += 1
        return ins

    def gather(self, out, table, idx_ap, reads=(), writes=()):
        eng = "pool"
        self._need(eng, reads, writes)
        sem = self._dma_sem(eng)
        ins = self.nc.gpsimd.indirect_dma_start(
            out=out, out_offset=None, in_=table,
            in_offset=bass.IndirectOffsetOnAxis(ap=idx_ap, axis=0))
        self.cnt[sem] += 16
        ins.then_inc(self.sems[sem], 16)
        self._record(sem, self.cnt[sem], reads, writes)
        self.n_ins += 1
        return ins

    def barrier(self):
        for e in self.engs:
            for s, v in self.cnt.items():
                if v > 0 and not (s == "c_" + e and e == "pe"):
                    self._wait(e, s, v)

    def finish(self):
        self.barrier()
        if self.phase is not None:
            self.phase.close()
            self.phase = None
        self.es.close()


def bc(ap, shape):
    return ap.to_broadcast(list(shape))


PGROUPS = [
    ("hy", 0, 1024), ("hy", 1024, 512), ("rgx", 1536, 512), ("rgg", 2048, 512),
    ("gqk", 2560, 512), ("gv", 3072, 512), ("glr", 3584, 32), ("glag", 3616, 512),
    ("mg", 4128, 1024), ("mg", 5152, 1024), ("mg", 6176, 1024),
]


class Prog:
    def __init__(self, layers=DEPTH, debug=()):
        self.layers = layers
        self.debug = set(debug)
        nc = bass.Bass("TRN2", target_bir_lowering=False)
        self.nc = nc
        self.k = KB(nc)
        self.dbg_outs = []
        self.declare_io()
        self.setup_consts()

    def inp(self, name, shape, dt=F32):
        return self.nc.dram_tensor(name, list(shape), dt, kind="ExternalInput").ap()

    def outp(self, name, shape, dt=F32):
        return self.nc.dram_tensor(name, list(shape), dt, kind="ExternalOutput").ap()

    def declare_io(self):
        k = self.k
        self.xT = self.inp("xT", [8, 128, T])
        self.cT = self.inp("cT", [128, 8, 2])
        self.w_mod = self.inp("w_mod", [DEPTH, D, 6 * D])
        self.b_modT = self.inp("b_modT", [128, DEPTH, 48])
        self.gmixT = self.inp("gmixT", [128, DEPTH, 8])
        self.gffnT = self.inp("gffnT", [128, DEPTH, 8])
        self.gfinT = self.inp("gfinT", [128, 8])
        self.w_in = self.inp("w_in", [DEPTH, D, IN_DIM])
        self.b_mergeT = self.inp("b_mergeT", [128, DEPTH, 24])
        self.xres = k.dram("xres", [8, 128, T])
        self.hT = k.dram("hT", [8, 128, T], BF16)
        self.p_hy = k.dram("p_hy", [12, 128, T])
        self.p_rgx = k.dram("p_rgx", [4, 128, T])
        self.p_rgg = k.dram("p_rgg", [4, 128, T])
        self.p_gqk = k.dram("p_gqk", [4, 128, T], BF16)
        self.p_gv = k.dram("p_gv", [4, 128, T], BF16)
        self.p_glr = k.dram("p_glr", [2, 16, T])
        self.p_glag = k.dram("p_glag", [4, 128, T])
        self.p_mg = k.dram("p_mg", [24, 128, T])

    def dbg(self, name, src_ap, shape, dt=F32, reads=()):
        if name not in self.debug:
            return
        o = self.outp("dbg_" + name, shape, dt)
        self.k.dma("sp", o, src_ap, reads=list(reads), writes=["dbg_" + name])
        self.dbg_outs.append("dbg_" + name)

    def setup_consts(self):
        k, nc = self.k, self.nc
        self.ones_bf = k.sb("ones_bf", [128, 128], BF16, perm=True)
        k.op("pool", lambda: nc.gpsimd.memset(self.ones_bf[:], 1.0), writes=["ones_bf"])
        self.ident = k.sb("ident", [128, 128], F32, perm=True)
        k.op("pool", lambda: nc.gpsimd.memset(self.ident[:], 1.0), writes=["ident"])
        k.op("pool", lambda: nc.gpsimd.affine_select(out=self.ident[:], in_=self.ident[:], pattern=[[-1, 128]],
                                                    compare_op=ALU.is_equal, fill=0.0, base=0, channel_multiplier=1),
             reads=["ident"], writes=["ident"])
        self.ident_bf = k.sb("ident_bf", [128, 128], BF16, perm=True)
        k.op("pool", lambda: nc.gpsimd.tensor_copy(out=self.ident_bf[:], in_=self.ident[:]), reads=["ident"], writes=["ident_bf"])
        self.onec = k.sb("onec", [128, 1], F32, perm=True)
        k.op("pool", lambda: nc.gpsimd.memset(self.onec[:], 1.0), writes=["onec"])
        self.epsc = k.sb("epsc", [128, 1], F32, perm=True)
        k.op("pool", lambda: nc.gpsimd.memset(self.epsc[:], EPS), writes=["epsc"])
        self.sc_t = k.sb("sc_t", [128, 8, 2], F32, perm=True)
        k.dma("sp", self.sc_t[:], self.cT, writes=["sc_t"])
        k.op("act", lambda: nc.scalar.activation(out=self.sc_t[:], in_=self.sc_t[:], func=AF.Silu), reads=["sc_t"], writes=["sc_t"])
        self.bmod = k.sb("bmod", [128, DEPTH, 48], F32, perm=True)
        k.dma("sp", self.bmod[:], self.b_modT, writes=["bmod"])
        self.gmix = k.sb("gmix", [128, DEPTH, 8], F32, perm=True)
        k.dma("sp", self.gmix[:], self.gmixT, writes=["gmix"])
        self.gffn = k.sb("gffn", [128, DEPTH, 8], F32, perm=True)
        k.dma("sp", self.gffn[:], self.gffnT, writes=["gffn"])
        self.gfin = k.sb("gfin", [128, 8], F32, perm=True)
        k.dma("sp", self.gfin[:], self.gfinT, writes=["gfin"])
        self.bmerge = k.sb("bmerge", [128, DEPTH, 24], F32, perm=True)
        k.dma("sp", self.bmerge[:], self.b_mergeT, writes=["bmerge"])
        self.modT = k.sb("modT", [128, 48, 2], F32, perm=True)
        self.A1 = k.sb("A1", [128, 8, 2], F32, perm=True)
        self.A2 = k.sb("A2", [128, 8, 2], F32, perm=True)

    def stage0(self, l):
        k, nc = self.k, self.nc
        k.begin_phase()
        wm = [k.sb(f"wm{i}", [128, 8, 1024]) for i in range(2)]
        pm = k.ps("pm", [128, 8, 2])
        for g in range(6):
            w = wm[g % 2]
            wk = f"wm{g % 2}"
            k.dma("sp", w[:], self.w_mod[l, :, g * 1024:(g + 1) * 1024].rearrange("(k p) c -> p k c", p=128), writes=[wk])
            for cc in range(8):
                for kk in range(8):
                    k.op("pe", lambda: nc.tensor.matmul(pm[:, cc, :], lhsT=w[:, kk, cc * 128:(cc + 1) * 128], rhs=self.sc_t[:, kk, :],
                                                        start=(kk == 0), stop=(kk == 7)),
                         reads=[wk, "sc_t"], writes=["pm"])
            k.op("dve", lambda: nc.vector.tensor_tensor(out=self.modT[:, g * 8:(g + 1) * 8, :], in0=pm[:],
                                                        in1=bc(self.bmod[:, l, g * 8:(g + 1) * 8].unsqueeze(2), [128, 8, 2]), op=ALU.add),
                 reads=["pm", "bmod"], writes=["modT"])
        k.op("dve", lambda: nc.vector.scalar_tensor_tensor(out=self.A1[:], in0=self.modT[:, 8:16, :], scalar=1.0,
                                                           in1=bc(self.gmix[:, l, :].unsqueeze(2), [128, 8, 2]), op0=ALU.add, op1=ALU.mult),
             reads=["modT", "gmix"], writes=["A1"])
        k.op("dve", lambda: nc.vector.scalar_tensor_tensor(out=self.A2[:], in0=self.modT[:, 32:40, :], scalar=1.0,
                                                           in1=bc(self.gffn[:, l, :].unsqueeze(2), [128, 8, 2]), op0=ALU.add, op1=ALU.mult),
             reads=["modT", "gffn"], writes=["A2"])
        k.end_phase()

    def stage_norm(self, l, which, xsrc, tiles=TILES):
        k, nc = self.k, self.nc
        A = self.A1 if which == 1 else self.A2
        Ak = "A1" if which == 1 else "A2"
        sh0 = 0 if which == 1 else 24
        k.begin_phase()
        NB = 2
        xt = [k.sb(f"n_xt{i}", [128, 8, 512]) for i in range(NB)]
        sq = [k.sb(f"n_sq{i}", [128, 8, 512], BF16) for i in range(NB)]
        ss = [k.ps(f"n_ss{i}", [128, 512]) for i in range(NB)]
        rstd = [k.sb(f"n_rstd{i}", [128, 512]) for i in range(NB)]
        tmp = [k.sb(f"n_tmp{i}", [128, 8, 512]) for i in range(NB)]
        hh = [k.sb(f"n_h{i}", [128, 8, 512], BF16) for i in range(NB)]
        xv = xsrc.rearrange("k p t -> p k t")
        hv = self.hT.rearrange("k p t -> p k t")
        for ti, (s0, W) in enumerate(tiles):
            b = ti % NB
            j = 1 if s0 < NCTX else 0
            k.dma("sp", xt[b][:, :, :W], xv[:, :, s0:s0 + W], reads=["xres"], writes=[f"n_xt{b}"])
            k.op("act", lambda: nc.scalar.activation(out=sq[b][:, :, :W], in_=xt[b][:, :, :W], func=AF.Square),
                 reads=[f"n_xt{b}"], writes=[f"n_sq{b}"])
            for kk in range(8):
                k.op("pe", lambda: nc.tensor.matmul(ss[b][:, :W], lhsT=self.ones_bf[:], rhs=sq[b][:, kk, :W], start=(kk == 0), stop=(kk == 7)),
                     reads=[f"n_sq{b}", "ones_bf"], writes=[f"n_ss{b}"])
            k.op("act", lambda: nc.scalar.activation(out=rstd[b][:, :W], in_=ss[b][:, :W], func=AF.Sqrt, scale=1.0 / D, bias=self.epsc[:, 0:1]),
                 reads=[f"n_ss{b}", "epsc"], writes=[f"n_rstd{b}"])
            k.op("dve", lambda: nc.vector.reciprocal(out=rstd[b][:, :W], in_=rstd[b][:, :W]),
                 reads=[f"n_rstd{b}"], writes=[f"n_rstd{b}"])
            k.op("dve", lambda: nc.vector.tensor_tensor(out=tmp[b][:, :, :W], in0=xt[b][:, :, :W],
                                                        in1=bc(rstd[b][:, :W].unsqueeze(1), [128, 8, W]), op=ALU.mult),
                 reads=[f"n_xt{b}", f"n_rstd{b}"], writes=[f"n_tmp{b}"])
            for kk in range(8):
                k.op("act", lambda: nc.scalar.activation(out=hh[b][:, kk, :W], in_=tmp[b][:, kk, :W], func=AF.Identity,
                                                         scale=A[:, kk, j:j + 1], bias=self.modT[:, sh0 + kk, j:j + 1]),
                     reads=[f"n_tmp{b}", Ak, "modT"], writes=[f"n_h{b}"])
            k.dma("pool", hv[:, :, s0:s0 + W], hh[b][:, :, :W], reads=[f"n_h{b}"], writes=["hT"])
        k.end_phase()

    def stage_inproj(self, l, tiles=TILES):
        k, nc = self.k, self.nc
        k.begin_phase()
        wst = k.sb("ip_wst", [128, 8, 1024])
        wbf = [k.sb(f"ip_wbf{i}", [128, 8, 1024], BF16) for i in range(2)]
        ht = [k.sb(f"ip_h{i}", [128, 8, 512], BF16) for i in range(2)]
        NP = 4
        pp = [k.ps(f"ip_p{i}", [128, 512]) for i in range(NP)]
        ost = [k.sb(f"ip_o{i}", [128, 512]) for i in range(NP)]
        ostb = [k.sb(f"ip_ob{i}", [128, 512], BF16) for i in range(NP)]
        hv = self.hT.rearrange("k p t -> p k t")
        cnt = 0
        hcnt = 0
        for gi, (name, c0, ncols) in enumerate(PGROUPS):
            wb = wbf[gi % 2]
            wbk = f"ip_wbf{gi % 2}"
            k.dma("sp", wst[:, :, :ncols], self.w_in[l, :, c0:c0 + ncols].rearrange("(k p) c -> p k c", p=128), writes=["ip_wst"])
            k.op("pool", lambda: nc.gpsimd.tensor_copy(out=wb[:, :, :ncols], in_=wst[:, :, :ncols]), reads=["ip_wst"], writes=[wbk])
            csz = 16 if name == "glr" else 128
            nch = ncols // csz
            for ti, (s0, W) in enumerate(tiles):
                hb = hcnt % 2
                hcnt += 1
                k.dma("sp", ht[hb][:, :, :W], hv[:, :, s0:s0 + W], reads=["hT"], writes=[f"ip_h{hb}"])
                for ch in range(nch):
                    pb = cnt % NP
                    cnt += 1
                    for kk in range(8):
                        k.op("pe", lambda: nc.tensor.matmul(pp[pb][:csz, :W], lhsT=wb[:, kk, ch * csz:(ch + 1) * csz], rhs=ht[hb][:, kk, :W],
                                                            start=(kk == 0), stop=(kk == 7)),
                             reads=[wbk, f"ip_h{hb}"], writes=[f"ip_p{pb}"])
                    gch = (c0 - {"hy": 0, "rgx": 1536, "rgg": 2048, "gqk": 2560, "gv": 3072, "glr": 3584, "glag": 3616, "mg": 4128}[name]) // csz + ch
                    src = pp[pb][:csz, :W]
                    pk = f"ip_p{pb}"
                    if name in ("hy", "rgx"):
                        dst = (self.p_hy if name == "hy" else self.p_rgx)
                        k.op("dve", lambda: nc.vector.tensor_copy(out=ost[pb][:, :W], in_=src), reads=[pk], writes=[f"ip_o{pb}"])
                        k.dma("pool", dst[gch, :, s0:s0 + W], ost[pb][:, :W], reads=[f"ip_o{pb}"], writes=["p_" + name])
                    elif name == "rgg":
                        k.op("act", lambda: nc.scalar.activation(out=ost[pb][:, :W], in_=src, func=AF.Gelu_apprx_tanh), reads=[pk], writes=[f"ip_o{pb}"])
                        k.dma("pool", self.p_rgg[gch, :, s0:s0 + W], ost[pb][:, :W], reads=[f"ip_o{pb}"], writes=["p_rgg"])
                    elif name == "glag":
                        k.op("act", lambda: nc.scalar.activation(out=ost[pb][:, :W], in_=src, func=AF.Silu), reads=[pk], writes=[f"ip_o{pb}"])
                        k.dma("pool", self.p_glag[gch, :, s0:s0 + W], ost[pb][:, :W], reads=[f"ip_o{pb}"], writes=["p_glag"])
                    elif name == "mg":
                        k.op("act", lambda: nc.scalar.activation(out=ost[pb][:, :W], in_=src, func=AF.Sigmoid, bias=self.bmerge[:, l, gch:gch + 1]),
                             reads=[pk, "bmerge"], writes=[f"ip_o{pb}"])
                        k.dma("pool", self.p_mg[gch, :, s0:s0 + W], ost[pb][:, :W], reads=[f"ip_o{pb}"], writes=["p_mg"])
                    elif name == "gqk":
                        sc = 0.125 if gch < 2 else 1.0
                        k.op("dve", lambda: nc.vector.tensor_scalar(out=ostb[pb][:, :W], in0=src, scalar1=sc, scalar2=None, op0=ALU.mult),
                             reads=[pk], writes=[f"ip_ob{pb}"])
                        k.dma("pool", self.p_gqk[gch, :, s0:s0 + W], ostb[pb][:, :W], reads=[f"ip_ob{pb}"], writes=["p_gqk"])
                    elif name == "gv":
                        k.op("dve", lambda: nc.vector.tensor_copy(out=ostb[pb][:, :W], in_=src), reads=[pk], writes=[f"ip_ob{pb}"])
                        k.dma("pool", self.p_gv[gch, :, s0:s0 + W], ostb[pb][:, :W], reads=[f"ip_ob{pb}"], writes=["p_gv"])
                    elif name == "glr":
                        k.op("dve", lambda: nc.vector.tensor_copy(out=ost[pb][:16, :W], in_=src), reads=[pk], writes=[f"ip_o{pb}"])
                        k.dma("pool", self.p_glr[gch, :, s0:s0 + W], ost[pb][:16, :W], reads=[f"ip_o{pb}"], writes=["p_glr"])
        k.end_phase()


def chunkT(v):
    v = np.asarray(v, np.float32)
    lead = v.shape[:-1]
    n = v.shape[-1] // 128
    w = v.reshape(*lead, n, 128)
    return np.ascontiguousarray(np.moveaxis(w, -1, 0))


def prep_shared(inp):
    sh = {}
    sh["w_mod"] = np.ascontiguousarray(inp["w_mod"], np.float32)
    sh["b_modT"] = chunkT(inp["b_mod"])
    sh["gmixT"] = chunkT(inp["g_norm_mix"])
    sh["gffnT"] = chunkT(inp["g_norm_ffn"])
    sh["gfinT"] = chunkT(inp["g_final"])
    sh["w_in"] = np.ascontiguousarray(inp["w_in"], np.float32)
    sh["b_mergeT"] = chunkT(inp["b_merge"])
    return sh


def prep_core(inp, b):
    m = {}
    xc = np.concatenate([inp["ctx"][b], inp["x"][b]], axis=0)
    m["xT"] = np.ascontiguousarray(xc.T.reshape(8, 128, T))
    cc = np.stack([inp["c"][b], inp["c_ctx"]], axis=-1)
    m["cT"] = np.ascontiguousarray(cc.reshape(8, 128, 2).transpose(1, 0, 2))
    return m


def declare_rg(self):
    k = self.k
    self.rg_cwT = self.inp("rg_cwT", [128, DEPTH, 2, 4, 4])
    self.rg_cbT = self.inp("rg_cbT", [128, DEPTH, 2, 4])
    self.rg_baT = self.inp("rg_baT", [128, DEPTH, 2, 4])
    self.rg_bxT = self.inp("rg_bxT", [128, DEPTH, 2, 4])
    self.rg_lamT = self.inp("rg_lamT", [128, DEPTH, 2, 4])
    self.rg_waBD = self.inp("rg_waBD", [128, DEPTH, 2, 4, 128])
    self.rg_wxBD = self.inp("rg_wxBD", [128, DEPTH, 2, 4, 128])
    self.y_rg = k.dram("y_rg", [4, 128, T], BF16)


def stage_rglru(self, l, tiles=TILES):
    k, nc = self.k, self.nc
    k.begin_phase()
    cw = k.sb("rg_cw", [128, 2, 4, 4]); cb = k.sb("rg_cb", [128, 2, 4]); ba = k.sb("rg_ba", [128, 2, 4])
    bx = k.sb("rg_bx", [128, 2, 4]); lam = k.sb("rg_lam", [128, 2, 4]); nsp = k.sb("rg_nsp", [128, 2, 4])
    wst = k.sb("rg_wst", [128, 2, 2, 4, 128]); wbd = k.sb("rg_wbd", [128, 2, 2, 4, 128], BF16)
    k.dma("sp", cw[:], self.rg_cwT[:, l], writes=["rg_cw"])
    k.dma("sp", cb[:], self.rg_cbT[:, l], writes=["rg_cb"])
    k.dma("sp", ba[:], self.rg_baT[:, l], writes=["rg_ba"])
    k.dma("sp", bx[:], self.rg_bxT[:, l], writes=["rg_bx"])
    k.dma("sp", lam[:], self.rg_lamT[:, l], writes=["rg_lam"])
    k.dma("sp", wst[:, 0], self.rg_waBD[:, l], writes=["rg_wst"])
    k.dma("sp", wst[:, 1], self.rg_wxBD[:, l], writes=["rg_wst"])
    k.op("pool", lambda: nc.gpsimd.tensor_copy(out=wbd[:], in_=wst[:]), reads=["rg_wst"], writes=["rg_wbd"])
    k.op("act", lambda: nc.scalar.activation(out=nsp[:], in_=lam[:], func=AF.Exp, scale=-1.0), reads=["rg_lam"], writes=["rg_nsp"])
    k.op("act", lambda: nc.scalar.activation(out=nsp[:], in_=nsp[:], func=AF.Ln, bias=self.onec[:, 0:1]), reads=["rg_nsp", "onec"], writes=["rg_nsp"])
    k.op("dve", lambda: nc.vector.tensor_scalar(out=nsp[:], in0=nsp[:], scalar1=-8.0, scalar2=None, op0=ALU.mult), reads=["rg_nsp"], writes=["rg_nsp"])

    u = k.sb("rg_u", [128, T]); gg = k.sb("rg_gg", [128, T]); hsum = k.sb("rg_hsum", [128, T])
    NB = 2
    xc = [k.sb(f"rg_xc{i}", [128, 512]) for i in range(NB)]
    xcb = [k.sb(f"rg_xcb{i}", [128, 512], BF16) for i in range(NB)]
    pa = [k.ps(f"rg_pa{i}", [128, 512]) for i in range(NB)]
    px = [k.ps(f"rg_px{i}", [128, 512]) for i in range(NB)]
    gr = [k.sb(f"rg_gr{i}", [128, 512]) for i in range(NB)]
    aa = [k.sb(f"rg_a{i}", [128, 512]) for i in range(NB)]
    gi = [k.sb(f"rg_gi{i}", [128, 512]) for i in range(NB)]
    t1 = [k.sb(f"rg_t1{i}", [128, 512]) for i in range(NB)]
    t3 = [k.sb(f"rg_t3{i}", [128, 512]) for i in range(NB)]
    hs = [k.sb(f"rg_hs{i}", [128, 512]) for i in range(NB)]
    yo = [k.sb(f"rg_yo{i}", [128, 512], BF16) for i in range(NB)]

    def rev(ap2d, n):
        a = ap2d
        return bass.AP(tensor=a.tensor, offset=a.offset + (n - 1) * a.ap[-1][0], ap=[list(a.ap[0]), [-a.ap[-1][0], n]])

    it = 0
    for cc in range(4):
        k.dma("sp", u[:], self.p_rgx[cc], reads=["p_rgx"], writes=["rg_u"])
        k.dma("sp", gg[:], self.p_rgg[cc], reads=["p_rgg"], writes=["rg_gg"])
        for d in range(2):
            order = list(range(len(tiles))) if d == 0 else [0] + list(range(len(tiles) - 1, 0, -1))
            prev = None
            for ti in order:
                s0, W = tiles[ti]
                seg0, seg1 = (0, NCTX) if s0 < NCTX else (NCTX, T)
                b = it % NB
                it += 1
                xk = f"rg_xc{b}"
                k.op("dve", lambda: nc.vector.tensor_scalar(out=xc[b][:, :W], in0=u[:, s0:s0 + W], scalar1=cw[:, d, cc, 3:4], scalar2=cb[:, d, cc:cc + 1],
                                                            op0=ALU.mult, op1=ALU.add), reads=["rg_u", "rg_cw", "rg_cb"], writes=[xk])
                for j in range(3):
                    sh = 3 - j
                    if d == 0:
                        lo = max(0, seg0 + sh - s0)
                        if lo >= W:
                            continue
                        o_ap = xc[b][:, lo:W]; i_ap = u[:, s0 + lo - sh:s0 + W - sh]
                    else:
                        hi = min(W, seg1 - sh - s0)
                        if hi <= 0:
                            continue
                        o_ap = xc[b][:, 0:hi]; i_ap = u[:, s0 + sh:s0 + hi + sh]
                    k.op("dve", lambda: nc.vector.scalar_tensor_tensor(out=o_ap, in0=i_ap, scalar=cw[:, d, cc, j:j + 1], in1=o_ap, op0=ALU.mult, op1=ALU.add),
                         reads=["rg_u", "rg_cw", xk], writes=[xk])
                k.op("act", lambda: nc.scalar.copy(out=xcb[b][:, :W], in_=xc[b][:, :W]), reads=[xk], writes=[f"rg_xcb{b}"])
                k.op("pe", lambda: nc.tensor.matmul(pa[b][:, :W], lhsT=wbd[:, 0, d, cc, :], rhs=xcb[b][:, :W], start=True, stop=True),
                     reads=["rg_wbd", f"rg_xcb{b}"], writes=[f"rg_pa{b}"])
                k.op("pe", lambda: nc.tensor.matmul(px[b][:, :W], lhsT=wbd[:, 1, d, cc, :], rhs=xcb[b][:, :W], start=True, stop=True),
                     reads=["rg_wbd", f"rg_xcb{b}"], writes=[f"rg_px{b}"])
                k.op("act", lambda: nc.scalar.activation(out=gr[b][:, :W], in_=pa[b][:, :W], func=AF.Sigmoid, bias=ba[:, d, cc:cc + 1]),
                     reads=[f"rg_pa{b}", "rg_ba"], writes=[f"rg_gr{b}"])
                k.op("act", lambda: nc.scalar.activation(out=gi[b][:, :W], in_=px[b][:, :W], func=AF.Sigmoid, bias=bx[:, d, cc:cc + 1]),
                     reads=[f"rg_px{b}", "rg_bx"], writes=[f"rg_gi{b}"])
                k.op("act", lambda: nc.scalar.activation(out=aa[b][:, :W], in_=gr[b][:, :W], func=AF.Exp, scale=nsp[:, d, cc:cc + 1]),
                     reads=[f"rg_gr{b}", "rg_nsp"], writes=[f"rg_a{b}"])
                k.op("dve", lambda: nc.vector.scalar_tensor_tensor(out=t1[b][:, :W], in0=aa[b][:, :W], scalar=-1.0, in1=aa[b][:, :W], op0=ALU.mult, op1=ALU.mult),
                     reads=[f"rg_a{b}"], writes=[f"rg_t1{b}"])
                k.op("act", lambda: nc.scalar.activation(out=t1[b][:, :W], in_=t1[b][:, :W], func=AF.Sqrt, bias=self.onec[:, 0:1]),
                     reads=[f"rg_t1{b}", "onec"], writes=[f"rg_t1{b}"])
                k.op("dve", lambda: nc.vector.tensor_tensor(out=t3[b][:, :W], in0=gi[b][:, :W], in1=xc[b][:, :W], op=ALU.mult),
                     reads=[f"rg_gi{b}", xk], writes=[f"rg_t3{b}"])
                k.op("dve", lambda: nc.vector.tensor_tensor(out=t3[b][:, :W], in0=t3[b][:, :W], in1=t1[b][:, :W], op=ALU.mult),
                     reads=[f"rg_t3{b}", f"rg_t1{b}"], writes=[f"rg_t3{b}"])
                init = 0.0 if prev is None else prev[0]
                rd = [f"rg_a{b}", f"rg_t3{b}"] + ([] if prev is None else [prev[1]])
                if d == 0:
                    k.op("dve", lambda: nc.vector.tensor_tensor_scan(out=hsum[:, s0:s0 + W], data0=aa[b][:, :W], data1=t3[b][:, :W], initial=init,
                                                                     op0=ALU.mult, op1=ALU.add), reads=rd, writes=["rg_hsum"])
                    prev = (hsum[:, s0 + W - 1:s0 + W], "rg_hsum")
                else:
                    k.op("dve", lambda: nc.vector.tensor_tensor_scan(out=rev(hs[b][:, :W], W), data0=rev(aa[b][:, :W], W), data1=rev(t3[b][:, :W], W),
                                                                     initial=init, op0=ALU.mult, op1=ALU.add), reads=rd, writes=[f"rg_hs{b}"])
                    prev = (hs[b][:, 0:1], f"rg_hs{b}")
                    k.op("pool", lambda: nc.gpsimd.tensor_tensor(out=t1[b][:, :W], in0=hs[b][:, :W], in1=hsum[:, s0:s0 + W], op=ALU.add),
                         reads=[f"rg_hs{b}", "rg_hsum"], writes=[f"rg_t1{b}"])
                    k.op("pool", lambda: nc.gpsimd.tensor_tensor(out=yo[b][:, :W], in0=t1[b][:, :W], in1=gg[:, s0:s0 + W], op=ALU.mult),
                         reads=[f"rg_t1{b}", "rg_gg"], writes=[f"rg_yo{b}"])
                    k.dma("sp", self.y_rg[cc, :, s0:s0 + W], yo[b][:, :W], reads=[f"rg_yo{b}"], writes=["y_rg"])
    k.end_phase()


Prog.declare_rg = declare_rg
Prog.stage_rglru = stage_rglru


def prep_rg(inp):
    sh = {}
    cw = np.asarray(inp["rg_conv_w"], np.float32)
    sh["rg_cwT"] = np.ascontiguousarray(cw.reshape(DEPTH, 2, 4, 4, 128).transpose(4, 0, 1, 3, 2))
    for nm, key in [("rg_cbT", "rg_conv_b"), ("rg_baT", "rg_ba"), ("rg_bxT", "rg_bx"), ("rg_lamT", "rg_lambda")]:
        sh[nm] = chunkT(inp[key])
    for nm, key in [("rg_waBD", "rg_wa"), ("rg_wxBD", "rg_wx")]:
        w = np.asarray(inp[key], np.float32)
        bd = np.zeros((128, DEPTH, 2, 4, 128), np.float32)
        for g in range(8):
            cc, h = g // 2, g % 2
            bd[h * 64:(h + 1) * 64, :, :, cc, h * 64:(h + 1) * 64] = w[:, :, g].transpose(2, 0, 1, 3)
        sh[nm] = bd
    return sh


def gcols(g):
    if g < 2:
        return slice(g * 128, (g + 1) * 128)
    col = g - 2
    return slice(NCTX + col, NCTX + col + 127 * 64 + 1, 64)


NGRP = 66


def declare_gla(self):
    k = self.k
    self.gla_wlr = self.inp("gla_wlr", [16, DEPTH, 2, 256])
    self.gla_nblr = self.inp("gla_blr", [64, DEPTH, 2, 4])
    self.gla_ng = self.inp("gla_ng", [128, DEPTH])
    self.c_scanmask = self.inp("c_scanmask", [64, 2, 128])
    self.c_tri = self.inp("c_tri", [128, 2, 128])
    self.y_gla = k.dram("y_gla", [4, 128, T], BF16)


def revap(a, n):
    return bass.AP(tensor=a.tensor, offset=a.offset + (n - 1) * a.ap[-1][0], ap=[list(a.ap[0]), [-a.ap[-1][0], n]])


def stage_gla(self, l, tiles=TILES, lvl=9, maxstep=NGRP):
    k, nc = self.k, self.nc
    k.begin_phase()
    wlr = k.sb("gl_wlr", [16, 2, 256]); blr = k.sb("gl_blr", [64, 2, 4]); nblr = k.sb("gl_nblr", [64, 2, 4]); ng = k.sb("gl_ng", [128, DEPTH])
    smask = k.sb("gl_smask", [64, 2, 128]); tri = k.sb("gl_tri", [128, 2, 128])
    k.dma("sp", wlr[:], self.gla_wlr[:, l], writes=["gl_wlr"])
    k.dma("sp", blr[:], self.gla_nblr[:, l], writes=["gl_blr"])
    k.dma("sp", ng[:], self.gla_ng, writes=["gl_ng"])
    k.dma("sp", smask[:], self.c_scanmask, writes=["gl_smask"])
    k.dma("sp", tri[:], self.c_tri, writes=["gl_tri"])
    k.op("dve", lambda: nc.vector.tensor_scalar(out=nblr[:], in0=blr[:], scalar1=-1.0, scalar2=None, op0=ALU.mult), reads=["gl_blr"], writes=["gl_nblr"])
    qh = k.sb("gl_q", [64, T], BF16); kh = k.sb("gl_k", [64, T], BF16); vh = k.sb("gl_v", [128, T], BF16)
    la = [k.sb(f"gl_la{d}", [64, T]) for d in range(2)]
    oacc = k.sb("gl_oacc", [128, T])
    lrt = [k.sb(f"gl_lrt{i}", [16, 512]) for i in range(2)]
    ps_la = k.ps("gl_psla", [128, 512])
    ps_T = [k.ps(f"gl_psT{d}", [128, 1024], BF16) for d in range(2)]
    ps_A = [[k.ps(f"gl_psA{d}{p}", [128, 4, 128]) for p in range(2)] for d in range(2)]

    def mk(name, shape, dt=F32):
        return [[k.sb(f"{name}{d}{p}", shape, dt) for p in range(2)] for d in range(2)]
    vT = mk("gl_vT", [128, 128], BF16); cum = mk("gl_cum", [64, 128]); eq = mk("gl_eq", [64, 128]); ek = mk("gl_ek", [64, 128])
    dec = mk("gl_dec", [64, 2]); qg = mk("gl_qg", [64, 128], BF16); kd = mk("gl_kd", [64, 128], BF16); kw = mk("gl_kw", [64, 128], BF16)
    kwT = mk("gl_kwT", [128, 2, 64], BF16); scm = mk("gl_scm", [128, 128], BF16)
    for d in range(2):
        for p in range(2):
            k.op("pool", lambda: nc.gpsimd.memset(kwT[d][p][:], 0.0), writes=[f"gl_kwT{d}{p}"])
    NS = 6
    S = [[k.sb(f"gl_S{d}_{r}", [64, 128]) for r in range(NS)] for d in range(2)]
    qgf = mk("gl_qgf", [64, 128])
    scur = [0, 0]
    fsq = k.sb("gl_fsq", [128, 512], BF16); frs = k.sb("gl_frs", [128, 512]); fgl = k.sb("gl_fgl", [128, 512]); fy = k.sb("gl_fy", [128, 512], BF16)
    orders = [list(range(NGRP)), [1, 0] + list(range(NGRP - 1, 1, -1))]
    nstep = min(NGRP, maxstep)

    def prep(step, d):
        p = step % 2
        sfx = f"{d}{p}"
        g = orders[d][step]
        cs = gcols(g)
        pT, pA = ps_T[d], ps_A[d][p]
        Tk, Ak = f"gl_psT{d}", f"gl_psA{sfx}"
        k.op("pe", lambda: nc.tensor.transpose(out=pT[:, 0:128], in_=vh[:, cs], identity=self.ident_bf[:]), reads=["gl_v", "ident_bf"], writes=[Tk])
        k.op("act", lambda: nc.scalar.copy(out=vT[d][p][:], in_=pT[:, 0:128]), writes=[Tk, f"gl_vT{sfx}"])
        lav = la[d][:, cs]
        c_ = cum[d][p]
        if d == 0:
            k.op("dve", lambda: nc.vector.tensor_tensor_scan(out=c_[:], data0=smask[:, 0, :], data1=lav, initial=0.0, op0=ALU.mult, op1=ALU.add),
                 reads=[f"gl_la{d}", "gl_smask"], writes=[f"gl_cum{sfx}"])
            cl = c_[:, 63:128:64]
        else:
            k.op("dve", lambda: nc.vector.tensor_tensor_scan(out=revap(c_[:], 128), data0=revap(smask[:, 1, :], 128), data1=revap(lav, 128),
                                                             initial=0.0, op0=ALU.mult, op1=ALU.add),
                 reads=[f"gl_la{d}", "gl_smask"], writes=[f"gl_cum{sfx}"])
            cl = c_[:, 0:128:64]
        k.op("act", lambda: nc.scalar.activation(out=eq[d][p][:], in_=c_[:], func=AF.Exp), reads=[f"gl_cum{sfx}"], writes=[f"gl_eq{sfx}"])
        k.op("act", lambda: nc.scalar.activation(out=ek[d][p][:], in_=c_[:], func=AF.Exp, scale=-1.0), reads=[f"gl_cum{sfx}"], writes=[f"gl_ek{sfx}"])
        k.op("act", lambda: nc.scalar.activation(out=dec[d][p][:], in_=cl, func=AF.Exp), reads=[f"gl_cum{sfx}"], writes=[f"gl_dec{sfx}"])
        k.op("dve", lambda: nc.vector.tensor_tensor(out=qgf[d][p][:], in0=qh[:, cs], in1=eq[d][p][:], op=ALU.mult), reads=["gl_q", f"gl_eq{sfx}"], writes=[f"gl_qgf{sfx}"])
        k.op("act", lambda: nc.scalar.copy(out=qg[d][p][:], in_=qgf[d][p][:]), reads=[f"gl_qgf{sfx}"], writes=[f"gl_qg{sfx}"])
        k.op("dve", lambda: nc.vector.tensor_tensor(out=kd[d][p][:], in0=kh[:, cs], in1=ek[d][p][:], op=ALU.mult), reads=["gl_k", f"gl_ek{sfx}"], writes=[f"gl_kd{sfx}"])
        k.op("dve", lambda: nc.vector.tensor_tensor(out=kw[d][p][:].rearrange("p (j c) -> p j c", j=2), in0=kd[d][p][:].rearrange("p (j c) -> p j c", j=2),
                                                    in1=bc(dec[d][p][:].unsqueeze(2), [64, 2, 64]), op=ALU.mult),
             reads=[f"gl_kd{sfx}", f"gl_dec{sfx}"], writes=[f"gl_kw{sfx}"])
        if lvl < 3:
            return
        k.op("pe", lambda: nc.tensor.transpose(out=pT[:, 128:192], in_=kw[d][p][:], identity=self.ident_bf[:64, :64]), reads=[f"gl_kw{sfx}", "ident_bf"], writes=[Tk])
        for j in range(2):
            k.op("act", lambda: nc.scalar.copy(out=kwT[d][p][j * 64:(j + 1) * 64, j, :], in_=pT[j * 64:(j + 1) * 64, 128:192]), writes=[Tk, f"gl_kwT{sfx}"])
        k.op("pe", lambda: nc.tensor.matmul(pA[:, 0, :], lhsT=kd[d][p][:], rhs=qg[d][p][:], start=True, stop=True),
             reads=[f"gl_kd{sfx}", f"gl_qg{sfx}"], writes=[Ak])
        k.op("dve", lambda: nc.vector.tensor_tensor(out=scm[d][p][:], in0=pA[:, 0, :], in1=tri[:, d, :], op=ALU.mult),
             reads=["gl_tri"], writes=[Ak, f"gl_scm{sfx}"])
        if lvl < 4:
            return
        for j in range(2):
            k.op("pe", lambda: nc.tensor.matmul(pA[:64, 2 + j, :], lhsT=kwT[d][p][:, j, :], rhs=vT[d][p][:], start=True, stop=True),
                 reads=[f"gl_kwT{sfx}", f"gl_vT{sfx}"], writes=[Ak])

    def state(step, d):
        p = step % 2
        sfx = f"{d}{p}"
        g = orders[d][step]
        cs = gcols(g)
        pA = ps_A[d][p]
        Ak = f"gl_psA{sfx}"
        if lvl < 4:
            return
        sidx = {}
        for j in ([0, 1] if d == 0 else [1, 0]):
            r0 = scur[d]
            r1 = (r0 + 1) % NS
            sidx[j] = r0
            k.op("dve", lambda: nc.vector.scalar_tensor_tensor(out=S[d][r1][:], in0=S[d][r0][:], scalar=dec[d][p][:, j:j + 1], in1=pA[:64, 2 + j, :], op0=ALU.mult, op1=ALU.add),
                 reads=[f"gl_S{d}_{r0}", f"gl_dec{sfx}"], writes=[Ak, f"gl_S{d}_{r1}"])
            scur[d] = r1
        if lvl < 5:
            return
        k.op("pe", lambda: nc.tensor.matmul(pA[:, 1, :], lhsT=vT[d][p][:], rhs=scm[d][p][:], start=True, stop=False),
             reads=[f"gl_vT{sfx}", f"gl_scm{sfx}"], writes=[Ak])
        for j in range(2):
            r = sidx[j]
            k.op("pe", lambda: nc.tensor.matmul(pA[:, 1, j * 64:(j + 1) * 64], lhsT=S[d][r][:], rhs=qgf[d][p][:, j * 64:(j + 1) * 64], start=False, stop=(j == 1)),
                 reads=[f"gl_S{d}_{r}", f"gl_qgf{sfx}"], writes=[Ak])
        k.op("dve", lambda: nc.vector.tensor_tensor(out=oacc[:, cs], in0=oacc[:, cs], in1=pA[:, 1, :], op=ALU.add),
             reads=["gl_oacc"], writes=[Ak, "gl_oacc"])

    for hd in range(4):
        k.dma("sp", qh[:], self.p_gqk[hd // 2, (hd % 2) * 64:(hd % 2) * 64 + 64, :], reads=["p_gqk"], writes=["gl_q"])
        k.dma("sp", kh[:], self.p_gqk[2 + hd // 2, (hd % 2) * 64:(hd % 2) * 64 + 64, :], reads=["p_gqk"], writes=["gl_k"])
        k.dma("sp", vh[:], self.p_gv[hd], reads=["p_gv"], writes=["gl_v"])
        k.op("pool", lambda: nc.gpsimd.memset(oacc[:], 0.0), writes=["gl_oacc"])
        it = 0
        for d in range(2):
            scur[d] = 0
            k.op("pool", lambda: nc.gpsimd.memset(S[d][0][:], 0.0), writes=[f"gl_S{d}_0"])
            for (s0, W) in tiles:
                b = it % 2
                it += 1
                k.dma("sp", lrt[b][:, :W], self.p_glr[d, :, s0:s0 + W], reads=["p_glr"], writes=[f"gl_lrt{b}"])
                k.op("pe", lambda: nc.tensor.matmul(ps_la[:64, :W], lhsT=wlr[:, d, hd * 64:(hd + 1) * 64], rhs=lrt[b][:, :W], start=True, stop=True),
                     reads=["gl_wlr", f"gl_lrt{b}"], writes=["gl_psla"])
                k.op("act", lambda: nc.scalar.activation(out=la[d][:, s0:s0 + W], in_=ps_la[:64, :W], func=AF.Exp, scale=-1.0, bias=nblr[:, d, hd:hd + 1]),
                     reads=["gl_nblr"], writes=["gl_psla", f"gl_la{d}"])
                k.op("act", lambda: nc.scalar.activation(out=la[d][:, s0:s0 + W], in_=la[d][:, s0:s0 + W], func=AF.Ln, bias=self.onec[:64, 0:1]),
                     reads=[f"gl_la{d}", "onec"], writes=[f"gl_la{d}"])
                k.op("dve", lambda: nc.vector.tensor_scalar(out=la[d][:, s0:s0 + W], in0=la[d][:, s0:s0 + W], scalar1=-1.0 / 16.0, scalar2=None, op0=ALU.mult),
                     reads=[f"gl_la{d}"], writes=[f"gl_la{d}"])
        if lvl >= 2:
            for d in range(2):
                prep(0, d)
            for step in range(nstep):
                if step + 1 < nstep:
                    for d in range(2):
                        prep(step + 1, d)
                for d in range(2):
                    state(step, d)
        for (s0, W) in (tiles if lvl >= 6 else []):
            k.op("act", lambda: nc.scalar.activation(out=fsq[:, :W], in_=oacc[:, s0:s0 + W], func=AF.Square), reads=["gl_oacc"], writes=["gl_fsq"])
            k.op("pe", lambda: nc.tensor.matmul(ps_la[:, :W], lhsT=self.ones_bf[:], rhs=fsq[:, :W], start=True, stop=True), reads=["gl_fsq", "ones_bf"], writes=["gl_psla"])
            k.op("act", lambda: nc.scalar.activation(out=frs[:, :W], in_=ps_la[:, :W], func=AF.Sqrt, scale=1.0 / 128.0, bias=self.epsc[:, 0:1]),
                 reads=["epsc"], writes=["gl_psla", "gl_frs"])
            k.op("dve", lambda: nc.vector.reciprocal(out=frs[:, :W], in_=frs[:, :W]), reads=["gl_frs"], writes=["gl_frs"])
            k.dma("sp", fgl[:, :W], self.p_glag[hd, :, s0:s0 + W], reads=["p_glag"], writes=["gl_fgl"])
            k.op("dve", lambda: nc.vector.scalar_tensor_tensor(out=frs[:, :W], in0=oacc[:, s0:s0 + W], scalar=ng[:, l:l + 1], in1=frs[:, :W], op0=ALU.mult, op1=ALU.mult),
                 reads=["gl_oacc", "gl_ng", "gl_frs"], writes=["gl_frs"])
            k.op("dve", lambda: nc.vector.tensor_tensor(out=fy[:, :W], in0=frs[:, :W], in1=fgl[:, :W], op=ALU.mult), reads=["gl_frs", "gl_fgl"], writes=["gl_fy"])
            k.dma("sp", self.y_gla[hd, :, s0:s0 + W], fy[:, :W], reads=["gl_fy"], writes=["y_gla"])
    k.end_phase()


Prog.declare_gla = declare_gla
Prog.stage_gla = stage_gla


def prep_gla(inp):
    sh = {}
    sh["gla_wlr"] = np.ascontiguousarray(np.asarray(inp["gla_w_lr"], np.float32).transpose(2, 0, 1, 3))
    b = np.asarray(inp["gla_b_lr"], np.float32).reshape(DEPTH, 2, 4, 64)
    sh["gla_blr"] = np.ascontiguousarray(b.transpose(3, 0, 1, 2))
    sh["gla_ng"] = np.ascontiguousarray(np.asarray(inp["gla_norm_g"], np.float32).T)
    sm = np.ones((64, 2, 128), np.float32)
    sm[:, 0, 0] = 0; sm[:, 0, 64] = 0; sm[:, 1, 127] = 0; sm[:, 1, 63] = 0
    sh["c_scanmask"] = sm
    s = np.arange(128)[:, None]; c = np.arange(128)[None, :]
    same = (s // 64) == (c // 64)
    tri = np.zeros((128, 2, 128), np.float32)
    tri[:, 0, :] = (same & (s <= c)); tri[:, 1, :] = (same & (s > c))
    sh["c_tri"] = tri
    return sh


def declare_merge(self):
    k = self.k
    self.w_bo = [self.inp(n, [DEPTH, 512, D]) for n in ("w_hy_o", "w_rg_o", "w_gla_o")]
    self.w_out = self.inp("w_out", [DEPTH, D, D])
    self.y_hy = k.dram("y_hy", [4, 128, T], BF16)


def stage_merge(self, l, xsrc, tiles=TILES):
    k, nc = self.k, self.nc
    k.begin_phase()
    wst = k.sb("mg_wst", [128, 4, 1024])
    wb = [k.sb(f"mg_wb{i}", [128, 4, 1024], BF16) for i in range(3)]
    wo = k.sb("mg_wo", [128, 8, 1024], BF16)
    for i in range(3):
        k.dma("sp", wst[:], self.w_bo[i][l].rearrange("(k p) c -> p k c", p=128), writes=["mg_wst"])
        k.op("pool", lambda: nc.gpsimd.tensor_copy(out=wb[i][:], in_=wst[:]), reads=["mg_wst"], writes=[f"mg_wb{i}"])
    for hh in range(2):
        k.dma("sp", wst[:], self.w_out[l, hh * 512:(hh + 1) * 512, :].rearrange("(k p) c -> p k c", p=128), writes=["mg_wst"])
        k.op("pool", lambda: nc.gpsimd.tensor_copy(out=wo[:, hh * 4:(hh + 1) * 4, :], in_=wst[:]), reads=["mg_wst"], writes=["mg_wo"])
    ysrc = [self.y_hy, self.y_rg, self.y_gla]
    ykeys = ["y_hy", "y_rg", "y_gla"]
    NB = 2
    yt = [[k.sb(f"mg_y{i}_{b}", [128, 4, 512], BF16) for b in range(NB)] for i in range(3)]
    gt = [k.sb(f"mg_g{b}", [128, 8, 512]) for b in range(2)]
    xt = [k.sb(f"mg_x{b}", [128, 8, 512]) for b in range(NB)]
    macc = k.sb("mg_macc", [128, 8, 512])
    tmp = [k.sb(f"mg_tmp{b}", [128, 512]) for b in range(2)]
    mb = [k.sb(f"mg_mb{b}", [128, 8, 512], BF16) for b in range(NB)]
    NP = 4
    pp = [k.ps(f"mg_p{i}", [128, 512]) for i in range(NP)]
    xv = xsrc.rearrange("k p t -> p k t")
    xo = self.xres.rearrange("k p t -> p k t")
    pc = 0
    tc_ = 0
    gc_ = 0
    for ti, (s0, W) in enumerate(tiles):
        b = ti % NB
        j = 1 if s0 < NCTX else 0
        k.dma("sp", xt[b][:, :, :W], xv[:, :, s0:s0 + W], reads=["xres"], writes=[f"mg_x{b}"])
        for i in range(3):
            k.dma("sp", yt[i][b][:, :, :W], ysrc[i].rearrange("k p t -> p k t")[:, :, s0:s0 + W], reads=[ykeys[i]], writes=[f"mg_y{i}_{b}"])
        for i in range(3):
            gb = gc_ % 2
            gc_ += 1
            k.dma("sp", gt[gb][:, :, :W], self.p_mg[i * 8:(i + 1) * 8].rearrange("k p t -> p k t")[:, :, s0:s0 + W], reads=["p_mg"], writes=[f"mg_g{gb}"])
            for dc in range(8):
                pb = pc % NP
                pc += 1
                for kk in range(4):
                    k.op("pe", lambda: nc.tensor.matmul(pp[pb][:, :W], lhsT=wb[i][:, kk, dc * 128:(dc + 1) * 128], rhs=yt[i][b][:, kk, :W], start=(kk == 0), stop=(kk == 3)),
                         reads=[f"mg_wb{i}", f"mg_y{i}_{b}"], writes=[f"mg_p{pb}"])
                if i == 0:
                    k.op("dve", lambda: nc.vector.tensor_tensor(out=macc[:, dc, :W], in0=pp[pb][:, :W], in1=gt[gb][:, dc, :W], op=ALU.mult),
                         reads=[f"mg_g{gb}"], writes=[f"mg_p{pb}", f"mg_macc{dc}"])
                else:
                    tb = tc_ % 2
                    tc_ += 1
                    k.op("dve", lambda: nc.vector.tensor_tensor(out=tmp[tb][:, :W], in0=pp[pb][:, :W], in1=gt[gb][:, dc, :W], op=ALU.mult),
                         reads=[f"mg_g{gb}"], writes=[f"mg_p{pb}", f"mg_tmp{tb}"])
                    if i == 1:
                        k.op("pool", lambda: nc.gpsimd.tensor_tensor(out=macc[:, dc, :W], in0=macc[:, dc, :W], in1=tmp[tb][:, :W], op=ALU.add),
                             reads=[f"mg_tmp{tb}"], writes=[f"mg_macc{dc}"])
                    else:
                        k.op("pool", lambda: nc.gpsimd.tensor_tensor(out=mb[b][:, dc, :W], in0=macc[:, dc, :W], in1=tmp[tb][:, :W], op=ALU.add),
                             reads=[f"mg_tmp{tb}", f"mg_macc{dc}"], writes=[f"mg_mb{b}"])
        for dc in range(8):
            pb = pc % NP
            pc += 1
            for kk in range(8):
                k.op("pe", lambda: nc.tensor.matmul(pp[pb][:, :W], lhsT=wo[:, kk, dc * 128:(dc + 1) * 128], rhs=mb[b][:, kk, :W], start=(kk == 0), stop=(kk == 7)),
                     reads=["mg_wo", f"mg_mb{b}"], writes=[f"mg_p{pb}"])
            k.op("dve", lambda: nc.vector.scalar_tensor_tensor(out=xt[b][:, dc, :W], in0=pp[pb][:, :W], scalar=self.modT[:, 16 + dc, j:j + 1], in1=xt[b][:, dc, :W],
                                                               op0=ALU.mult, op1=ALU.add),
                 reads=["modT"], writes=[f"mg_p{pb}", f"mg_x{b}"])
        k.dma("pool", xo[:, :, s0:s0 + W], xt[b][:, :, :W], reads=[f"mg_x{b}"], writes=["xres"])
    k.end_phase()


Prog.declare_merge = declare_merge
Prog.stage_merge = stage_merge


def prep_merge(inp):
    return {n: np.ascontiguousarray(inp[n], np.float32) for n in ("w_hy_o", "w_rg_o", "w_gla_o", "w_out")}


NEXP = 16384


def declare_peer(self):
    k = self.k
    self.peer_wq = self.inp("peer_wq", [DEPTH, D, 2048])
    self.peer_keysT = self.inp("peer_keysT", [128, DEPTH, 16, 128])
    self.peer_u = self.inp("peer_u", [DEPTH, NEXP, D])
    self.peer_v = self.inp("peer_v", [DEPTH, NEXP, D])
    self.c_iota16 = self.inp("c_iota16", [128, 16])
    self.uv_bf = k.dram("uv_bf", [NEXP, 2, D], BF16)


def stage_peer_prep(self, l):
    k, nc = self.k, self.nc
    k.begin_phase()
    st = [k.sb(f"pp_st{i}", [128, 4, 1024]) for i in range(3)]
    sbf = [k.sb(f"pp_bf{i}", [128, 4, 1024], BF16) for i in range(3)]
    it = 0
    dv = self.uv_bf.rearrange("(p r) two c -> p r two c", p=128)
    for which, tab in enumerate((self.peer_u, self.peer_v)):
        src = tab[l].rearrange("(p r) c -> p r c", p=128)
        for r0 in range(0, 128, 4):
            b = it % 3
            eng = ["dve", "act", "pool"][it % 3]
            it += 1
            k.dma("sp", st[b][:], src[:, r0:r0 + 4, :], writes=[f"pp_st{b}"])
            if eng == "dve":
                k.op("dve", lambda: nc.vector.tensor_copy(out=sbf[b][:], in_=st[b][:]), reads=[f"pp_st{b}"], writes=[f"pp_bf{b}"])
            elif eng == "act":
                k.op("act", lambda: nc.scalar.copy(out=sbf[b][:], in_=st[b][:]), reads=[f"pp_st{b}"], writes=[f"pp_bf{b}"])
            else:
                k.op("pool", lambda: nc.gpsimd.tensor_copy(out=sbf[b][:], in_=st[b][:]), reads=[f"pp_st{b}"], writes=[f"pp_bf{b}"])
            k.dma("act", dv[:, r0:r0 + 4, which, :], sbf[b][:], reads=[f"pp_bf{b}"], writes=["uv_bf"])
    k.end_phase()


def stage_peer(self, l, t_tiles, lvl=9):
    k, nc = self.k, self.nc
    k.begin_phase()
    wq = k.sb("pr_wq", [128, 8, 2048], BF16)
    k.begin_phase()
    wst = [k.sb(f"pr_wst{i}", [128, 8, 512]) for i in range(2)]
    for c4 in range(4):
        k.dma("sp", wst[c4 % 2][:], self.peer_wq[l, :, c4 * 512:(c4 + 1) * 512].rearrange("(k p) c -> p k c", p=128), writes=[f"pr_wst{c4 % 2}"])
        k.op("pool", lambda: nc.gpsimd.tensor_copy(out=wq[:, :, c4 * 512:(c4 + 1) * 512], in_=wst[c4 % 2][:]), reads=[f"pr_wst{c4 % 2}"], writes=["pr_wq"])
    k.end_phase()
    keysT = k.sb("pr_keysT", [128, 16, 128])
    k.dma("sp", keysT[:], self.peer_keysT[:, l], writes=["pr_keysT"])
    iota16 = k.sb("pr_iota", [128, 16])
    k.dma("sp", iota16[:], self.c_iota16, writes=["pr_iota"])
    h2 = [k.sb(f"pr_h2{i}", [128, 8, 128], BF16) for i in range(2)]
    xt = [k.sb(f"pr_x{i}", [128, 8, 128]) for i in range(2)]
    qT = k.sb("pr_qT", [128, 16, 128])
    s_all = [k.sb(f"pr_s{i}", [128, 16, 128]) for i in range(2)]
    work = k.sb("pr_work", [128, 16, 128])
    s_top = [k.sb(f"pr_stop{i}", [128, 16, 16]) for i in range(2)]
    i_top = [k.sb(f"pr_itop{i}", [128, 16, 16], U32) for i in range(2)]
    i_f = k.sb("pr_if", [128, 16, 16])
    cand = k.sb("pr_cand", [128, 8, 256]); cwork = work[:].rearrange("p (h x) n -> p h (x n)", h=8)
    best = k.sb("pr_best", [128, 8, 16]); pos = k.sb("pr_pos", [128, 8, 16], U32)
    pa_u = k.sb("pr_pau", [128, 8, 16], U32); pb_u = k.sb("pr_pbu", [128, 8, 16], U32)
    pa_f = k.sb("pr_paf", [128, 8, 16]); pb_f = k.sb("pr_pbf", [128, 8, 16])
    oh = k.sb("pr_oh", [128, 8, 16, 16]); oh2 = k.sb("pr_oh2", [128, 8, 16, 16])
    i1sel = k.sb("pr_i1sel", [128, 8, 16]); i2sel = k.sb("pr_i2sel", [128, 8, 16])
    e_f = k.sb("pr_ef", [128, 128]); e_i = k.sb("pr_ei", [128, 128], I32)
    ex = k.sb("pr_ex", [128, 8, 16]); zz = k.sb("pr_zz", [128, 8]); wgt = k.sb("pr_wgt", [128, 128])
    h2tm = [k.sb(f"pr_h2tm{i}", [128, 1024], BF16) for i in range(2)]
    BS = 8
    NBLK = 128 // BS
    gb = [[k.sb(f"pr_g{a}_{i}", [128, 2048], BF16) for i in range(BS)] for a in range(2)]
    junk = k.sb("pr_junk", [128, 1024], BF16)
    junk2 = k.sb("pr_junk2", [128, 1024], BF16)
    ptmp = [k.sb(f"pr_ptmp{i}", [128, 1024], BF16) for i in range(2)]
    act_ = k.sb("pr_act", [128, 128]); cg = k.sb("pr_cg", [128, 128]); coef = k.sb("pr_coef", [128, 128])
    dg = [k.sb(f"pr_dg{i}", [128, 128], BF16) for i in range(4)]
    po_sb = k.sb("pr_posb", [128, 1024])
    psA = [k.ps(f"pr_psA{i}", [128, 4, 128]) for i in range(4)]
    psB = k.ps("pr_psB", [128, 8, 128], BF16)
    psO = [k.ps(f"pr_psO{i}", [128, 512]) for i in range(2)]
    hv = self.hT.rearrange("k p t -> p k t")
    xo = self.xres.rearrange("k p t -> p k t")
    uvt = self.uv_bf.rearrange("e two c -> e (two c)")
    NTL = len(t_tiles)

    def front_a(i):
        t0 = t_tiles[i]
        b = i % 2
        k.dma("sp", h2[b][:], hv[:, :, t0:t0 + 128], reads=["hT"], writes=[f"pr_h2{b}"])
        k.dma("sp", xt[b][:], xo[:, :, t0:t0 + 128], reads=["xres"], writes=[f"pr_x{b}"])
        for hp in range(16):
            pa = psA[(hp // 4) % 4]
            pk = f"pr_psA{(hp // 4) % 4}"
            for kk in range(8):
                k.op("pe", lambda: nc.tensor.matmul(pa[:, hp % 4, :], lhsT=wq[:, kk, hp * 128:(hp + 1) * 128], rhs=h2[b][:, kk, :], start=(kk == 0), stop=(kk == 7)),
                     reads=["pr_wq", f"pr_h2{b}"], writes=[pk])
            if hp % 4 == 3:
                k.op("act", lambda: nc.scalar.copy(out=qT[:, hp - 3:hp + 1, :], in_=pa[:]), writes=[pk, "pr_qT"])
        for hp in range(16):
            pa = psA[(hp // 4) % 4]
            pk = f"pr_psA{(hp // 4) % 4}"
            k.op("pe", lambda: nc.tensor.matmul(pa[:, hp % 4, :], lhsT=qT[:, hp, :], rhs=keysT[:, hp, :], start=True, stop=True),
                 reads=["pr_qT", "pr_keysT"], writes=[pk])
            if hp % 4 == 3:
                k.op("act", lambda: nc.scalar.copy(out=s_all[b][:, hp - 3:hp + 1, :], in_=pa[:]), writes=[pk, f"pr_s{b}"])
        for kk in range(8):
            k.op("pe", lambda: nc.tensor.transpose(out=psB[:, kk, :], in_=h2[b][:, kk, :], identity=self.ident_bf[:]), reads=[f"pr_h2{b}", "ident_bf"], writes=["pr_psB"])
        k.op("act", lambda: nc.scalar.copy(out=h2tm[b][:], in_=psB[:].rearrange("p k c -> p (k c)")), writes=["pr_psB", f"pr_h2tm{b}"])

    def topk1(i, hp):
        b = i % 2
        sa, st_, it_ = s_all[b], s_top[b], i_top[b]
        k.op("dve", lambda: nc.vector.max(out=st_[:, hp, 0:8], in_=sa[:, hp, :]), reads=[f"pr_s{b}"], writes=[f"pr_stop{b}"])
        k.op("dve", lambda: nc.vector.max_index(out=it_[:, hp, 0:8], in_max=st_[:, hp, 0:8], in_values=sa[:, hp, :]), reads=[f"pr_s{b}", f"pr_stop{b}"], writes=[f"pr_itop{b}"])
        k.op("dve", lambda: nc.vector.match_replace(out=work[:, hp, :], in_to_replace=st_[:, hp, 0:8], in_values=sa[:, hp, :], imm_value=-1e30),
             reads=[f"pr_s{b}", f"pr_stop{b}"], writes=["pr_work"])
        k.op("dve", lambda: nc.vector.max(out=st_[:, hp, 8:16], in_=work[:, hp, :]), reads=["pr_work"], writes=[f"pr_stop{b}"])
        k.op("dve", lambda: nc.vector.max_index(out=it_[:, hp, 8:16], in_max=st_[:, hp, 8:16], in_values=work[:, hp, :]), reads=["pr_work", f"pr_stop{b}"], writes=[f"pr_itop{b}"])

    def front_b(i):
        b = i % 2
        k.op("dve", lambda: nc.vector.tensor_copy(out=i_f[:], in_=i_top[b][:]), reads=[f"pr_itop{b}"], writes=["pr_if"])
        st4 = s_top[b][:].rearrange("p (h two) a -> p h two a", two=2)
        if4 = i_f[:].rearrange("p (h two) a -> p h two a", two=2)
        cand4 = cand[:].rearrange("p h (a b) -> p h a b", a=16)
        k.op("dve", lambda: nc.vector.tensor_tensor(out=cand4, in0=bc(st4[:, :, 0, :].unsqueeze(3), [128, 8, 16, 16]), in1=bc(st4[:, :, 1, :].unsqueeze(2), [128, 8, 16, 16]), op=ALU.add),
             reads=[f"pr_stop{b}"], writes=["pr_cand"])
        for h in range(8):
            k.op("dve", lambda: nc.vector.max(out=best[:, h, 0:8], in_=cand[:, h, :]), reads=["pr_cand"], writes=["pr_best"])
            k.op("dve", lambda: nc.vector.max_index(out=pos[:, h, 0:8], in_max=best[:, h, 0:8], in_values=cand[:, h, :]), reads=["pr_cand", "pr_best"], writes=["pr_pos"])
            k.op("dve", lambda: nc.vector.match_replace(out=cwork[:, h, :], in_to_replace=best[:, h, 0:8], in_values=cand[:, h, :], imm_value=-1e30),
                 reads=["pr_cand", "pr_best"], writes=["pr_work"])
            k.op("dve", lambda: nc.vector.max(out=best[:, h, 8:16], in_=cwork[:, h, :]), reads=["pr_work"], writes=["pr_best"])
            k.op("dve", lambda: nc.vector.max_index(out=pos[:, h, 8:16], in_max=best[:, h, 8:16], in_values=cwork[:, h, :]), reads=["pr_work", "pr_best"], writes=["pr_pos"])
        k.op("dve", lambda: nc.vector.tensor_single_scalar(out=pa_u[:], in_=pos[:], scalar=4, op=ALU.logical_shift_right), reads=["pr_pos"], writes=["pr_pau"])
        k.op("dve", lambda: nc.vector.tensor_single_scalar(out=pb_u[:], in_=pos[:], scalar=15, op=ALU.bitwise_and), reads=["pr_pos"], writes=["pr_pbu"])
        k.op("dve", lambda: nc.vector.tensor_copy(out=pa_f[:], in_=pa_u[:]), reads=["pr_pau"], writes=["pr_paf"])
        k.op("dve", lambda: nc.vector.tensor_copy(out=pb_f[:], in_=pb_u[:]), reads=["pr_pbu"], writes=["pr_pbf"])
        io4 = bc(iota16[:].unsqueeze(1).unsqueeze(1), [128, 8, 16, 16])
        for (pf, pfk, half, sel, selk) in ((pa_f, "pr_paf", 0, i1sel, "pr_i1sel"), (pb_f, "pr_pbf", 1, i2sel, "pr_i2sel")):
            k.op("dve", lambda: nc.vector.tensor_tensor(out=oh[:], in0=bc(pf[:].unsqueeze(3), [128, 8, 16, 16]), in1=io4, op=ALU.is_equal),
                 reads=[pfk, "pr_iota"], writes=["pr_oh"])
            k.op("dve", lambda: nc.vector.tensor_tensor(out=oh2[:], in0=oh[:], in1=bc(if4[:, :, half, :].unsqueeze(2), [128, 8, 16, 16]), op=ALU.mult),
                 reads=["pr_oh", "pr_if"], writes=["pr_oh2"])
            k.op("dve", lambda: nc.vector.tensor_reduce(out=sel[:], in_=oh2[:], axis=AX.X, op=ALU.add), reads=["pr_oh2"], writes=[selk])
        k.op("dve", lambda: nc.vector.scalar_tensor_tensor(out=e_f[:], in0=i1sel[:].rearrange("p h r -> p (h r)"), scalar=128.0, in1=i2sel[:].rearrange("p h r -> p (h r)"),
                                                           op0=ALU.mult, op1=ALU.add), reads=["pr_i1sel", "pr_i2sel"], writes=["pr_ef"])
        k.op("dve", lambda: nc.vector.tensor_copy(out=e_i[:], in_=e_f[:]), reads=["pr_ef"], writes=["pr_ei"])
        k.op("dve", lambda: nc.vector.tensor_tensor(out=ex[:], in0=best[:], in1=bc(best[:, :, 0:1], [128, 8, 16]), op=ALU.subtract), reads=["pr_best"], writes=["pr_ex"])
        k.op("act", lambda: nc.scalar.activation(out=ex[:], in_=ex[:], func=AF.Exp), reads=["pr_ex"], writes=["pr_ex"])
        k.op("dve", lambda: nc.vector.tensor_reduce(out=zz[:], in_=ex[:], axis=AX.X, op=ALU.add), reads=["pr_ex"], writes=["pr_zz"])
        k.op("dve", lambda: nc.vector.reciprocal(out=zz[:], in_=zz[:]), reads=["pr_zz"], writes=["pr_zz"])
        k.op("dve", lambda: nc.vector.tensor_tensor(out=wgt[:].rearrange("p (h r) -> p h r", h=8), in0=ex[:], in1=bc(zz[:].unsqueeze(2), [128, 8, 16]), op=ALU.mult),
             reads=["pr_ex", "pr_zz"], writes=["pr_wgt"])

    def gathers(j):
        a = j % 2
        for si in range(BS):
            s_ = j * BS + si
            k.gather(gb[a][si][:], uvt, e_i[:, s_:s_ + 1], reads=["uv_bf", "pr_ei"], writes=[f"pr_g{a}_{si}"])

    def loop(i):
        b = i % 2
        gathers(0)
        for j in range(NBLK):
            a = j % 2
            if j + 1 < NBLK:
                gathers(j + 1)
            for si in range(BS):
                s_ = j * BS + si
                if si == BS - 1:
                    k.op("pool", lambda: nc.gpsimd.tensor_tensor(out=ptmp[a][:], in0=gb[a][si][:, 0:1024], in1=h2tm[b][:], op=ALU.mult),
                         reads=[f"pr_g{a}_{si}", f"pr_h2tm{b}"], writes=[f"pr_ptmp{a}"])
                    k.op("act", lambda: nc.scalar.activation(out=junk2[:], in_=ptmp[a][:], func=AF.Copy, accum_out=act_[:, s_:s_ + 1]),
                         reads=[f"pr_ptmp{a}"], writes=["pr_junk2", f"pr_act{a}"])
                    continue
                k.op("dve", lambda: nc.vector.scalar_tensor_tensor(out=junk[:], in0=gb[a][si][:, 0:1024], scalar=1.0, in1=h2tm[b][:], op0=ALU.mult, op1=ALU.mult, accum_out=act_[:, s_:s_ + 1]),
                     reads=[f"pr_g{a}_{si}", f"pr_h2tm{b}"], writes=["pr_junk", f"pr_act{a}"])
            sl = slice(j * BS, (j + 1) * BS)
            k.op("act", lambda: nc.scalar.activation(out=cg[:, sl], in_=act_[:, sl], func=AF.Gelu_apprx_tanh), reads=[f"pr_act{a}"], writes=[f"pr_cg{a}"])
            k.op("dve", lambda: nc.vector.tensor_tensor(out=coef[:, sl], in0=cg[:, sl], in1=wgt[:, sl], op=ALU.mult), reads=[f"pr_cg{a}", "pr_wgt"], writes=[f"pr_coef{a}"])
            if i + 1 < NTL and j < 16:
                topk1(i + 1, j)
            for si in range(BS):
                s_ = j * BS + si
                dgi = s_ % 4
                k.op("act", lambda: nc.scalar.activation(out=dg[dgi][:], in_=self.ident_bf[:], func=AF.Copy, scale=coef[:, s_:s_ + 1]),
                     reads=["ident_bf", f"pr_coef{a}"], writes=[f"pr_dg{dgi}"])
                for hf in range(2):
                    k.op("pe", lambda: nc.tensor.matmul(psO[hf][:], lhsT=dg[dgi][:], rhs=gb[a][si][:, 1024 + hf * 512:1024 + (hf + 1) * 512], start=(s_ == 0), stop=(s_ == 127)),
                         reads=[f"pr_dg{dgi}", f"pr_g{a}_{si}"], writes=[f"pr_psO{hf}"])

    def back(i):
        t0 = t_tiles[i]
        b = i % 2
        j = 1 if t0 < NCTX else 0
        for hf in range(2):
            k.op("act", lambda: nc.scalar.copy(out=po_sb[:, hf * 512:(hf + 1) * 512], in_=psO[hf][:]), writes=[f"pr_psO{hf}", "pr_posb"])
        for dc in range(8):
            pa = psA[dc // 4]
            pk = f"pr_psA{dc // 4}"
            k.op("pe", lambda: nc.tensor.transpose(out=pa[:, dc % 4, :], in_=po_sb[:, dc * 128:(dc + 1) * 128], identity=self.ident[:]), reads=["pr_posb", "ident"], writes=[pk])
            k.op("dve", lambda: nc.vector.scalar_tensor_tensor(out=xt[b][:, dc, :], in0=pa[:, dc % 4, :], scalar=self.modT[:, 40 + dc, j:j + 1], in1=xt[b][:, dc, :],
                                                               op0=ALU.mult, op1=ALU.add), reads=["modT"], writes=[pk, f"pr_x{b}"])
        k.dma("sp", xo[:, :, t0:t0 + 128], xt[b][:], reads=[f"pr_x{b}"], writes=["xres"])

    front_a(0)
    for hp in range(16):
        topk1(0, hp)
    front_b(0)
    for i in range(NTL):
        if i + 1 < NTL:
            front_a(i + 1)
        loop(i)
        back(i)
        if i + 1 < NTL:
            front_b(i + 1)
    k.end_phase()


Prog.declare_peer = declare_peer
Prog.stage_peer_prep = stage_peer_prep
Prog.stage_peer = stage_peer


def prep_peer(inp):
    sh = {}
    sh["peer_wq"] = np.ascontiguousarray(inp["peer_wq"], np.float32)
    ks = np.asarray(inp["peer_keys"], np.float32).reshape(DEPTH, 16, 128, 128)
    sh["peer_keysT"] = np.ascontiguousarray(ks.transpose(3, 0, 1, 2))
    sh["peer_u"] = np.ascontiguousarray(inp["peer_u"], np.float32)
    sh["peer_v"] = np.ascontiguousarray(inp["peer_v"], np.float32)
    sh["c_iota16"] = np.tile(np.arange(16, dtype=np.float32)[None, :], (128, 1))
    return sh


HY_W = 512
MAGIC = 12582912.0
TWO_PI_LO = 6.283185


def declare_hyena(self):
    k = self.k
    self.hy_cwT = self.inp("hy_cwT", [128, DEPTH, 12, 3])
    self.hy_cbT = self.inp("hy_cbT", [128, DEPTH, 12])
    self.hy_w1 = self.inp("hy_w1", [33, DEPTH, 64])
    self.hy_w2 = self.inp("hy_w2", [64, DEPTH, 64])
    self.hy_w3 = self.inp("hy_w3", [64, DEPTH, 2048])
    self.hy_b1 = self.inp("hy_b1T", [64, DEPTH]); self.hy_b2 = self.inp("hy_b2T", [64, DEPTH]); self.hy_fr = self.inp("hy_frT", [64, DEPTH])
    self.hy_skipb = self.inp("hy_skipb", [128, DEPTH, 2, 512])
    self.c_F1 = self.inp("c_F1", [128, 512], BF16)
    self.c_TT = self.inp("c_TT", [128, 2, 512])
    self.c_BD = self.inp("c_BD", [128, 4, 128], BF16)
    self.c_R = self.inp("c_R", [128, 2, 256], BF16)
    self.c_TAB = self.inp("c_TAB", [128, 2, 512])
    self.c_ICS = self.inp("c_ICS", [128, 2, 2, 128], BF16)
    self.c_feat_lat = self.inp("c_feat_lat", [33, NLAT])
    self.c_feat_ctx = self.inp("c_feat_ctx", [33, NCTX])
    self.c_win_lat = self.inp("c_win_lat", [4, 128, 128, 64])
    self.c_win_ctx = self.inp("c_win_ctx", [4, 4, 128, 64])
    self.c_F1c = self.inp("c_F1c", [4, 16], BF16)
    self.c_TTc = self.inp("c_TTc", [128, 2, 512])
    self.c_TABc = self.inp("c_TABc", [8, 2, 512])
    self.c_ICSc = self.inp("c_ICSc", [8, 2, 4], BF16)
    self.spec = [k.dram("spec0", [2, 4, 64, 128, 2, 512]),
                 k.dram("spec1", [2, 4, 2, 128, 2, 512])]


def swap_view(ap2d, half):
    a = ap2d
    st = a.ap[-1][0]
    return bass.AP(tensor=a.tensor, offset=a.offset + half * st, ap=[list(a.ap[0]), [-half * st, 2], [st, half]])


def swap_view_ri(ap2d):
    a = ap2d
    st = a.ap[-1][0]
    return bass.AP(tensor=a.tensor, offset=a.offset + 128 * st, ap=[list(a.ap[0]), [256 * st, 2], [-128 * st, 2], [st, 128]])


class HyCtx:
    pass


def hy_setup(self, l, nbanks=7, bankb=True, NT=6, NA=8, src=0, conv=True):
    k, nc = self.k, self.nc
    h = HyCtx()
    h.BD = k.sb("hy_BD", [128, 4, 128], BF16); k.dma("sp", h.BD[:], self.c_BD, writes=["hy_BD"])
    if src == 0:
        h.F1 = k.sb("hy_F1", [128, 512], BF16); k.dma("sp", h.F1[:], self.c_F1, writes=["hy_F1"])
        h.TT = k.sb("hy_TT", [128, 2, 512]); k.dma("sp", h.TT[:], self.c_TT, writes=["hy_TT"])
    else:
        h.F1c = k.sb("hy_F1c", [4, 16], BF16); k.dma("sp", h.F1c[:], self.c_F1c, writes=["hy_F1c"])
        h.TTc = k.sb("hy_TTc", [128, 2, 512]); k.dma("sp", h.TTc[:], self.c_TTc, writes=["hy_TTc"])
    if conv:
        h.R = k.sb("hy_R", [128, 2, 256], BF16); k.dma("sp", h.R[:], self.c_R, writes=["hy_R"])
        if src == 0:
            h.TAB = k.sb("hy_TAB", [128, 2, 512]); k.dma("sp", h.TAB[:], self.c_TAB, writes=["hy_TAB"])
            h.ICS = k.sb("hy_ICS", [128, 2, 2, 128], BF16); k.dma("sp", h.ICS[:], self.c_ICS, writes=["hy_ICS"])
        else:
            h.TABc = k.sb("hy_TABc", [8, 2, 512]); k.dma("sp", h.TABc[:], self.c_TABc, writes=["hy_TABc"])
            h.ICSc = k.sb("hy_ICSc", [8, 2, 4], BF16); k.dma("sp", h.ICSc[:], self.c_ICSc, writes=["hy_ICSc"])
    h.bank = [k.ps(f"hy_bank{i}", [128, 512]) for i in range(nbanks)]
    if bankb:
        h.bankb = k.ps("hy_bankb", [128, 1024], BF16)
    h.NT = NT
    h.NA = NA
    h.tt = [k.sb(f"hy_t{i}", [128, 512]) for i in range(h.NT)]
    h.uu = [k.sb(f"hy_u{i}", [128, 512]) for i in range(h.NT)]
    h.Ap = [k.sb(f"hy_Ap{i}", [128, 512], BF16) for i in range(h.NA)]
    h.cnt = 0
    h.acnt = 0
    return h


def hy_cmul(self, h, src_ps, src_key, tabA, tabB, tab_reads, out_ap, out_key, ri_layout=False, npart=128):
    k, nc = self.k, self.nc
    i = h.cnt % h.NT
    h.cnt += 1
    t, u = h.tt[i][:npart], h.uu[i][:npart]
    if ri_layout:
        sv = swap_view_ri(src_ps)
        k.op("dve", lambda: nc.vector.tensor_tensor(out=t[:], in0=src_ps, in1=tabA, op=ALU.mult), reads=tab_reads, writes=[src_key, f"hy_t{i}"])
        k.op("dve", lambda: nc.vector.tensor_tensor(out=u[:].rearrange("p (j r c) -> p j r c", j=2, r=2), in0=sv, in1=tabB.rearrange("p (j r c) -> p j r c", j=2, r=2), op=ALU.mult),
             reads=tab_reads, writes=[src_key, f"hy_u{i}"])
    else:
        sv = swap_view(src_ps, 256)
        k.op("dve", lambda: nc.vector.tensor_tensor(out=t[:], in0=src_ps, in1=tabA, op=ALU.mult), reads=tab_reads, writes=[src_key, f"hy_t{i}"])
        k.op("dve", lambda: nc.vector.tensor_tensor(out=u[:].rearrange("p (r c) -> p r c", r=2), in0=sv, in1=tabB.rearrange("p (r c) -> p r c", r=2), op=ALU.mult),
             reads=tab_reads, writes=[src_key, f"hy_u{i}"])
    k.op("pool", lambda: nc.gpsimd.tensor_tensor(out=out_ap, in0=t[:], in1=u[:], op=ALU.add), reads=[f"hy_t{i}", f"hy_u{i}"], writes=[out_key])


def hy_s12(self, h, lhsT_ap, lhsT_key, b1):
    k, nc = self.k, self.nc
    ps1 = h.bank[b1]
    k.op("pe", lambda: nc.tensor.matmul(ps1[:], lhsT=lhsT_ap, rhs=h.F1[:], start=True, stop=True), reads=[lhsT_key, "hy_F1"], writes=[f"hy_bank{b1}"])
    ai = h.acnt % h.NA
    h.acnt += 1
    hy_cmul(self, h, ps1[:], f"hy_bank{b1}", h.TT[:, 0, :], h.TT[:, 1, :], ["hy_TT"], h.Ap[ai][:], f"hy_Ap{ai}")
    return ai


def hy_s12_ctx(self, h, W2d, wkey, bt, b1):
    k, nc = self.k, self.nc
    ps1 = h.bank[b1]
    o4 = ps1[:].rearrange("p (r pr kb) -> p r pr kb", r=2, pr=32)
    f1 = h.F1c[:].rearrange("p (r kb) -> p r kb", r=2)
    for p in range(32):
        pr = bt * 32 + p
        k.op("pe", lambda: nc.tensor.matmul(o4[:, :, p, :], lhsT=W2d[0:4, pr * 128:(pr + 1) * 128], rhs=f1, start=True, stop=True),
             reads=[wkey(pr), "hy_F1c"], writes=[f"hy_bank{b1}"])
    ai = h.acnt % h.NA
    h.acnt += 1
    hy_cmul(self, h, ps1[:], f"hy_bank{b1}", h.TTc[:, 0, :], h.TTc[:, 1, :], ["hy_TTc"], h.Ap[ai][:], f"hy_Ap{ai}")
    return ai


def hy_s3(self, h, ai, b3):
    k, nc = self.k, self.nc
    ps3 = h.bank[b3]
    Ap = h.Ap[ai]
    ak = f"hy_Ap{ai}"
    k.op("pe", lambda: nc.tensor.matmul(ps3[:, 0:256], lhsT=h.BD[:, 0, :], rhs=Ap[:, 0:256], start=True, stop=False), reads=[ak, "hy_BD"], writes=[f"hy_bank{b3}"])
    k.op("pe", lambda: nc.tensor.matmul(ps3[:, 0:256], lhsT=h.BD[:, 1, :], rhs=Ap[:, 256:512], start=False, stop=True), reads=[ak, "hy_BD"], writes=[f"hy_bank{b3}"])
    k.op("pe", lambda: nc.tensor.matmul(ps3[:, 256:512], lhsT=h.BD[:, 0, :], rhs=Ap[:, 256:512], start=True, stop=False), reads=[ak, "hy_BD"], writes=[f"hy_bank{b3}"])
    k.op("pe", lambda: nc.tensor.matmul(ps3[:, 256:512], lhsT=h.BD[:, 2, :], rhs=Ap[:, 0:256], start=False, stop=True), reads=[ak, "hy_BD"], writes=[f"hy_bank{b3}"])


def hy_s3_fb(self, h, af, ab, b3):
    k, nc = self.k, self.nc
    ps3 = h.bank[b3]
    Af, Ab = h.Ap[af], h.Ap[ab]
    rd = [f"hy_Ap{af}", f"hy_Ap{ab}", "hy_BD"]
    wk = [f"hy_bank{b3}"]
    k.op("pe", lambda: nc.tensor.matmul(ps3[:, 0:256], lhsT=h.BD[:, 0, :], rhs=Af[:, 0:256], start=True, stop=False), reads=rd, writes=wk)
    k.op("pe", lambda: nc.tensor.matmul(ps3[:, 0:256], lhsT=h.BD[:, 1, :], rhs=Af[:, 256:512], start=False, stop=False), reads=rd, writes=wk)
    k.op("pe", lambda: nc.tensor.matmul(ps3[:, 0:256], lhsT=h.BD[:, 0, :], rhs=Ab[:, 0:256], start=False, stop=False), reads=rd, writes=wk)
    k.op("pe", lambda: nc.tensor.matmul(ps3[:, 0:256], lhsT=h.BD[:, 1, :], rhs=Ab[:, 256:512], start=False, stop=True), reads=rd, writes=wk)
    k.op("pe", lambda: nc.tensor.matmul(ps3[:, 256:512], lhsT=h.BD[:, 0, :], rhs=Af[:, 256:512], start=True, stop=False), reads=rd, writes=wk)
    k.op("pe", lambda: nc.tensor.matmul(ps3[:, 256:512], lhsT=h.BD[:, 2, :], rhs=Af[:, 0:256], start=False, stop=False), reads=rd, writes=wk)
    k.op("pe", lambda: nc.tensor.matmul(ps3[:, 256:512], lhsT=h.BD[:, 3, :], rhs=Ab[:, 256:512], start=False, stop=False), reads=rd, writes=wk)
    k.op("pe", lambda: nc.tensor.matmul(ps3[:, 256:512], lhsT=h.BD[:, 1, :], rhs=Ab[:, 0:256], start=False, stop=True), reads=rd, writes=wk)


def hy_gtab(self, h, b3, g, gk):
    k, nc = self.k, self.nc
    ps3 = h.bank[b3]
    bk = f"hy_bank{b3}"
    k.op("act", lambda: nc.scalar.copy(out=g[:, 0, :].rearrange("p (r c) -> p r c", r=2), in_=bc(ps3[:, 0:256].unsqueeze(1), [128, 2, 256])), writes=[bk, gk])
    k.op("act", lambda: nc.scalar.mul(out=g[:, 1, 0:256], in_=ps3[:, 256:512], mul=-1.0), writes=[bk, gk])
    k.op("act", lambda: nc.scalar.copy(out=g[:, 1, 256:512], in_=ps3[:, 256:512]), writes=[bk, gk])


def stage_hyena_filters(self, l, src):
    k, nc = self.k, self.nc
    n = NLAT if src == 0 else NCTX
    NP = n // 64
    k.begin_phase()
    h = hy_setup(self, l, nbanks=8, bankb=False, NT=4, NA=6, src=src, conv=False)
    w3b = k.sb("hf_w3b", [64, 2048], BF16)
    h2b = k.sb("hf_h2b", [64, n], BF16)
    k.begin_phase()
    w1 = k.sb("hf_w1", [33, 64]); w2 = k.sb("hf_w2", [64, 64]); w3 = k.sb("hf_w3", [64, 2048])
    pv = k.sb("hf_pv", [64, 3, DEPTH])
    k.dma("sp", w1[:], self.hy_w1[:, l], writes=["hf_w1"]); k.dma("sp", w2[:], self.hy_w2[:, l], writes=["hf_w2"]); k.dma("sp", w3[:], self.hy_w3[:, l], writes=["hf_w3"])
    k.dma("sp", pv[:, 0, :], self.hy_b1, writes=["hf_pv"]); k.dma("sp", pv[:, 1, :], self.hy_b2, writes=["hf_pv"]); k.dma("sp", pv[:, 2, :], self.hy_fr, writes=["hf_pv"])
    k.op("pool", lambda: nc.gpsimd.tensor_copy(out=w3b[:], in_=w3[:]), reads=["hf_w3"], writes=["hf_w3b"])
    sc = k.sb("hf_sc", [64, 3])
    k.op("dve", lambda: nc.vector.tensor_scalar(out=sc[:, 0:1], in0=pv[:, 2, l:l + 1], scalar1=1.0 / (2 * math.pi), scalar2=None, op0=ALU.mult), reads=["hf_pv"], writes=["hf_sc"])
    for i in range(2):
        k.op("dve", lambda: nc.vector.tensor_tensor(out=sc[:, 1 + i:2 + i], in0=pv[:, i, l:l + 1], in1=sc[:, 0:1], op=ALU.mult), reads=["hf_pv", "hf_sc"], writes=["hf_sc"])
    ft = [k.sb(f"hf_ft{i}", [33, 512]) for i in range(2)]
    yy = [k.sb(f"hf_y{i}", [64, 512]) for i in range(2)]
    rr = [k.sb(f"hf_r{i}", [64, 512]) for i in range(2)]
    h1 = [k.sb(f"hf_h1{i}", [64, 512]) for i in range(2)]
    feat = self.c_feat_lat if src == 0 else self.c_feat_ctx
    TW = min(512, n)
    for ti in range(n // TW):
        b = ti % 2
        s0 = ti * TW
        k.dma("sp", ft[b][:, :TW], feat[:, s0:s0 + TW], writes=[f"hf_ft{b}"])
        cur_in, cur_key, wmat, wkey = ft[b][:, :TW], f"hf_ft{b}", w1, "hf_w1"
        for layer in range(2):
            pb = h.bank[layer]
            k.op("pe", lambda: nc.tensor.matmul(pb[:64, :TW], lhsT=wmat[:], rhs=cur_in, start=True, stop=True), reads=[cur_key, wkey], writes=[f"hy_bank{layer}"])
            k.op("act", lambda: nc.scalar.activation(out=yy[b][:, :TW], in_=pb[:64, :TW], func=AF.Identity, scale=sc[:, 0:1], bias=sc[:, 1 + layer:2 + layer]),
                 reads=["hf_sc"], writes=[f"hy_bank{layer}", f"hf_y{b}"])
            k.op("dve", lambda: nc.vector.tensor_scalar(out=rr[b][:, :TW], in0=yy[b][:, :TW], scalar1=MAGIC, scalar2=None, op0=ALU.add), reads=[f"hf_y{b}"], writes=[f"hf_r{b}"])
            k.op("dve", lambda: nc.vector.tensor_scalar(out=rr[b][:, :TW], in0=rr[b][:, :TW], scalar1=MAGIC, scalar2=None, op0=ALU.subtract), reads=[f"hf_r{b}"], writes=[f"hf_r{b}"])
            k.op("dve", lambda: nc.vector.tensor_tensor(out=yy[b][:, :TW], in0=yy[b][:, :TW], in1=rr[b][:, :TW], op=ALU.subtract), reads=[f"hf_y{b}", f"hf_r{b}"], writes=[f"hf_y{b}"])
            if layer == 0:
                k.op("act", lambda: nc.scalar.activation(out=h1[b][:, :TW], in_=yy[b][:, :TW], func=AF.Sin, scale=TWO_PI_LO), reads=[f"hf_y{b}"], writes=[f"hf_h1{b}"])
                cur_in, cur_key, wmat, wkey = h1[b][:, :TW], f"hf_h1{b}", w2, "hf_w2"
            else:
                k.op("act", lambda: nc.scalar.activation(out=h2b[:, s0:s0 + TW], in_=yy[b][:, :TW], func=AF.Sin, scale=TWO_PI_LO), reads=[f"hf_y{b}"], writes=["hf_h2b"])
    k.end_phase()
    Wh = [k.sb(f"hf_Wh{i}", [128, 128, 64]) for i in range(2)]
    Whb = [k.sb(f"hf_Whb{i}", [128, 128 * 64], BF16) for i in range(2)]
    win = k.sb("hf_win", [128, 128, 64])
    asum = k.sb("hf_asum", [128, 2, 128]); rn = k.sb("hf_rn", [128, 128])
    sb3 = [k.sb(f"hf_sb3{i}", [128, 512]) for i in range(2)]
    gab = [k.sb(f"hf_gab{i}", [128, 2, 512]) for i in range(3)]
    onesf = k.sb("hf_ones", [128, 128]); k.op("pool", lambda: nc.gpsimd.memset(onesf[:], 1.0), writes=["hf_ones"])
    if NP < 128:
        for i in range(2):
            k.op("pool", lambda: nc.gpsimd.memset(Whb[i][:], 0.0), writes=[f"hf_Whb{i}"])
            k.op("pool", lambda: nc.gpsimd.memset(Wh[i][:], 0.0), writes=[f"hf_Wh{i}"])
    gcount = 0
    for cc in range(4):
        if src == 0:
            k.dma("sp", win[:], self.c_win_lat[cc], writes=["hf_win"])
        else:
            k.dma("sp", win[:NP], self.c_win_ctx[cc], writes=["hf_win"])
        for o in range(2):
            for dd in range(2):
                col0 = (o * 2 + dd) * 512 + cc * 128
                for q0 in range(0, 64, 4):
                    pb = h.bank[(q0 // 4) % 2]
                    pk = f"hy_bank{(q0 // 4) % 2}"
                    for qi in range(4):
                        q = q0 + qi
                        k.op("pe", lambda: nc.tensor.matmul(pb[:NP, qi * 128:(qi + 1) * 128], lhsT=h2b[:, q:q + (NP - 1) * 64 + 1:64], rhs=w3b[:, col0:col0 + 128], start=True, stop=True),
                             reads=["hf_h2b", "hf_w3b"], writes=[pk])
                    k.op("dve", lambda: nc.vector.tensor_tensor(out=Wh[dd][:NP].rearrange("p c q -> p q c")[:, q0:q0 + 4, :], in0=pb[:NP, :].rearrange("p (q c) -> p q c", q=4),
                                                                in1=win[:NP].rearrange("p c q -> p q c")[:, q0:q0 + 4, :], op=ALU.mult),
                         reads=["hf_win"], writes=[pk, f"hf_Wh{dd}"])
                k.op("dve", lambda: nc.vector.tensor_reduce(out=asum[:, dd, :], in_=Wh[dd][:], axis=AX.X, op=ALU.add, apply_absolute_value=True),
                     reads=[f"hf_Wh{dd}"], writes=["hf_asum"])
            k.op("dve", lambda: nc.vector.tensor_tensor(out=asum[:, 0, :], in0=asum[:, 0, :], in1=asum[:, 1, :], op=ALU.add), reads=["hf_asum"], writes=["hf_asum"])
            pb = h.bank[2]
            k.op("pe", lambda: nc.tensor.matmul(pb[:, 0:128], lhsT=onesf[:], rhs=asum[:, 0, :], start=True, stop=True), reads=["hf_ones", "hf_asum"], writes=["hy_bank2"])
            k.op("dve", lambda: nc.vector.tensor_scalar(out=rn[:], in0=pb[:, 0:128], scalar1=1e-6, scalar2=None, op0=ALU.add), writes=["hy_bank2", "hf_rn"])
            k.op("dve", lambda: nc.vector.reciprocal(out=rn[:], in_=rn[:]), reads=["hf_rn"], writes=["hf_rn"])
            for dd in range(2):
                k.op("dve", lambda: nc.vector.tensor_tensor(out=Whb[dd][:NP].rearrange("p (c q) -> p c q", q=64), in0=Wh[dd][:NP], in1=bc(rn[:NP].unsqueeze(2), [NP, 128, 64]), op=ALU.mult),
                     reads=[f"hf_Wh{dd}", "hf_rn"], writes=[f"hf_Whb{dd}"])
            if src == 1:
                for bt in range(2):
                    a0 = hy_s12_ctx(self, h, Whb[0], lambda pr: "hf_Whb0", bt, 0)
                    a1 = hy_s12_ctx(self, h, Whb[1], lambda pr: "hf_Whb1", bt, 1)
                    hy_s3_fb(self, h, a0, a1, 4)
                    g = gab[gcount % 3]
                    gk = f"hf_gab{gcount % 3}"
                    gcount += 1
                    hy_gtab(self, h, 4, g, gk)
                    k.dma("sp", self.spec[1][o, cc, bt], g[:], reads=[gk], writes=["spec1"])
                continue
            SK = 2
            ais = {}
            for it in range(64 + 2 * SK):
                if it < 64:
                    pr = it
                    ais[(pr, 0)] = hy_s12(self, h, Whb[0][:, pr * 128:(pr + 1) * 128], "hf_Whb0", pr % 2)
                    ais[(pr, 1)] = hy_s12(self, h, Whb[1][:, pr * 128:(pr + 1) * 128], "hf_Whb1", 2 + pr % 2)
                pr = it - SK
                if 0 <= pr < 64:
                    b3 = 4 + pr % 4
                    hy_s3_fb(self, h, ais.pop((pr, 0)), ais.pop((pr, 1)), b3)
                    g = gab[gcount % 3]
                    gk = f"hf_gab{gcount % 3}"
                    gcount += 1
                    hy_gtab(self, h, b3, g, gk)
                    k.dma("sp", self.spec[src][o, cc, pr], g[:], reads=[gk], writes=[f"spec{src}"])
    k.end_phase()


def stage_hyena_conv(self, l, src):
    k, nc = self.k, self.nc
    n = NLAT if src == 0 else NCTX
    NP = n // 64
    t0 = NCTX if src == 0 else 0
    k.begin_phase()
    h = hy_setup(self, l, NT=5, NA=4, src=src, conv=True)
    cw = k.sb("hc_cw", [128, 12, 3]); cb = k.sb("hc_cb", [128, 12]); skb = k.sb("hc_skb", [128, 2, 512])
    k.dma("sp", cw[:], self.hy_cwT[:, l], writes=["hc_cw"]); k.dma("sp", cb[:], self.hy_cbT[:, l], writes=["hc_cb"]); k.dma("sp", skb[:], self.hy_skipb[:, l], writes=["hc_skb"])
    fa = k.sb("hc_fa", [128, n]); fb = k.sb("hc_fb", [128, n])
    Wt = {nm: k.sb(f"hc_W{nm}", [128, 128 * 64], BF16) for nm in ("v", "x1", "x2")}
    yfm = k.sb("hc_yfm", [128, n], BF16)
    NYH, NCP, NGAB = 4, 2, 4
    Yh = [k.sb(f"hc_Yh{i}", [128, 512], BF16) for i in range(NYH)]
    Cp = [k.sb(f"hc_Cp{i}", [128, 4, 512], BF16) for i in range(NCP)]
    gab = [k.sb(f"hc_gab{i}", [128, 2, 512]) for i in range(NGAB)]
    WVK = [f"hc_Wv_g{g}" for g in range(16)]
    ea = [k.sb(f"hc_ea{i}", [128, 512]) for i in range(2)]
    eb = [k.sb(f"hc_eb{i}", [128, 512]) for i in range(2)]
    if NP < 128:
        for nm in Wt:
            k.op("pool", lambda: nc.gpsimd.memset(Wt[nm][:], 0.0), writes=(WVK if nm == "v" else [f"hc_W{nm}"]))
    gcount = 0
    for cc in range(4):
        for ui, nm in enumerate(("v", "x1", "x2")):
            ch = ui * 4 + cc
            k.dma("sp", fa[:], self.p_hy[ch, :, t0:t0 + n], reads=["p_hy"], writes=["hc_fa"])
            k.op("dve", lambda: nc.vector.tensor_scalar(out=fb[:], in0=fa[:], scalar1=cw[:, ch, 1:2], scalar2=cb[:, ch:ch + 1], op0=ALU.mult, op1=ALU.add),
                 reads=["hc_fa", "hc_cw", "hc_cb"], writes=["hc_fb"])
            k.op("dve", lambda: nc.vector.scalar_tensor_tensor(out=fb[:, 1:n], in0=fa[:, 0:n - 1], scalar=cw[:, ch, 0:1], in1=fb[:, 1:n], op0=ALU.mult, op1=ALU.add),
                 reads=["hc_fa", "hc_cw", "hc_fb"], writes=["hc_fb"])
            k.op("dve", lambda: nc.vector.scalar_tensor_tensor(out=fb[:, 0:n - 1], in0=fa[:, 1:n], scalar=cw[:, ch, 2:3], in1=fb[:, 0:n - 1], op0=ALU.mult, op1=ALU.add),
                 reads=["hc_fa", "hc_cw", "hc_fb"], writes=["hc_fb"])
            Wd = Wt[nm][:].rearrange("p (c q) -> p q c", q=64)
            for q0 in range(0, 64, 4):
                pb = h.bank[(q0 // 4) % 2]
                pk = f"hy_bank{(q0 // 4) % 2}"
                for qi in range(4):
                    q = q0 + qi
                    k.op("pe", lambda: nc.tensor.transpose(out=pb[:NP, qi * 128:(qi + 1) * 128], in_=fb[:, q:q + (NP - 1) * 64 + 1:64], identity=self.ident[:]),
                         reads=["hc_fb", "ident"], writes=[pk])
                k.op("act", lambda: nc.scalar.copy(out=Wd[:NP, q0:q0 + 4, :], in_=pb[:NP, :].rearrange("p (q c) -> p q c", q=4)), writes=[pk] + (WVK if nm == "v" else [f"hc_W{nm}"]))
        SK = 2
        for o in range(2):
            sig, gate, outn = (("v", "x1", "v") if o == 0 else ("v", "x2", "v"))
            Wsig, Wg, Wo = Wt[sig], Wt[gate], Wt[outn]
            if src == 1:
                for bt in range(2):
                    gi = gcount % NGAB
                    gcount += 1
                    k.dma("sp", gab[gi][:], self.spec[1][o, cc, bt], reads=["spec1"], writes=[f"hc_gab{gi}"])
                    ai = hy_s12_ctx(self, h, Wsig, lambda pr: f"hc_Wv_g{pr // 4}", bt, 0)
                    hy_s3(self, h, ai, 2)
                    yh = Yh[bt % NYH]
                    yk = f"hc_Yh{bt % NYH}"
                    hy_cmul(self, h, h.bank[2][:], "hy_bank2", gab[gi][:, 0, :], gab[gi][:, 1, :], [f"hc_gab{gi}"], yh[:], yk)
                    for duo in range(16):
                        b5 = 4 + duo % 2
                        ps5 = h.bank[b5]
                        for s2 in range(2):
                            p = duo * 2 + s2
                            k.op("pe", lambda: nc.tensor.matmul(ps5[0:8, s2 * 256:(s2 + 1) * 256], lhsT=yh[:, p * 8:p * 8 + 8], rhs=h.R[:, 0, :], start=True, stop=False),
                                 reads=[yk, "hy_R"], writes=[f"hy_bank{b5}"])
                            k.op("pe", lambda: nc.tensor.matmul(ps5[0:8, s2 * 256:(s2 + 1) * 256], lhsT=yh[:, 256 + p * 8:256 + p * 8 + 8], rhs=h.R[:, 1, :], start=False, stop=True),
                                 reads=[yk, "hy_R"], writes=[f"hy_bank{b5}"])
                        grp = bt * 8 + duo // 2
                        cpi = grp % NCP
                        hy_cmul(self, h, ps5[0:8, :], f"hy_bank{b5}", h.TABc[:, 0, :], h.TABc[:, 1, :], ["hy_TABc"], Cp[cpi][0:8, duo % 2, :], f"hc_Cp{cpi}", ri_layout=True, npart=8)
                        if duo % 2 == 1:
                            ps6 = h.bank[6]
                            c5 = Cp[cpi][0:8, 0:2, :].rearrange("p d (s r c) -> p (d s) r c", s=2, r=2)
                            for ri in range(2):
                                k.op("pe", lambda: nc.tensor.matmul(ps6[:NP, :].rearrange("p (g c) -> p g c", g=4), lhsT=h.ICSc[:, ri, :], rhs=c5[:, :, ri, :], start=(ri == 0), stop=(ri == 1)),
                                     reads=[f"hc_Cp{cpi}", "hy_ICSc"], writes=["hy_bank6"])
                            c0 = grp * 8 * 64
                            ei = grp % 2
                            wk = f"hc_Wv_g{grp}"
                            skv = bc(skb[:NP, o, cc * 128 + grp * 8:cc * 128 + grp * 8 + 8].unsqueeze(2), [NP, 8, 64])
                            k.op("pool", lambda: nc.gpsimd.tensor_tensor(out=ea[ei][:NP].rearrange("p (c q) -> p c q", q=64), in0=Wsig[:NP, c0:c0 + 512].rearrange("p (c q) -> p c q", q=64), in1=skv, op=ALU.mult),
                                 reads=[wk, "hc_skb"], writes=[f"hc_ea{ei}"])
                            k.op("dve", lambda: nc.vector.tensor_tensor(out=eb[ei][:NP], in0=ps6[:NP, :], in1=ea[ei][:NP], op=ALU.add), reads=[f"hc_ea{ei}"], writes=["hy_bank6", f"hc_eb{ei}"])
                            k.op("pool", lambda: nc.gpsimd.tensor_tensor(out=Wo[:NP, c0:c0 + 512], in0=eb[ei][:NP], in1=Wg[:NP, c0:c0 + 512], op=ALU.mult),
                                 reads=[f"hc_eb{ei}", f"hc_W{gate}"], writes=[wk])
                continue
            st = {}
            for it in range(64 + 3 * SK):
                if it < 64:
                    pr = it
                    gi = gcount % NGAB
                    gcount += 1
                    k.dma("sp", gab[gi][:], self.spec[src][o, cc, pr], reads=[f"spec{src}"], writes=[f"hc_gab{gi}"])
                    ai = hy_s12(self, h, Wsig[:, pr * 128:(pr + 1) * 128], f"hc_Wv_g{pr // 4}", pr % 2)
                    st[pr] = [gi, ai, None]
                pr = it - SK
                if 0 <= pr < 64:
                    gi, ai, _ = st[pr]
                    b3 = 2 + pr % 2
                    hy_s3(self, h, ai, b3)
                    yi = pr % NYH
                    hy_cmul(self, h, h.bank[b3][:], f"hy_bank{b3}", gab[gi][:, 0, :], gab[gi][:, 1, :], [f"hc_gab{gi}"], Yh[yi][:], f"hc_Yh{yi}")
                    st[pr][2] = yi
                pr = it - 2 * SK
                if 0 <= pr < 64:
                    yi = st[pr][2]
                    yh = Yh[yi]
                    b5 = 4 + pr % 2
                    ps5 = h.bank[b5]
                    for j in range(2):
                        k.op("pe", lambda: nc.tensor.matmul(ps5[:, j * 256:(j + 1) * 256], lhsT=yh[:, j * 128:(j + 1) * 128], rhs=h.R[:, 0, :], start=True, stop=False),
                             reads=[f"hc_Yh{yi}", "hy_R"], writes=[f"hy_bank{b5}"])
                        k.op("pe", lambda: nc.tensor.matmul(ps5[:, j * 256:(j + 1) * 256], lhsT=yh[:, 256 + j * 128:256 + (j + 1) * 128], rhs=h.R[:, 1, :], start=False, stop=True),
                             reads=[f"hc_Yh{yi}", "hy_R"], writes=[f"hy_bank{b5}"])
                    grp = pr // 4
                    cpi = grp % NCP
                    hy_cmul(self, h, ps5[:], f"hy_bank{b5}", h.TAB[:, 0, :], h.TAB[:, 1, :], ["hy_TAB"], Cp[cpi][:, pr % 4, :], f"hc_Cp{cpi}", ri_layout=True)
                    del st[pr]
                pr = it - 3 * SK
                if 0 <= pr < 64 and pr % 4 == 3:
                    grp = pr // 4
                    cpi = grp % NCP
                    ps6 = h.bank[6]
                    c5 = Cp[cpi][:].rearrange("p g (j r c) -> p g j r c", j=2, r=2)
                    n_mm = 0
                    for j in range(2):
                        for ri in range(2):
                            k.op("pe", lambda: nc.tensor.matmul(ps6[:NP, :].rearrange("p (g c) -> p g c", g=4), lhsT=h.ICS[:, ri, j, 0:NP], rhs=c5[:, :, j, ri, :], start=(n_mm == 0), stop=(n_mm == 3)),
                                 reads=[f"hc_Cp{cpi}", "hy_ICS"], writes=["hy_bank6"])
                            n_mm += 1
                    c0 = grp * 8 * 64
                    ei = grp % 2
                    wk = f"hc_Wv_g{grp}"
                    skv = bc(skb[:NP, o, cc * 128 + grp * 8:cc * 128 + grp * 8 + 8].unsqueeze(2), [NP, 8, 64])
                    k.op("pool", lambda: nc.gpsimd.tensor_tensor(out=ea[ei][:NP].rearrange("p (c q) -> p c q", q=64), in0=Wsig[:NP, c0:c0 + 512].rearrange("p (c q) -> p c q", q=64), in1=skv, op=ALU.mult),
                         reads=[wk, "hc_skb"], writes=[f"hc_ea{ei}"])
                    k.op("dve", lambda: nc.vector.tensor_tensor(out=eb[ei][:NP], in0=ps6[:NP, :], in1=ea[ei][:NP], op=ALU.add), reads=[f"hc_ea{ei}"], writes=["hy_bank6", f"hc_eb{ei}"])
                    k.op("pool", lambda: nc.gpsimd.tensor_tensor(out=Wo[:NP, c0:c0 + 512], in0=eb[ei][:NP], in1=Wg[:NP, c0:c0 + 512], op=ALU.mult),
                         reads=[f"hc_eb{ei}", f"hc_W{gate}"], writes=[wk])
        Wy3 = Wt["v"][:].rearrange("p (c q) -> p c q", q=64)
        for q0 in range(0, 64, 8):
            for qi in range(8):
                q = q0 + qi
                k.op("pe", lambda: nc.tensor.transpose(out=h.bankb[:, qi * 128:qi * 128 + NP], in_=Wy3[:NP, :, q], identity=self.ident_bf[:NP, :NP]),
                     reads=WVK + ["ident_bf"], writes=["hy_bankb"])
            ov = bass.AP(tensor=yfm[:].tensor, offset=yfm[:].offset + q0, ap=[list(yfm[:].ap[0]), [1, 8], [64, NP]])
            k.op("act", lambda: nc.scalar.copy(out=ov, in_=h.bankb[:].rearrange("p (q c) -> p q c", q=8)[:, :, 0:NP]), writes=["hy_bankb", "hc_yfm"])
        k.dma("sp", self.y_hy[cc, :, t0:t0 + n], yfm[:], reads=["hc_yfm"], writes=["y_hy"])
    k.end_phase()


Prog.declare_hyena = declare_hyena
Prog.stage_hyena_filters = stage_hyena_filters
Prog.stage_hyena_conv = stage_hyena_conv


def prep_hyena(inp):
    sh = {}
    L = DEPTH
    cw = np.asarray(inp["hy_conv_w"], np.float32)
    sh["hy_cwT"] = np.ascontiguousarray(cw.reshape(L, 3, 12, 128).transpose(3, 0, 2, 1))
    sh["hy_cbT"] = chunkT(inp["hy_conv_b"])
    sh["hy_w1"] = np.ascontiguousarray(np.asarray(inp["hy_w1"], np.float32).transpose(1, 0, 2))
    sh["hy_w2"] = np.ascontiguousarray(np.asarray(inp["hy_w2"], np.float32).transpose(1, 0, 2))
    sh["hy_w3"] = np.ascontiguousarray(np.asarray(inp["hy_w3"], np.float32).transpose(1, 0, 2))
    sh["hy_b1T"] = np.ascontiguousarray(np.asarray(inp["hy_b1"], np.float32).T)
    sh["hy_b2T"] = np.ascontiguousarray(np.asarray(inp["hy_b2"], np.float32).T)
    sh["hy_frT"] = np.ascontiguousarray(np.asarray(inp["hy_freq"], np.float32).T)
    sk = np.asarray(inp["hy_skip"], np.float32)
    sh["hy_skipb"] = np.ascontiguousarray(np.broadcast_to(sk[None], (128, L, 2, 512)))
    bf = ml_dtypes.bfloat16
    P_ = np.arange(128, dtype=np.float64)[:, None]
    kb = np.arange(256, dtype=np.float64)[None, :]
    a = 2 * np.pi * P_ * kb / 256.0
    sh["c_F1"] = np.concatenate([np.cos(a), -np.sin(a)], 1).astype(bf)
    q = (np.arange(128) % 64).astype(np.float64)[:, None]
    a = 2 * np.pi * q * kb / 16384.0
    sh["c_TT"] = np.stack([np.concatenate([np.cos(a), np.cos(a)], 1), np.concatenate([np.sin(a), -np.sin(a)], 1)], 1).astype(np.float32)
    qq = np.arange(64, dtype=np.float64)
    a = 2 * np.pi * np.outer(qq, qq) / 64.0
    def bd(m):
        z = np.zeros((128, 128)); z[:64, :64] = m; z[64:, 64:] = m
        return z
    BDC, BDS = bd(np.cos(a)), bd(np.sin(a))
    sh["c_BD"] = np.stack([BDC, BDS, -BDS, -BDC], 1).astype(bf)
    sh["c_R"] = np.stack([np.concatenate([BDC, BDS], 1), np.concatenate([-BDS, BDC], 1)], 1).astype(bf)
    p_ = np.arange(128, dtype=np.float64)
    TA = np.zeros((128, 2, 2, 2, 64)); TB = np.zeros((128, 2, 2, 2, 64))
    for j in range(2):
        ang = 2 * np.pi * np.outer(j * 128 + p_, qq) / 16384.0
        TA[:, j, :, :, :] = np.cos(ang)[:, None, None, :]
        TB[:, j, 0, :, :] = -np.sin(ang)[:, None, :]
        TB[:, j, 1, :, :] = np.sin(ang)[:, None, :]
    sh["c_TAB"] = np.stack([TA.reshape(128, 512), TB.reshape(128, 512)], 1).astype(np.float32)
    ICS = np.zeros((128, 2, 2, 128))
    Pn = np.arange(128, dtype=np.float64)
    for j in range(2):
        ang = 2 * np.pi * np.outer(j * 128 + p_, Pn) / 256.0
        ICS[:, 0, j, :] = np.cos(ang) / 16384.0
        ICS[:, 1, j, :] = -np.sin(ang) / 16384.0
    sh["c_ICS"] = ICS.astype(bf)
    P4 = np.arange(4, dtype=np.float64)[:, None]; m8 = np.arange(8, dtype=np.float64)[None, :]
    a = 2 * np.pi * P4 * m8 / 8.0
    sh["c_F1c"] = np.concatenate([np.cos(a), -np.sin(a)], 1).astype(bf)
    a = 2 * np.pi * q * m8 / 512.0
    ct = np.tile(np.cos(a)[:, None, :], (1, 32, 1)).reshape(128, 256); st_ = np.tile(np.sin(a)[:, None, :], (1, 32, 1)).reshape(128, 256)
    sh["c_TTc"] = np.stack([np.concatenate([ct, ct], 1), np.concatenate([st_, -st_], 1)], 1).astype(np.float32)
    TAc = np.zeros((8, 2, 2, 2, 64)); TBc = np.zeros((8, 2, 2, 2, 64))
    ang = 2 * np.pi * np.outer(np.arange(8, dtype=np.float64), qq) / 512.0
    TAc[:] = np.cos(ang)[:, None, None, None, :]
    TBc[:, :, 0, :, :] = -np.sin(ang)[:, None, None, :]
    TBc[:, :, 1, :, :] = np.sin(ang)[:, None, None, :]
    sh["c_TABc"] = np.stack([TAc.reshape(8, 512), TBc.reshape(8, 512)], 1).astype(np.float32)
    ang = 2 * np.pi * np.outer(np.arange(8, dtype=np.float64), np.arange(4, dtype=np.float64)) / 8.0
    sh["c_ICSc"] = np.stack([np.cos(ang) / 512.0, -np.sin(ang) / 512.0], 1).astype(bf)
    bands = np.linspace(1e-4, 15, 16)
    deltas = np.abs(np.linspace(math.log(1e-2) / 0.3, math.log(1e-2) / 1.5, 512))
    for nm, n in (("lat", NLAT), ("ctx", NCTX)):
        idx = np.arange(n, dtype=np.float64)
        tn = idx / (n - 1)
        ang = (2 * np.pi / n) * idx[:, None] * bands[None, :]
        feats = np.concatenate([tn[:, None], np.cos(ang), -np.sin(ang)], -1)
        sh["c_feat_" + nm] = np.ascontiguousarray(feats.T).astype(np.float32)
        win = np.exp(-tn[:, None] * deltas[None, :])
        NPn = n // 64
        w = win.reshape(NPn, 64, 4, 128).transpose(2, 0, 3, 1)
        sh["c_win_" + nm] = np.ascontiguousarray(w).astype(np.float32)
    return sh


def stage_final(self):
    k, nc = self.k, self.nc
    self.out = self.outp("out", [8, 128, NLAT])
    k.begin_phase()
    NB = 2
    xt = [k.sb(f"f_xt{i}", [128, 8, 512]) for i in range(NB)]
    sq = [k.sb(f"f_sq{i}", [128, 8, 512], BF16) for i in range(NB)]
    ss = [k.ps(f"f_ss{i}", [128, 512]) for i in range(NB)]
    rstd = [k.sb(f"f_rstd{i}", [128, 512]) for i in range(NB)]
    ot = [k.sb(f"f_o{i}", [128, 8, 512]) for i in range(NB)]
    xv = self.xres.rearrange("k p t -> p k t")
    ov = self.out.rearrange("k p t -> p k t")
    for ti, (s0, W) in enumerate(TILES[1:]):
        b = ti % NB
        k.dma("sp", xt[b][:], xv[:, :, s0:s0 + W], reads=["xres"], writes=[f"f_xt{b}"])
        k.op("act", lambda: nc.scalar.activation(out=sq[b][:], in_=xt[b][:], func=AF.Square), reads=[f"f_xt{b}"], writes=[f"f_sq{b}"])
        for kk in range(8):
            k.op("pe", lambda: nc.tensor.matmul(ss[b][:], lhsT=self.ones_bf[:], rhs=sq[b][:, kk, :], start=(kk == 0), stop=(kk == 7)),
                 reads=[f"f_sq{b}", "ones_bf"], writes=[f"f_ss{b}"])
        k.op("act", lambda: nc.scalar.activation(out=rstd[b][:], in_=ss[b][:], func=AF.Sqrt, scale=1.0 / D, bias=self.epsc[:, 0:1]),
             reads=["epsc"], writes=[f"f_ss{b}", f"f_rstd{b}"])
        k.op("dve", lambda: nc.vector.reciprocal(out=rstd[b][:], in_=rstd[b][:]), reads=[f"f_rstd{b}"], writes=[f"f_rstd{b}"])
        for kk in range(8):
            k.op("dve", lambda: nc.vector.scalar_tensor_tensor(out=ot[b][:, kk, :], in0=xt[b][:, kk, :], scalar=self.gfin[:, kk:kk + 1], in1=rstd[b][:], op0=ALU.mult, op1=ALU.mult),
                 reads=[f"f_xt{b}", "gfin", f"f_rstd{b}"], writes=[f"f_o{b}"])
        k.dma("pool", ov[:, :, s0 - NCTX:s0 - NCTX + W], ot[b][:], reads=[f"f_o{b}"], writes=["out"])
    k.end_phase()


Prog.stage_final = stage_final


def build_full(layers=DEPTH):
    P = Prog(layers=layers)
    P.declare_rg(); P.declare_gla(); P.declare_merge(); P.declare_hyena(); P.declare_peer()
    for l in range(layers):
        need_ctx = l < DEPTH - 1
        xsrc = P.xT if l == 0 else P.xres
        tiles = TILES if need_ctx else TILES[1:]
        P.stage0(l)
        P.stage_norm(l, 1, xsrc)
        P.stage_inproj(l)
        P.stage_rglru(l)
        P.stage_gla(l)
        P.stage_hyena_filters(l, 0)
        P.stage_hyena_conv(l, 0)
        if need_ctx:
            P.stage_hyena_filters(l, 1)
            P.stage_hyena_conv(l, 1)
        P.stage_merge(l, xsrc, tiles)
        P.stage_norm(l, 2, P.xres, tiles)
        P.stage_peer_prep(l)
        P.stage_peer(l, list(range(0 if need_ctx else NCTX, T, 128)))
    P.stage_final()
    P.k.finish()
    return P


def prep_all_shared(inp):
    sh = prep_shared(inp)
    sh.update(prep_rg(inp)); sh.update(prep_gla(inp)); sh.update(prep_merge(inp)); sh.update(prep_hyena(inp)); sh.update(prep_peer(inp))
    return sh


N_CORES = 4


def kernel(**inputs):
    inp = {k_: np.asarray(v) for k_, v in inputs.items()}
    P = build_full()
    sh = prep_all_shared(inp)
    in_maps = []
    for b in range(N_CORES):
        m = dict(sh)
        m.update(prep_core(inp, b))
        in_maps.append(m)
    res = run_bass_kernel_spmd(P.nc, in_maps, core_ids=list(range(N_CORES)))
    outs = []
    for b in range(N_CORES):
        o = np.asarray(res.results[b]["out"], np.float32).reshape(D, NLAT)
        outs.append(np.ascontiguousarray(o.T))
    return np.stack(outs, 0).astype(np.float32)
```

```python
import math
from contextlib import ExitStack

import numpy as np
import ml_dtypes
import concourse.bass as bass
import concourse.mybir as mybir
from concourse.bass_utils import run_bass_kernel_spmd

F32 = mybir.dt.float32
BF16 = mybir.dt.bfloat16
I32 = mybir.dt.int32
U32 = mybir.dt.uint32
AF = mybir.ActivationFunctionType
ALU = mybir.AluOpType
AX = mybir.AxisListType

D = 1024
NCTX = 256
NLAT = 8192
T = NCTX + NLAT
DEPTH = 4
IN_DIM = 7200
EPS = 1e-6
NDMASEM = 8
TILES = [(0, 256)] + [(256 + i * 512, 512) for i in range(16)]


class KB:
    def __init__(self, nc, self_wait=True):
        self.nc = nc
        self.es = ExitStack()
        self.engs = {"pe": nc.tensor, "dve": nc.vector, "act": nc.scalar, "pool": nc.gpsimd, "sp": nc.sync}
        self.sems = {}
        self.cnt = {}
        for e in self.engs:
            self._mksem("c_" + e)
            for i in range(NDMASEM):
                self._mksem(f"d_{e}{i}")
        self.dma_rr = {e: 0 for e in self.engs}
        self.waited = {e: {} for e in self.engs}
        self.last_w = {}
        self.readers = {}
        self.self_wait = self_wait
        self.n_ins = 0
        self.n_wait = 0
        self.phase = None

    def _mksem(self, name):
        self.sems[name] = self.es.enter_context(self.nc.semaphore(name))
        self.cnt[name] = 0

    def begin_phase(self):
        self.pstack = getattr(self, "pstack", [])
        self.pstack.append(self.phase)
        self.phase = ExitStack()

    def end_phase(self):
        self.barrier()
        self.phase.close()
        self.phase = self.pstack.pop()

    def sb(self, name, shape, dt=F32, perm=False):
        st = self.es if (perm or self.phase is None) else self.phase
        self.uid = getattr(self, "uid", 0) + 1
        return st.enter_context(self.nc.sbuf_tensor(f"{name}_{self.uid}", list(shape), dt))

    def ps(self, name, shape, dt=F32, perm=False):
        st = self.es if (perm or self.phase is None) else self.phase
        self.uid = getattr(self, "uid", 0) + 1
        return st.enter_context(self.nc.psum_tensor(f"{name}_{self.uid}", list(shape), dt))

    def dram(self, name, shape, dt=F32, kind="Internal"):
        return self.nc.dram_tensor(name, list(shape), dt, kind=kind).ap()

    def _wait(self, eng, s, v):
        if self.waited[eng].get(s, 0) >= v:
            return
        self.engs[eng].wait_ge(self.sems[s], v)
        self.waited[eng][s] = v
        self.n_wait += 1

    def _need(self, eng, reads, writes):
        need = {}

        def add(sv):
            s, v = sv
            if need.get(s, 0) < v:
                need[s] = v

        for r in reads:
            if r in self.last_w:
                add(self.last_w[r])
        for w in writes:
            if w in self.last_w:
                add(self.last_w[w])
            for sv in self.readers.get(w, ()):
                add(sv)
        own = "c_" + eng
        for s, v in need.items():
            if s == own and (eng == "pe" or not self.self_wait):
                continue
            self._wait(eng, s, v)

    def _record(self, sem, val, reads, writes):
        for r in reads:
            self.readers.setdefault(r, []).append((sem, val))
        for w in writes:
            self.last_w[w] = (sem, val)
            self.readers[w] = []

    def op(self, eng, fn, reads=(), writes=()):
        self._need(eng, reads, writes)
        ins = fn()
        sem = "c_" + eng
        self.cnt[sem] += 1
        ins.then_inc(self.sems[sem], 1)
        self._record(sem, self.cnt[sem], reads, writes)
        self.n_ins += 1
        return ins

    def _dma_sem(self, eng):
        i = self.dma_rr[eng]
        self.dma_rr[eng] = (i + 1) % NDMASEM
        return f"d_{eng}{i}"

    def dma(self, eng, out, in_, reads=(), writes=(), **kw):
        self._need(eng, reads, writes)
        sem = self._dma_sem(eng)
        ins = self.engs[eng].dma_start(out=out, in_=in_, **kw)
        self.cnt[sem] += 16
        ins.then_inc(self.sems[sem], 16)
        self._record(sem, self.cnt[sem], reads, writes)
        self.n_ins += 1
        return ins

    def gather(self, out, table, idx_ap, reads=(), writes=()):
        eng = "pool"
        self._need(eng, reads, writes)
        sem = self._dma_sem(eng)
        ins = self.nc.gpsimd.indirect_dma_start(
            out=out, out_offset=None, in_=table,
            in_offset=bass.IndirectOffsetOnAxis(ap=idx_ap, axis=0))
        self.cnt[sem] += 16
        ins.then_inc(self.sems[sem], 16)
        self._record(sem, self.cnt[sem], reads, writes)
        self.n_ins += 1
        return ins

    def barrier(self):
        for e in self.engs:
            for s, v in self.cnt.items():
                if v > 0 and not (s == "c_" + e and e == "pe"):
                    self._wait(e, s, v)

    def finish(self):
        self.barrier()
        if self.phase is not None:
            self.phase.close()
            self.phase = None
        self.es.close()


def bc(ap, shape):
    return ap.to_broadcast(list(shape))


PGROUPS = [
    ("hy", 0, 1024), ("hy", 1024, 512), ("rgx", 1536, 512), ("rgg", 2048, 512),
    ("gqk", 2560, 512), ("gv", 3072, 512), ("glr", 3584, 32), ("glag", 3616, 512),
    ("mg", 4128, 1024), ("mg", 5152, 1024), ("mg", 6176, 1024),
]


class Prog:
    def __init__(self, layers=DEPTH, debug=()):
        self.layers = layers
        self.debug = set(debug)
        nc = bass.Bass("TRN2", target_bir_lowering=False)
        self.nc = nc
        self.k = KB(nc)
        self.dbg_outs = []
        self.declare_io()
        self.setup_consts()

    def inp(self, name, shape, dt=F32):
        return self.nc.dram_tensor(name, list(shape), dt, kind="ExternalInput").ap()

    def outp(self, name, shape, dt=F32):
        return self.nc.dram_tensor(name, list(shape), dt, kind="ExternalOutput").ap()

    def declare_io(self):
        k = self.k
        self.xT = self.inp("xT", [8, 128, T])
        self.cT = self.inp("cT", [128, 8, 2])
        self.w_mod = self.inp("w_mod", [DEPTH, D, 6 * D])
        self.b_modT = self.inp("b_modT", [128, DEPTH, 48])
        self.gmixT = self.inp("gmixT", [128, DEPTH, 8])
        self.gffnT = self.inp("gffnT", [128, DEPTH, 8])
        self.gfinT = self.inp("gfinT", [128, 8])
        self.w_in = self.inp("w_in", [DEPTH, D, IN_DIM])
        self.b_mergeT = self.inp("b_mergeT", [128, DEPTH, 24])
        self.xres = k.dram("xres", [8, 128, T])
        self.hT = k.dram("hT", [8, 128, T], BF16)
        self.p_hy = k.dram("p_hy", [12, 128, T])
        self.p_rgx = k.dram("p_rgx", [4, 128, T])
        self.p_rgg = k.dram("p_rgg", [4, 128, T])
        self.p_gqk = k.dram("p_gqk", [4, 128, T], BF16)
        self.p_gv = k.dram("p_gv", [4, 128, T], BF16)
        self.p_glr = k.dram("p_glr", [2, 16, T])
        self.p_glag = k.dram("p_glag", [4, 128, T])
        self.p_mg = k.dram("p_mg", [24, 128, T])

    def dbg(self, name, src_ap, shape, dt=F32, reads=()):
        if name not in self.debug:
            return
        o = self.outp("dbg_" + name, shape, dt)
        self.k.dma("sp", o, src_ap, reads=list(reads), writes=["dbg_" + name])
        self.dbg_outs.append("dbg_" + name)

    def setup_consts(self):
        k, nc = self.k, self.nc
        self.ones_bf = k.sb("ones_bf", [128, 128], BF16, perm=True)
        k.op("pool", lambda: nc.gpsimd.memset(self.ones_bf[:], 1.0), writes=["ones_bf"])
        self.ident = k.sb("ident", [128, 128], F32, perm=True)
        k.op("pool", lambda: nc.gpsimd.memset(self.ident[:], 1.0), writes=["ident"])
        k.op("pool", lambda: nc.gpsimd.affine_select(out=self.ident[:], in_=self.ident[:], pattern=[[-1, 128]],
                                                    compare_op=ALU.is_equal, fill=0.0, base=0, channel_multiplier=1),
             reads=["ident"], writes=["ident"])
        self.ident_bf = k.sb("ident_bf", [128, 128], BF16, perm=True)
        k.op("pool", lambda: nc.gpsimd.tensor_copy(out=self.ident_bf[:], in_=self.ident[:]), reads=["ident"], writes=["ident_bf"])
        self.onec = k.sb("onec", [128, 1], F32, perm=True)
        k.op("pool", lambda: nc.gpsimd.memset(self.onec[:], 1.0), writes=["onec"])
        self.epsc = k.sb("epsc", [128, 1], F32, perm=True)
        k.op("pool", lambda: nc.gpsimd.memset(self.epsc[:], EPS), writes=["epsc"])
        self.sc_t = k.sb("sc_t", [128, 8, 2], F32, perm=True)
        k.dma("sp", self.sc_t[:], self.cT, writes=["sc_t"])
        k.op("act", lambda: nc.scalar.activation(out=self.sc_t[:], in_=self.sc_t[:], func=AF.Silu), reads=["sc_t"], writes=["sc_t"])
        self.bmod = k.sb("bmod", [128, DEPTH, 48], F32, perm=True)
        k.dma("sp", self.bmod[:], self.b_modT, writes=["bmod"])
        self.gmix = k.sb("gmix", [128, DEPTH, 8], F32, perm=True)
        k.dma("sp", self.gmix[:], self.gmixT, writes=["gmix"])
        self.gffn = k.sb("gffn", [128, DEPTH, 8], F32, perm=True)
        k.dma("sp", self.gffn[:], self.gffnT, writes=["gffn"])
        self.gfin = k.sb("gfin", [128, 8], F32, perm=True)
        k.dma("sp", self.gfin[:], self.gfinT, writes=["gfin"])
        self.bmerge = k.sb("bmerge", [128, DEPTH, 24], F32, perm=True)
        k.dma("sp", self.bmerge[:], self.b_mergeT, writes=["bmerge"])
        self.modT = k.sb("modT", [128, 48, 2], F32, perm=True)
        self.A1 = k.sb("A1", [128, 8, 2], F32, perm=True)
        self.A2 = k.sb("A2", [128, 8, 2], F32, perm=True)

    def stage0(self, l):
        k, nc = self.k, self.nc
        k.begin_phase()
        wm = [k.sb(f"wm{i}", [128, 8, 1024]) for i in range(2)]
        pm = k.ps("pm", [128, 8, 2])
        for g in range(6):
            w = wm[g % 2]
            wk = f"wm{g % 2}"
            k.dma("sp", w[:], self.w_mod[l, :, g * 1024:(g + 1) * 1024].rearrange("(k p) c -> p k c", p=128), writes=[wk])
            for cc in range(8):
                for kk in range(8):
                    k.op("pe", lambda: nc.tensor.matmul(pm[:, cc, :], lhsT=w[:, kk, cc * 128:(cc + 1) * 128], rhs=self.sc_t[:, kk, :],
                                                        start=(kk == 0), stop=(kk == 7)),
                         reads=[wk, "sc_t"], writes=["pm"])
            k.op("dve", lambda: nc.vector.tensor_tensor(out=self.modT[:, g * 8:(g + 1) * 8, :], in0=pm[:],
                                                        in1=bc(self.bmod[:, l, g * 8:(g + 1) * 8].unsqueeze(2), [128, 8, 2]), op=ALU.add),
                 reads=["pm", "bmod"], writes=["modT"])
        k.op("dve", lambda: nc.vector.scalar_tensor_tensor(out=self.A1[:], in0=self.modT[:, 8:16, :], scalar=1.0,
                                                           in1=bc(self.gmix[:, l, :].unsqueeze(2), [128, 8, 2]), op0=ALU.add, op1=ALU.mult),
             reads=["modT", "gmix"], writes=["A1"])
        k.op("dve", lambda: nc.vector.scalar_tensor_tensor(out=self.A2[:], in0=self.modT[:, 32:40, :], scalar=1.0,
                                                           in1=bc(self.gffn[:, l, :].unsqueeze(2), [128, 8, 2]), op0=ALU.add, op1=ALU.mult),
             reads=["modT", "gffn"], writes=["A2"])
        k.end_phase()

    def stage_norm(self, l, which, xsrc, tiles=TILES):
        k, nc = self.k, self.nc
        A = self.A1 if which == 1 else self.A2
        Ak = "A1" if which == 1 else "A2"
        sh0 = 0 if which == 1 else 24
        k.begin_phase()
        NB = 2
        xt = [k.sb(f"n_xt{i}", [128, 8, 512]) for i in range(NB)]
        sq = [k.sb(f"n_sq{i}", [128, 8, 512], BF16) for i in range(NB)]
        ss = [k.ps(f"n_ss{i}", [128, 512]) for i in range(NB)]
        rstd = [k.sb(f"n_rstd{i}", [128, 512]) for i in range(NB)]
        tmp = [k.sb(f"n_tmp{i}", [128, 8, 512]) for i in range(NB)]
        hh = [k.sb(f"n_h{i}", [128, 8, 512], BF16) for i in range(NB)]
        xv = xsrc.rearrange("k p t -> p k t")
        hv = self.hT.rearrange("k p t -> p k t")
        for ti, (s0, W) in enumerate(tiles):
            b = ti % NB
            j = 1 if s0 < NCTX else 0
            k.dma("sp", xt[b][:, :, :W], xv[:, :, s0:s0 + W], reads=["xres"], writes=[f"n_xt{b}"])
            k.op("act", lambda: nc.scalar.activation(out=sq[b][:, :, :W], in_=xt[b][:, :, :W], func=AF.Square),
                 reads=[f"n_xt{b}"], writes=[f"n_sq{b}"])
            for kk in range(8):
                k.op("pe", lambda: nc.tensor.matmul(ss[b][:, :W], lhsT=self.ones_bf[:], rhs=sq[b][:, kk, :W], start=(kk == 0), stop=(kk == 7)),
                     reads=[f"n_sq{b}", "ones_bf"], writes=[f"n_ss{b}"])
            k.op("act", lambda: nc.scalar.activation(out=rstd[b][:, :W], in_=ss[b][:, :W], func=AF.Sqrt, scale=1.0 / D, bias=self.epsc[:, 0:1]),
                 reads=[f"n_ss{b}", "epsc"], writes=[f"n_rstd{b}"])
            k.op("dve", lambda: nc.vector.reciprocal(out=rstd[b][:, :W], in_=rstd[b][:, :W]),
                 reads=[f"n_rstd{b}"], writes=[f"n_rstd{b}"])
            k.op("dve", lambda: nc.vector.tensor_tensor(out=tmp[b][:, :, :W], in0=xt[b][:, :, :W],
                                                        in1=bc(rstd[b][:, :W].unsqueeze(1), [128, 8, W]), op=ALU.mult),
                 reads=[f"n_xt{b}", f"n_rstd{b}"], writes=[f"n_tmp{b}"])
            for kk in range(8):
                k.op("act", lambda: nc.scalar.activation(out=hh[b][:, kk, :W], in_=tmp[b][:, kk, :W], func=AF.Identity,
                                                         scale=A[:, kk, j:j + 1], bias=self.modT[:, sh0 + kk, j:j + 1]),
                     reads=[f"n_tmp{b}", Ak, "modT"], writes=[f"n_h{b}"])
            k.dma("pool", hv[:, :, s0:s0 + W], hh[b][:, :, :W], reads=[f"n_h{b}"], writes=["hT"])
        k.end_phase()

    def stage_inproj(self, l, tiles=TILES):
        k, nc = self.k, self.nc
        k.begin_phase()
        wst = k.sb("ip_wst", [128, 8, 1024])
        wbf = [k.sb(f"ip_wbf{i}", [128, 8, 1024], BF16) for i in range(2)]
        ht = [k.sb(f"ip_h{i}", [128, 8, 512], BF16) for i in range(2)]
        NP = 4
        pp = [k.ps(f"ip_p{i}", [128, 512]) for i in range(NP)]
        ost = [k.sb(f"ip_o{i}", [128, 512]) for i in range(NP)]
        ostb = [k.sb(f"ip_ob{i}", [128, 512], BF16) for i in range(NP)]
        hv = self.hT.rearrange("k p t -> p k t")
        cnt = 0
        hcnt = 0
        for gi, (name, c0, ncols) in enumerate(PGROUPS):
            wb = wbf[gi % 2]
            wbk = f"ip_wbf{gi % 2}"
            k.dma("sp", wst[:, :, :ncols], self.w_in[l, :, c0:c0 + ncols].rearrange("(k p) c -> p k c", p=128), writes=["ip_wst"])
            k.op("pool", lambda: nc.gpsimd.tensor_copy(out=wb[:, :, :ncols], in_=wst[:, :, :ncols]), reads=["ip_wst"], writes=[wbk])
            csz = 16 if name == "glr" else 128
            nch = ncols // csz
            for ti, (s0, W) in enumerate(tiles):
                hb = hcnt % 2
                hcnt += 1
                k.dma("sp", ht[hb][:, :, :W], hv[:, :, s0:s0 + W], reads=["hT"], writes=[f"ip_h{hb}"])
                for ch in range(nch):
                    pb = cnt % NP
                    cnt += 1
                    for kk in range(8):
                        k.op("pe", lambda: nc.tensor.matmul(pp[pb][:csz, :W], lhsT=wb[:, kk, ch * csz:(ch + 1) * csz], rhs=ht[hb][:, kk, :W],
                                                            start=(kk == 0), stop=(kk == 7)),
                             reads=[wbk, f"ip_h{hb}"], writes=[f"ip_p{pb}"])
                    gch = (c0 - {"hy": 0, "rgx": 1536, "rgg": 2048, "gqk": 2560, "gv": 3072, "glr": 3584, "glag": 3616, "mg": 4128}[name]) // csz + ch
                    src = pp[pb][:csz, :W]
                    pk = f"ip_p{pb}"
                    if name in ("hy", "rgx"):
                        dst = (self.p_hy if name == "hy" else self.p_rgx)
                        k.op("dve", lambda: nc.vector.tensor_copy(out=ost[pb][:, :W], in_=src), reads=[pk], writes=[f"ip_o{pb}"])
                        k.dma("pool", dst[gch, :, s0:s0 + W], ost[pb][:, :W], reads=[f"ip_o{pb}"], writes=["p_" + name])
                    elif name == "rgg":
                        k.op("act", lambda: nc.scalar.activation(out=ost[pb][:, :W], in_=src, func=AF.Gelu_apprx_tanh), reads=[pk], writes=[f"ip_o{pb}"])
                        k.dma("pool", self.p_rgg[gch, :, s0:s0 + W], ost[pb][:, :W], reads=[f"ip_o{pb}"], writes=["p_rgg"])
                    elif name == "glag":
                        k.op("act", lambda: nc.scalar.activation(out=ost[pb][:, :W], in_=src, func=AF.Silu), reads=[pk], writes=[f"ip_o{pb}"])
                        k.dma("pool", self.p_glag[gch, :, s0:s0 + W], ost[pb][:, :W], reads=[f"ip_o{pb}"], writes=["p_glag"])
                    elif name == "mg":
                        k.op("act", lambda: nc.scalar.activation(out=ost[pb][:, :W], in_=src, func=AF.Sigmoid, bias=self.bmerge[:, l, gch:gch + 1]),
                             reads=[pk, "bmerge"], writes=[f"ip_o{pb}"])
                        k.dma("pool", self.p_mg[gch, :, s0:s0 + W], ost[pb][:, :W], reads=[f"ip_o{pb}"], writes=["p_mg"])
                    elif name == "gqk":
                        sc = 0.125 if gch < 2 else 1.0
                        k.op("dve", lambda: nc.vector.tensor_scalar(out=ostb[pb][:, :W], in0=src, scalar1=sc, scalar2=None, op0=ALU.mult),
                             reads=[pk], writes=[f"ip_ob{pb}"])
                        k.dma("pool", self.p_gqk[gch, :, s0:s0 + W], ostb[pb][:, :W], reads=[f"ip_ob{pb}"], writes=["p_gqk"])
                    elif name == "gv":
                        k.op("dve", lambda: nc.vector.tensor_copy(out=ostb[pb][:, :W], in_=src), reads=[pk], writes=[f"ip_ob{pb}"])
                        k.dma("pool", self.p_gv[gch, :, s0:s0 + W], ostb[pb][:, :W], reads=[f"ip_ob{pb}"], writes=["p_gv"])
                    elif name == "glr":
                        k.op("dve", lambda: nc.vector.tensor_copy(out=ost[pb][:16, :W], in_=src), reads=[pk], writes=[f"ip_o{pb}"])
                        k.dma("pool", self.p_glr[gch, :, s0:s0 + W], ost[pb][:16, :W], reads=[f"ip_o{pb}"], writes=["p_glr"])
        k.end_phase()


def chunkT(v):
    v = np.asarray(v, np.float32)
    lead = v.shape[:-1]
    n = v.shape[-1] // 128
    w = v.reshape(*lead, n, 128)
    return np.ascontiguousarray(np.moveaxis(w, -1, 0))


def prep_shared(inp):
    sh = {}
    sh["w_mod"] = np.ascontiguousarray(inp["w_mod"], np.float32)
    sh["b_modT"] = chunkT(inp["b_mod"])
    sh["gmixT"] = chunkT(inp["g_norm_mix"])
    sh["gffnT"] = chunkT(inp["g_norm_ffn"])
    sh["gfinT"] = chunkT(inp["g_final"])
    sh["w_in"] = np.ascontiguousarray(inp["w_in"], np.float32)
    sh["b_mergeT"] = chunkT(inp["b_merge"])
    return sh


def prep_core(inp, b):
    m = {}
    xc = np.concatenate([inp["ctx"][b], inp["x"][b]], axis=0)
    m["xT"] = np.ascontiguousarray(xc.T.reshape(8, 128, T))
    cc = np.stack([inp["c"][b], inp["c_ctx"]], axis=-1)
    m["cT"] = np.ascontiguousarray(cc.reshape(8, 128, 2).transpose(1, 0, 2))
    return m


def declare_rg(self):
    k = self.k
    self.rg_cwT = self.inp("rg_cwT", [128, DEPTH, 2, 4, 4])
    self.rg_cbT = self.inp("rg_cbT", [128, DEPTH, 2, 4])
    self.rg_baT = self.inp("rg_baT", [128, DEPTH, 2, 4])
    self.rg_bxT = self.inp("rg_bxT", [128, DEPTH, 2, 4])
    self.rg_lamT = self.inp("rg_lamT", [128, DEPTH, 2, 4])
    self.rg_waBD = self.inp("rg_waBD", [128, DEPTH, 2, 4, 128])
    self.rg_wxBD = self.inp("rg_wxBD", [128, DEPTH, 2, 4, 128])
    self.y_rg = k.dram("y_rg", [4, 128, T], BF16)


def stage_rglru(self, l, tiles=TILES):
    k, nc = self.k, self.nc
    k.begin_phase()
    cw = k.sb("rg_cw", [128, 2, 4, 4]); cb = k.sb("rg_cb", [128, 2, 4]); ba = k.sb("rg_ba", [128, 2, 4])
    bx = k.sb("rg_bx", [128, 2, 4]); lam = k.sb("rg_lam", [128, 2, 4]); nsp = k.sb("rg_nsp", [128, 2, 4])
    wst = k.sb("rg_wst", [128, 2, 2, 4, 128]); wbd = k.sb("rg_wbd", [128, 2, 2, 4, 128], BF16)
    k.dma("sp", cw[:], self.rg_cwT[:, l], writes=["rg_cw"])
    k.dma("sp", cb[:], self.rg_cbT[:, l], writes=["rg_cb"])
    k.dma("sp", ba[:], self.rg_baT[:, l], writes=["rg_ba"])
    k.dma("sp", bx[:], self.rg_bxT[:, l], writes=["rg_bx"])
    k.dma("sp", lam[:], self.rg_lamT[:, l], writes=["rg_lam"])
    k.dma("sp", wst[:, 0], self.rg_waBD[:, l], writes=["rg_wst"])
    k.dma("sp", wst[:, 1], self.rg_wxBD[:, l], writes=["rg_wst"])
    k.op("pool", lambda: nc.gpsimd.tensor_copy(out=wbd[:], in_=wst[:]), reads=["rg_wst"], writes=["rg_wbd"])
    k.op("act", lambda: nc.scalar.activation(out=nsp[:], in_=lam[:], func=AF.Exp, scale=-1.0), reads=["rg_lam"], writes=["rg_nsp"])
    k.op("act", lambda: nc.scalar.activation(out=nsp[:], in_=nsp[:], func=AF.Ln, bias=self.onec[:, 0:1]), reads=["rg_nsp", "onec"], writes=["rg_nsp"])
    k.op("dve", lambda: nc.vector.tensor_scalar(out=nsp[:], in0=nsp[:], scalar1=-8.0, scalar2=None, op0=ALU.mult), reads=["rg_nsp"], writes=["rg_nsp"])

    u = k.sb("rg_u", [128, T]); gg = k.sb("rg_gg", [128, T]); hsum = k.sb("rg_hsum", [128, T])
    NB = 2
    xc = [k.sb(f"rg_xc{i}", [128, 512]) for i in range(NB)]
    xcb = [k.sb(f"rg_xcb{i}", [128, 512], BF16) for i in range(NB)]
    pa = [k.ps(f"rg_pa{i}", [128, 512]) for i in range(NB)]
    px = [k.ps(f"rg_px{i}", [128, 512]) for i in range(NB)]
    gr = [k.sb(f"rg_gr{i}", [128, 512]) for i in range(NB)]
    aa = [k.sb(f"rg_a{i}", [128, 512]) for i in range(NB)]
    gi = [k.sb(f"rg_gi{i}", [128, 512]) for i in range(NB)]
    t1 = [k.sb(f"rg_t1{i}", [128, 512]) for i in range(NB)]
    t3 = [k.sb(f"rg_t3{i}", [128, 512]) for i in range(NB)]
    hs = [k.sb(f"rg_hs{i}", [128, 512]) for i in range(NB)]
    yo = [k.sb(f"rg_yo{i}", [128, 512], BF16) for i in range(NB)]

    def rev(ap2d, n):
        a = ap2d
        return bass.AP(tensor=a.tensor, offset=a.offset + (n - 1) * a.ap[-1][0], ap=[list(a.ap[0]), [-a.ap[-1][0], n]])

    it = 0
    for cc in range(4):
        k.dma("sp", u[:], self.p_rgx[cc], reads=["p_rgx"], writes=["rg_u"])
        k.dma("sp", gg[:], self.p_rgg[cc], reads=["p_rgg"], writes=["rg_gg"])
        for d in range(2):
            order = list(range(len(tiles))) if d == 0 else [0] + list(range(len(tiles) - 1, 0, -1))
            prev = None
            for ti in order:
                s0, W = tiles[ti]
                seg0, seg1 = (0, NCTX) if s0 < NCTX else (NCTX, T)
                b = it % NB
                it += 1
                xk = f"rg_xc{b}"
                k.op("dve", lambda: nc.vector.tensor_scalar(out=xc[b][:, :W], in0=u[:, s0:s0 + W], scalar1=cw[:, d, cc, 3:4], scalar2=cb[:, d, cc:cc + 1],
                                                            op0=ALU.mult, op1=ALU.add), reads=["rg_u", "rg_cw", "rg_cb"], writes=[xk])
                for j in range(3):
                    sh = 3 - j
                    if d == 0:
                        lo = max(0, seg0 + sh - s0)
                        if lo >= W:
                            continue
                        o_ap = xc[b][:, lo:W]; i_ap = u[:, s0 + lo - sh:s0 + W - sh]
                    else:
                        hi = min(W, seg1 - sh - s0)
                        if hi <= 0:
                            continue
                        o_ap = xc[b][:, 0:hi]; i_ap = u[:, s0 + sh:s0 + hi + sh]
                    k.op("dve", lambda: nc.vector.scalar_tensor_tensor(out=o_ap, in0=i_ap, scalar=cw[:, d, cc, j:j + 1], in1=o_ap, op0=ALU.mult, op1=ALU.add),
                         reads=["rg_u", "rg_cw", xk], writes=[xk])
                k.op("act", lambda: nc.scalar.copy(out=xcb[b][:, :W], in_=xc[b][:, :W]), reads=[xk], writes=[f"rg_xcb{b}"])
                k.op("pe", lambda: nc.tensor.matmul(pa[b][:, :W], lhsT=wbd[:, 0, d, cc, :], rhs=xcb[b][:, :W], start=True, stop=True),
                     reads=["rg_wbd", f"rg_xcb{b}"], writes=[f"rg_pa{b}"])
                k.op("pe", lambda: nc.tensor.matmul(px[b][:, :W], lhsT=wbd[:, 1, d, cc, :], rhs=xcb[b][:, :W], start=True, stop=True),
                     reads=["rg_wbd", f"rg_xcb{b}"], writes=[f"rg_px{b}"])
                k.op("act", lambda: nc.scalar.activation(out=gr[b][:, :W], in_=pa[b][:, :W], func=AF.Sigmoid, bias=ba[:, d, cc:cc + 1]),
                     reads=[f"rg_pa{b}", "rg_ba"], writes=[f"rg_gr{b}"])
                k.op("act", lambda: nc.scalar.activation(out=gi[b][:, :W], in_=px[b][:, :W], func=AF.Sigmoid, bias=bx[:, d, cc:cc + 1]),
                     reads=[f"rg_px{b}", "rg_bx"], writes=[f"rg_gi{b}"])
                k.op("act", lambda: nc.scalar.activation(out=aa[b][:, :W], in_=gr[b][:, :W], func=AF.Exp, scale=nsp[:, d, cc:cc + 1]),
                     reads=[f"rg_gr{b}", "rg_nsp"], writes=[f"rg_a{b}"])
                k.op("dve", lambda: nc.vector.scalar_tensor_tensor(out=t1[b][:, :W], in0=aa[b][:, :W], scalar=-1.0, in1=aa[b][:, :W], op0=ALU.mult, op1=ALU.mult),
                     reads=[f"rg_a{b}"], writes=[f"rg_t1{b}"])
                k.op("act", lambda: nc.scalar.activation(out=t1[b][:, :W], in_=t1[b][:, :W], func=AF.Sqrt, bias=self.onec[:, 0:1]),
                     reads=[f"rg_t1{b}", "onec"], writes=[f"rg_t1{b}"])
                k.op("dve", lambda: nc.vector.tensor_tensor(out=t3[b][:, :W], in0=gi[b][:, :W], in1=xc[b][:, :W], op=ALU.mult),
                     reads=[f"rg_gi{b}", xk], writes=[f"rg_t3{b}"])
                k.op("dve", lambda: nc.vector.tensor_tensor(out=t3[b][:, :W], in0=t3[b][:, :W], in1=t1[b][:, :W], op=ALU.mult),
                     reads=[f"rg_t3{b}", f"rg_t1{b}"], writes=[f"rg_t3{b}"])
                init = 0.0 if prev is None else prev[0]
                rd = [f"rg_a{b}", f"rg_t3{b}"] + ([] if prev is None else [prev[1]])
                if d == 0:
                    k.op("dve", lambda: nc.vector.tensor_tensor_scan(out=hsum[:, s0:s0 + W], data0=aa[b][:, :W], data1=t3[b][:, :W], initial=init,
                                                                     op0=ALU.mult, op1=ALU.add), reads=rd, writes=["rg_hsum"])
                    prev = (hsum[:, s0 + W - 1:s0 + W], "rg_hsum")
                else:
                    k.op("dve", lambda: nc.vector.tensor_tensor_scan(out=rev(hs[b][:, :W], W), data0=rev(aa[b][:, :W], W), data1=rev(t3[b][:, :W], W),
                                                                     initial=init, op0=ALU.mult, op1=ALU.add), reads=rd, writes=[f"rg_hs{b}"])
                    prev = (hs[b][:, 0:1], f"rg_hs{b}")
                    k.op("pool", lambda: nc.gpsimd.tensor_tensor(out=t1[b][:, :W], in0=hs[b][:, :W], in1=hsum[:, s0:s0 + W], op=ALU.add),
                         reads=[f"rg_hs{b}", "rg_hsum"], writes=[f"rg_t1{b}"])
                    k.op("pool", lambda: nc.gpsimd.tensor_tensor(out=yo[b][:, :W], in0=t1[b][:, :W], in1=gg[:, s0:s0 + W], op=ALU.mult),
                         reads=[f"rg_t1{b}", "rg_gg"], writes=[f"rg_yo{b}"])
                    k.dma("sp", self.y_rg[cc, :, s0:s0 + W], yo[b][:, :W], reads=[f"rg_yo{b}"], writes=["y_rg"])
    k.end_phase()


Prog.declare_rg = declare_rg
Prog.stage_rglru = stage_rglru


def prep_rg(inp):
    sh = {}
    cw = np.asarray(inp["rg_conv_w"], np.float32)
    sh["rg_cwT"] = np.ascontiguousarray(cw.reshape(DEPTH, 2, 4, 4, 128).transpose(4, 0, 1, 3, 2))
    for nm, key in [("rg_cbT", "rg_conv_b"), ("rg_baT", "rg_ba"), ("rg_bxT", "rg_bx"), ("rg_lamT", "rg_lambda")]:
        sh[nm] = chunkT(inp[key])
    for nm, key in [("rg_waBD", "rg_wa"), ("rg_wxBD", "rg_wx")]:
        w = np.asarray(inp[key], np.float32)
        bd = np.zeros((128, DEPTH, 2, 4, 128), np.float32)
        for g in range(8):
            cc, h = g // 2, g % 2
            bd[h * 64:(h + 1) * 64, :, :, cc, h * 64:(h + 1) * 64] = w[:, :, g].transpose(2, 0, 1, 3)
        sh[nm] = bd
    return sh


def gcols(g):
    if g < 2:
        return slice(g * 128, (g + 1) * 128)
    col = g - 2
    return slice(NCTX + col, NCTX + col + 127 * 64 + 1, 64)


NGRP = 66


def declare_gla(self):
    k = self.k
    self.gla_wlr = self.inp("gla_wlr", [16, DEPTH, 2, 256])
    self.gla_nblr = self.inp("gla_blr", [64, DEPTH, 2, 4])
    self.gla_ng = self.inp("gla_ng", [128, DEPTH])
    self.c_scanmask = self.inp("c_scanmask", [64, 2, 128])
    self.c_tri = self.inp("c_tri", [128, 2, 128])
    self.y_gla = k.dram("y_gla", [4, 128, T], BF16)


def revap(a, n):
    return bass.AP(tensor=a.tensor, offset=a.offset + (n - 1) * a.ap[-1][0], ap=[list(a.ap[0]), [-a.ap[-1][0], n]])


def stage_gla(self, l, tiles=TILES, lvl=9, maxstep=NGRP):
    k, nc = self.k, self.nc
    k.begin_phase()
    wlr = k.sb("gl_wlr", [16, 2, 256]); blr = k.sb("gl_blr", [64, 2, 4]); nblr = k.sb("gl_nblr", [64, 2, 4]); ng = k.sb("gl_ng", [128, DEPTH])
    smask = k.sb("gl_smask", [64, 2, 128]); tri = k.sb("gl_tri", [128, 2, 128])
    k.dma("sp", wlr[:], self.gla_wlr[:, l], writes=["gl_wlr"])
    k.dma("sp", blr[:], self.gla_nblr[:, l], writes=["gl_blr"])
    k.dma("sp", ng[:], self.gla_ng, writes=["gl_ng"])
    k.dma("sp", smask[:], self.c_scanmask, writes=["gl_smask"])
    k.dma("sp", tri[:], self.c_tri, writes=["gl_tri"])
    k.op("dve", lambda: nc.vector.tensor_scalar(out=nblr[:], in0=blr[:], scalar1=-1.0, scalar2=None, op0=ALU.mult), reads=["gl_blr"], writes=["gl_nblr"])
    qh = k.sb("gl_q", [64, T], BF16); kh = k.sb("gl_k", [64, T], BF16); vh = k.sb("gl_v", [128, T], BF16)
    la = [k.sb(f"gl_la{d}", [64, T]) for d in range(2)]
    oacc = k.sb("gl_oacc", [128, T])
    lrt = [k.sb(f"gl_lrt{i}", [16, 512]) for i in range(2)]
    ps_la = k.ps("gl_psla", [128, 512])
    ps_T = [k.ps(f"gl_psT{d}", [128, 1024], BF16) for d in range(2)]
    ps_A = [[k.ps(f"gl_psA{d}{p}", [128, 4, 128]) for p in range(2)] for d in range(2)]

    def mk(name, shape, dt=F32):
        return [[k.sb(f"{name}{d}{p}", shape, dt) for p in range(2)] for d in range(2)]
    vT = mk("gl_vT", [128, 128], BF16); cum = mk("gl_cum", [64, 128]); eq = mk("gl_eq", [64, 128]); ek = mk("gl_ek", [64, 128])
    dec = mk("gl_dec", [64, 2]); qg = mk("gl_qg", [64, 128], BF16); kd = mk("gl_kd", [64, 128], BF16); kw = mk("gl_kw", [64, 128], BF16)
    kwT = mk("gl_kwT", [128, 2, 64], BF16); scm = mk("gl_scm", [128, 128], BF16)
    for d in range(2):
        for p in range(2):
            k.op("pool", lambda: nc.gpsimd.memset(kwT[d][p][:], 0.0), writes=[f"gl_kwT{d}{p}"])
    NS = 6
    S = [[k.sb(f"gl_S{d}_{r}", [64, 128]) for r in range(NS)] for d in range(2)]
    qgf = mk("gl_qgf", [64, 128])
    scur = [0, 0]
    fsq = k.sb("gl_fsq", [128, 512], BF16); frs = k.sb("gl_frs", [128, 512]); fgl = k.sb("gl_fgl", [128, 512]); fy = k.sb("gl_fy", [128, 512], BF16)
    orders = [list(range(NGRP)), [1, 0] + list(range(NGRP - 1, 1, -1))]
    nstep = min(NGRP, maxstep)

    def prep(step, d):
        p = step % 2
        sfx = f"{d}{p}"
        g = orders[d][step]
        cs = gcols(g)
        pT, pA = ps_T[d], ps_A[d][p]
        Tk, Ak = f"gl_psT{d}", f"gl_psA{sfx}"
        k.op("pe", lambda: nc.tensor.transpose(out=pT[:, 0:128], in_=vh[:, cs], identity=self.ident_bf[:]), reads=["gl_v", "ident_bf"], writes=[Tk])
        k.op("act", lambda: nc.scalar.copy(out=vT[d][p][:], in_=pT[:, 0:128]), writes=[Tk, f"gl_vT{sfx}"])
        lav = la[d][:, cs]
        c_ = cum[d][p]
        if d == 0:
            k.op("dve", lambda: nc.vector.tensor_tensor_scan(out=c_[:], data0=smask[:, 0, :], data1=lav, initial=0.0, op0=ALU.mult, op1=ALU.add),
                 reads=[f"gl_la{d}", "gl_smask"], writes=[f"gl_cum{sfx}"])
            cl = c_[:, 63:128:64]
        else:
            k.op("dve", lambda: nc.vector.tensor_tensor_scan(out=revap(c_[:], 128), data0=revap(smask[:, 1, :], 128), data1=revap(lav, 128),
                                                             initial=0.0, op0=ALU.mult, op1=ALU.add),
                 reads=[f"gl_la{d}", "gl_smask"], writes=[f"gl_cum{sfx}"])
            cl = c_[:, 0:128:64]
        k.op("act", lambda: nc.scalar.activation(out=eq[d][p][:], in_=c_[:], func=AF.Exp), reads=[f"gl_cum{sfx}"], writes=[f"gl_eq{sfx}"])
        k.op("act", lambda: nc.scalar.activation(out=ek[d][p][:], in_=c_[:], func=AF.Exp, scale=-1.0), reads=[f"gl_cum{sfx}"], writes=[f"gl_ek{sfx}"])
        k.op("act", lambda: nc.scalar.activation(out=dec[d][p][:], in_=cl, func=AF.Exp), reads=[f"gl_cum{sfx}"], writes=[f"gl_dec{sfx}"])
        k.op("dve", lambda: nc.vector.tensor_tensor(out=qgf[d][p][:], in0=qh[:, cs], in1=eq[d][p][:], op=ALU.mult), reads=["gl_q", f"gl_eq{sfx}"], writes=[f"gl_qgf{sfx}"])
        k.op("act", lambda: nc.scalar.copy(out=qg[d][p][:], in_=qgf[d][p][:]), reads=[f"gl_qgf{sfx}"], writes=[f"gl_qg{sfx}"])
        k.op("dve", lambda: nc.vector.tensor_tensor(out=kd[d][p][:], in0=kh[:, cs], in1=ek[d][p][:], op=ALU.mult), reads=["gl_k", f"gl_ek{sfx}"], writes=[f"gl_kd{sfx}"])
        k.op("dve", lambda: nc.vector.tensor_tensor(out=kw[d][p][:].rearrange("p (j c) -> p j c", j=2), in0=kd[d][p][:].rearrange("p (j c) -> p j c", j=2),
                                                    in1=bc(dec[d][p][:].unsqueeze(2), [64, 2, 64]), op=ALU.mult),
             reads=[f"gl_kd{sfx}", f"gl_dec{sfx}"], writes=[f"gl_kw{sfx}"])
        if lvl < 3:
            return
        k.op("pe", lambda: nc.tensor.transpose(out=pT[:, 128:192], in_=kw[d][p][:], identity=self.ident_bf[:64, :64]), reads=[f"gl_kw{sfx}", "ident_bf"], writes=[Tk])
        for j in range(2):
            k.op("act", lambda: nc.scalar.copy(out=kwT[d][p][j * 64:(j + 1) * 64, j, :], in_=pT[j * 64:(j + 1) * 64, 128:192]), writes=[Tk, f"gl_kwT{sfx}"])
        k.op("pe", lambda: nc.tensor.matmul(pA[:, 0, :], lhsT=kd[d][p][:], rhs=qg[d][p][:], start=True, stop=True),
             reads=[f"gl_kd{sfx}", f"gl_qg{sfx}"], writes=[Ak])
        k.op("dve", lambda: nc.vector.tensor_tensor(out=scm[d][p][:], in0=pA[:, 0, :], in1=tri[:, d, :], op=ALU.mult),
             reads=["gl_tri"], writes=[Ak, f"gl_scm{sfx}"])
        if lvl < 4:
            return
        for j in range(2):
            k.op("pe", lambda: nc.tensor.matmul(pA[:64, 2 + j, :], lhsT=kwT[d][p][:, j, :], rhs=vT[d][p][:], start=True, stop=True),
                 reads=[f"gl_kwT{sfx}", f"gl_vT{sfx}"], writes=[Ak])

    def state(step, d):
        p = step % 2
        sfx = f"{d}{p}"
        g = orders[d][step]
        cs = gcols(g)
        pA = ps_A[d][p]
        Ak = f"gl_psA{sfx}"
        if lvl < 4:
            return
        sidx = {}
        for j in ([0, 1] if d == 0 else [1, 0]):
            r0 = scur[d]
            r1 = (r0 + 1) % NS
            sidx[j] = r0
            k.op("dve", lambda: nc.vector.scalar_tensor_tensor(out=S[d][r1][:], in0=S[d][r0][:], scalar=dec[d][p][:, j:j + 1], in1=pA[:64, 2 + j, :], op0=ALU.mult, op1=ALU.add),
                 reads=[f"gl_S{d}_{r0}", f"gl_dec{sfx}"], writes=[Ak, f"gl_S{d}_{r1}"])
            scur[d] = r1
        if lvl < 5:
            return
        k.op("pe", lambda: nc.tensor.matmul(pA[:, 1, :], lhsT=vT[d][p][:], rhs=scm[d][p][:], start=True, stop=False),
             reads=[f"gl_vT{sfx}", f"gl_scm{sfx}"], writes=[Ak])
        for j in range(2):
            r = sidx[j]
            k.op("pe", lambda: nc.tensor.matmul(pA[:, 1, j * 64:(j + 1) * 64], lhsT=S[d][r][:], rhs=qgf[d][p][:, j * 64:(j + 1) * 64], start=False, stop=(j == 1)),
                 reads=[f"gl_S{d}_{r}", f"gl_qgf{sfx}"], writes=[Ak])
        k.op("dve", lambda: nc.vector.tensor_tensor(out=oacc[:, cs], in0=oacc[:, cs], in1=pA[:, 1, :], op=ALU.add),
             reads=["gl_oacc"], writes=[Ak, "gl_oacc"])

    for hd in range(4):
        k.dma("sp", qh[:], self.p_gqk[hd // 2, (hd % 2) * 64:(hd % 2) * 64 + 64, :], reads=["p_gqk"], writes=["gl_q"])
        k.dma("sp", kh[:], self.p_gqk[2 + hd // 2, (hd % 2) * 64:(hd % 2) * 64 + 64, :], reads=["p_gqk"], writes=["gl_k"])
        k.dma("sp", vh[:], self.p_gv[hd], reads=["p_gv"], writes=["gl_v"])
        k.op("pool", lambda: nc.gpsimd.memset(oacc[:], 0.0), writes=["gl_oacc"])
        it = 0
        for d in range(2):
            scur[d] = 0
            k.op("pool", lambda: nc.gpsimd.memset(S[d][0][:], 0.0), writes=[f"gl_S{d}_0"])
            for (s0, W) in tiles:
                b = it % 2
                it += 1
                k.dma("sp", lrt[b][:, :W], self.p_glr[d, :, s0:s0 + W], reads=["p_glr"], writes=[f"gl_lrt{b}"])
                k.op("pe", lambda: nc.tensor.matmul(ps_la[:64, :W], lhsT=wlr[:, d, hd * 64:(hd + 1) * 64], rhs=lrt[b][:, :W], start=True, stop=True),
                     reads=["gl_wlr", f"gl_lrt{b}"], writes=["gl_psla"])
                k.op("act", lambda: nc.scalar.activation(out=la[d][:, s0:s0 + W], in_=ps_la[:64, :W], func=AF.Exp, scale=-1.0, bias=nblr[:, d, hd:hd + 1]),
                     reads=["gl_nblr"], writes=["gl_psla", f"gl_la{d}"])
                k.op("act", lambda: nc.scalar.activation(out=la[d][:, s0:s0 + W], in_=la[d][:, s0:s0 + W], func=AF.Ln, bias=self.onec[:64, 0:1]),
                     reads=[f"gl_la{d}", "onec"], writes=[f"gl_la{d}"])
                k.op("dve", lambda: nc.vector.tensor_scalar(out=la[d][:, s0:s0 + W], in0=la[d][:, s0:s0 + W], scalar1=-1.0 / 16.0, scalar2=None, op0=ALU.mult),
                     reads=[f"gl_la{d}"], writes=[f"gl_la{d}"])
        if lvl >= 2:
            for d in range(2):
                prep(0, d)
            for step in range(nstep):
                if step + 1 < nstep:
                    for d in range(2):
                        prep(step + 1, d)
                for d in range(2):
                    state(step, d)
        for (s0, W) in (tiles if lvl >= 6 else []):
            k.op("act", lambda: nc.scalar.activation(out=fsq[:, :W], in_=oacc[:, s0:s0 + W], func=AF.Square), reads=["gl_oacc"], writes=["gl_fsq"])
            k.op("pe", lambda: nc.tensor.matmul(ps_la[:, :W], lhsT=self.ones_bf[:], rhs=fsq[:, :W], start=True, stop=True), reads=["gl_fsq", "ones_bf"], writes=["gl_psla"])
            k.op("act", lambda: nc.scalar.activation(out=frs[:, :W], in_=ps_la[:, :W], func=AF.Sqrt, scale=1.0 / 128.0, bias=self.epsc[:, 0:1]),
                 reads=["epsc"], writes=["gl_psla", "gl_frs"])
            k.op("dve", lambda: nc.vector.reciprocal(out=frs[:, :W], in_=frs[:, :W]), reads=["gl_frs"], writes=["gl_frs"])
            k.dma("sp", fgl[:, :W], self.p_glag[hd, :, s0:s0 + W], reads=["p_glag"], writes=["gl_fgl"])
            k.op("dve", lambda: nc.vector.scalar_tensor_tensor(out=frs[:, :W], in0=oacc[:, s0:s0 + W], scalar=ng[:, l:l + 1], in1=frs[:, :W], op0=ALU.mult, op1=ALU.mult),
                 reads=["gl_oacc", "gl_ng", "gl_frs"], writes=["gl_frs"])
            k.op("dve", lambda: nc.vector.tensor_tensor(out=fy[:, :W], in0=frs[:, :W], in1=fgl[:, :W], op=ALU.mult), reads=["gl_frs", "gl_fgl"], writes=["gl_fy"])
            k.dma("sp", self.y_gla[hd, :, s0:s0 + W], fy[:, :W], reads=["gl_fy"], writes=["y_gla"])
    k.end_phase()


Prog.declare_gla = declare_gla
Prog.stage_gla = stage_gla


def prep_gla(inp):
    sh = {}
    sh["gla_wlr"] = np.ascontiguousarray(np.asarray(inp["gla_w_lr"], np.float32).transpose(2, 0, 1, 3))
    b = np.asarray(inp["gla_b_lr"], np.float32).reshape(DEPTH, 2, 4, 64)
    sh["gla_blr"] = np.ascontiguousarray(b.transpose(3, 0, 1, 2))
    sh["gla_ng"] = np.ascontiguousarray(np.asarray(inp["gla_norm_g"], np.float32).T)
    sm = np.ones((64, 2, 128), np.float32)
    sm[:, 0, 0] = 0; sm[:, 0, 64] = 0; sm[:, 1, 127] = 0; sm[:, 1, 63] = 0
    sh["c_scanmask"] = sm
    s = np.arange(128)[:, None]; c = np.arange(128)[None, :]
    same = (s // 64) == (c // 64)
    tri = np.zeros((128, 2, 128), np.float32)
    tri[:, 0, :] = (same & (s <= c)); tri[:, 1, :] = (same & (s > c))
    sh["c_tri"] = tri
    return sh


def declare_merge(self):
    k = self.k
    self.w_bo = [self.inp(n, [DEPTH, 512, D]) for n in ("w_hy_o", "w_rg_o", "w_gla_o")]
    self.w_out = self.inp("w_out", [DEPTH, D, D])
    self.y_hy = k.dram("y_hy", [4, 128, T], BF16)


def stage_merge(self, l, xsrc, tiles=TILES):
    k, nc = self.k, self.nc
    k.begin_phase()
    wst = k.sb("mg_wst", [128, 4, 1024])
    wb = [k.sb(f"mg_wb{i}", [128, 4, 1024], BF16) for i in range(3)]
    wo = k.sb("mg_wo", [128, 8, 1024], BF16)
    for i in range(3):
        k.dma("sp", wst[:], self.w_bo[i][l].rearrange("(k p) c -> p k c", p=128), writes=["mg_wst"])
        k.op("pool", lambda: nc.gpsimd.tensor_copy(out=wb[i][:], in_=wst[:]), reads=["mg_wst"], writes=[f"mg_wb{i}"])
    for hh in range(2):
        k.dma("sp", wst[:], self.w_out[l, hh * 512:(hh + 1) * 512, :].rearrange("(k p) c -> p k c", p=128), writes=["mg_wst"])
        k.op("pool", lambda: nc.gpsimd.tensor_copy(out=wo[:, hh * 4:(hh + 1) * 4, :], in_=wst[:]), reads=["mg_wst"], writes=["mg_wo"])
    ysrc = [self.y_hy, self.y_rg, self.y_gla]
    ykeys = ["y_hy", "y_rg", "y_gla"]
    NB = 2
    yt = [[k.sb(f"mg_y{i}_{b}", [128, 4, 512], BF16) for b in range(NB)] for i in range(3)]
    gt = [k.sb(f"mg_g{b}", [128, 8, 512]) for b in range(2)]
    xt = [k.sb(f"mg_x{b}", [128, 8, 512]) for b in range(NB)]
    macc = k.sb("mg_macc", [128, 8, 512])
    tmp = [k.sb(f"mg_tmp{b}", [128, 512]) for b in range(2)]
    mb = [k.sb(f"mg_mb{b}", [128, 8, 512], BF16) for b in range(NB)]
    NP = 4
    pp = [k.ps(f"mg_p{i}", [128, 512]) for i in range(NP)]
    xv = xsrc.rearrange("k p t -> p k t")
    xo = self.xres.rearrange("k p t -> p k t")
    pc = 0
    tc_ = 0
    gc_ = 0
    for ti, (s0, W) in enumerate(tiles):
        b = ti % NB
        j = 1 if s0 < NCTX else 0
        k.dma("sp", xt[b][:, :, :W], xv[:, :, s0:s0 + W], reads=["xres"], writes=[f"mg_x{b}"])
        for i in range(3):
            k.dma("sp", yt[i][b][:, :, :W], ysrc[i].rearrange("k p t -> p k t")[:, :, s0:s0 + W], reads=[ykeys[i]], writes=[f"mg_y{i}_{b}"])
        for i in range(3):
            gb = gc_ % 2
            gc_ += 1
            k.dma("sp", gt[gb][:, :, :W], self.p_mg[i * 8:(i + 1) * 8].rearrange("k p t -> p k t")[:, :, s0:s0 + W], reads=["p_mg"], writes=[f"mg_g{gb}"])
            for dc in range(8):
                pb = pc % NP
                pc += 1
                for kk in range(4):
                    k.op("pe", lambda: nc.tensor.matmul(pp[pb][:, :W], lhsT=wb[i][:, kk, dc * 128:(dc + 1) * 128], rhs=yt[i][b][:, kk, :W], start=(kk == 0), stop=(kk == 3)),
                         reads=[f"mg_wb{i}", f"mg_y{i}_{b}"], writes=[f"mg_p{pb}"])
                if i == 0:
                    k.op("dve", lambda: nc.vector.tensor_tensor(out=macc[:, dc, :W], in0=pp[pb][:, :W], in1=gt[gb][:, dc, :W], op=ALU.mult),
                         reads=[f"mg_g{gb}"], writes=[f"mg_p{pb}", f"mg_macc{dc}"])
                else:
                    tb = tc_ % 2
                    tc_ += 1
                    k.op("dve", lambda: nc.vector.tensor_tensor(out=tmp[tb][:, :W], in0=pp[pb][:, :W], in1=gt[gb][:, dc, :W], op=ALU.mult),
                         reads=[f"mg_g{gb}"], writes=[f"mg_p{pb}", f"mg_tmp{tb}"])
                    if i == 1:
                        k.op("pool", lambda: nc.gpsimd.tensor_tensor(out=macc[:, dc, :W], in0=macc[:, dc, :W], in1=tmp[tb][:, :W], op=ALU.add),
                             reads=[f"mg_tmp{tb}"], writes=[f"mg_macc{dc}"])
                    else:
                        k.op("pool", lambda: nc.gpsimd.tensor_tensor(out=mb[b][:, dc, :W], in0=macc[:, dc, :W], in1=tmp[tb][:, :W], op=ALU.add),
                             reads=[f"mg_tmp{tb}", f"mg_macc{dc}"], writes=[f"mg_mb{b}"])
        for dc in range(8):
            pb = pc % NP
            pc += 1
            for kk in range(8):
                k.op("pe", lambda: nc.tensor.matmul(pp[pb][:, :W], lhsT=wo[:, kk, dc * 128:(dc + 1) * 128], rhs=mb[b][:, kk, :W], start=(kk == 0), stop=(kk == 7)),
                     reads=["mg_wo", f"mg_mb{b}"], writes=[f"mg_p{pb}"])
            k.op("dve", lambda: nc.vector.scalar_tensor_tensor(out=xt[b][:, dc, :W], in0=pp[pb][:, :W], scalar=self.modT[:, 16 + dc, j:j + 1], in1=xt[b][:, dc, :W],
                                                               op0=ALU.mult, op1=ALU.add),
                 reads=["modT"], writes=[f"mg_p{pb}", f"mg_x{b}"])
        k.dma("pool", xo[:, :, s0:s0 + W], xt[b][:, :, :W], reads=[f"mg_x{b}"], writes=["xres"])
    k.end_phase()


Prog.declare_merge = declare_merge
Prog.stage_merge = stage_merge


def prep_merge(inp):
    return {n: np.ascontiguousarray(inp[n], np.float32) for n in ("w_hy_o", "w_rg_o", "w_gla_o", "w_out")}


NEXP = 16384


def declare_peer(self):
    k = self.k
    self.peer_wq = self.inp("peer_wq", [DEPTH, D, 2048])
    self.peer_keysT = self.inp("peer_keysT", [128, DEPTH, 16, 128])
    self.peer_u = self.inp("peer_u", [DEPTH, NEXP, D])
    self.peer_v = self.inp("peer_v", [DEPTH, NEXP, D])
    self.c_iota16 = self.inp("c_iota16", [128, 16])
    self.uv_bf = k.dram("uv_bf", [NEXP, 2, D], BF16)


def stage_peer_prep(self, l):
    k, nc = self.k, self.nc
    k.begin_phase()
    st = [k.sb(f"pp_st{i}", [128, 4, 1024]) for i in range(3)]
    sbf = [k.sb(f"pp_bf{i}", [128, 4, 1024], BF16) for i in range(3)]
    it = 0
    dv = self.uv_bf.rearrange("(p r) two c -> p r two c", p=128)
    for which, tab in enumerate((self.peer_u, self.peer_v)):
        src = tab[l].rearrange("(p r) c -> p r c", p=128)
        for r0 in range(0, 128, 4):
            b = it % 3
            eng = ["dve", "act", "pool"][it % 3]
            it += 1
            k.dma("sp", st[b][:], src[:, r0:r0 + 4, :], writes=[f"pp_st{b}"])
            if eng == "dve":
                k.op("dve", lambda: nc.vector.tensor_copy(out=sbf[b][:], in_=st[b][:]), reads=[f"pp_st{b}"], writes=[f"pp_bf{b}"])
            elif eng == "act":
                k.op("act", lambda: nc.scalar.copy(out=sbf[b][:], in_=st[b][:]), reads=[f"pp_st{b}"], writes=[f"pp_bf{b}"])
            else:
                k.op("pool", lambda: nc.gpsimd.tensor_copy(out=sbf[b][:], in_=st[b][:]), reads=[f"pp_st{b}"], writes=[f"pp_bf{b}"])
            k.dma("act", dv[:, r0:r0 + 4, which, :], sbf[b][:], reads=[f"pp_bf{b}"], writes=["uv_bf"])
    k.end_phase()


def stage_peer(self, l, t_tiles, lvl=9):
    k, nc = self.k, self.nc
    k.begin_phase()
    wq = k.sb("pr_wq", [128, 8, 2048], BF16)
    k.begin_phase()
    wst = [k.sb(f"pr_wst{i}", [128, 8, 512]) for i in range(2)]
    for c4 in range(4):
        k.dma("sp", wst[c4 % 2][:], self.peer_wq[l, :, c4 * 512:(c4 + 1) * 512].rearrange("(k p) c -> p k c", p=128), writes=[f"pr_wst{c4 % 2}"])
        k.op("pool", lambda: nc.gpsimd.tensor_copy(out=wq[:, :, c4 * 512:(c4 + 1) * 512], in_=wst[c4 % 2][:]), reads=[f"pr_wst{c4 % 2}"], writes=["pr_wq"])
    k.end_phase()
    keysT = k.sb("pr_keysT", [128, 16, 128])
    k.dma("sp", keysT[:], self.peer_keysT[:, l], writes=["pr_keysT"])
    iota16 = k.sb("pr_iota", [128, 16])
    k.dma("sp", iota16[:], self.c_iota16, writes=["pr_iota"])
    h2 = [k.sb(f"pr_h2{i}", [128, 8, 128], BF16) for i in range(2)]
    xt = [k.sb(f"pr_x{i}", [128, 8, 128]) for i in range(2)]
    qT = k.sb("pr_qT", [128, 16, 128])
    s_all = [k.sb(f"pr_s{i}", [128, 16, 128]) for i in range(2)]
    work = k.sb("pr_work", [128, 16, 128])
    s_top = [k.sb(f"pr_stop{i}", [128, 16, 16]) for i in range(2)]
    i_top = [k.sb(f"pr_itop{i}", [128, 16, 16], U32) for i in range(2)]
    i_f = k.sb("pr_if", [128, 16, 16])
    cand = k.sb("pr_cand", [128, 8, 256]); cwork = work[:].rearrange("p (h x) n -> p h (x n)", h=8)
    best = k.sb("pr_best", [128, 8, 16]); pos = k.sb("pr_pos", [128, 8, 16], U32)
    pa_u = k.sb("pr_pau", [128, 8, 16], U32); pb_u = k.sb("pr_pbu", [128, 8, 16], U32)
    pa_f = k.sb("pr_paf", [128, 8, 16]); pb_f = k.sb("pr_pbf", [128, 8, 16])
    oh = k.sb("pr_oh", [128, 8, 16, 16]); oh2 = k.sb("pr_oh2", [128, 8, 16, 16])
    i1sel = k.sb("pr_i1sel", [128, 8, 16]); i2sel = k.sb("pr_i2sel", [128, 8, 16])
    e_f = k.sb("pr_ef", [128, 128]); e_i = k.sb("pr_ei", [128, 128], I32)
    ex = k.sb("pr_ex", [128, 8, 16]); zz = k.sb("pr_zz", [128, 8]); wgt = k.sb("pr_wgt", [128, 128])
    h2tm = [k.sb(f"pr_h2tm{i}", [128, 1024], BF16) for i in range(2)]
    BS = 8
    NBLK = 128 // BS
    gb = [[k.sb(f"pr_g{a}_{i}", [128, 2048], BF16) for i in range(BS)] for a in range(2)]
    junk = k.sb("pr_junk", [128, 1024], BF16)
    act_ = k.sb("pr_act", [128, 128]); cg = k.sb("pr_cg", [128, 128]); coef = k.sb("pr_coef", [128, 128])
    dg = [k.sb(f"pr_dg{i}", [128, 128], BF16) for i in range(4)]
    po_sb = k.sb("pr_posb", [128, 1024])
    psA = [k.ps(f"pr_psA{i}", [128, 4, 128]) for i in range(4)]
    psB = k.ps("pr_psB", [128, 8, 128], BF16)
    psO = [k.ps(f"pr_psO{i}", [128, 512]) for i in range(2)]
    hv = self.hT.rearrange("k p t -> p k t")
    xo = self.xres.rearrange("k p t -> p k t")
    uvt = self.uv_bf.rearrange("e two c -> e (two c)")
    NTL = len(t_tiles)

    def front_a(i):
        t0 = t_tiles[i]
        b = i % 2
        k.dma("sp", h2[b][:], hv[:, :, t0:t0 + 128], reads=["hT"], writes=[f"pr_h2{b}"])
        k.dma("sp", xt[b][:], xo[:, :, t0:t0 + 128], reads=["xres"], writes=[f"pr_x{b}"])
        for hp in range(16):
            pa = psA[(hp // 4) % 4]
            pk = f"pr_psA{(hp // 4) % 4}"
            for kk in range(8):
                k.op("pe", lambda: nc.tensor.matmul(pa[:, hp % 4, :], lhsT=wq[:, kk, hp * 128:(hp + 1) * 128], rhs=h2[b][:, kk, :], start=(kk == 0), stop=(kk == 7)),
                     reads=["pr_wq", f"pr_h2{b}"], writes=[pk])
            if hp % 4 == 3:
                k.op("act", lambda: nc.scalar.copy(out=qT[:, hp - 3:hp + 1, :], in_=pa[:]), writes=[pk, "pr_qT"])
        for hp in range(16):
            pa = psA[(hp // 4) % 4]
            pk = f"pr_psA{(hp // 4) % 4}"
            k.op("pe", lambda: nc.tensor.matmul(pa[:, hp % 4, :], lhsT=qT[:, hp, :], rhs=keysT[:, hp, :], start=True, stop=True),
                 reads=["pr_qT", "pr_keysT"], writes=[pk])
            if hp % 4 == 3:
                k.op("act", lambda: nc.scalar.copy(out=s_all[b][:, hp - 3:hp + 1, :], in_=pa[:]), writes=[pk, f"pr_s{b}"])
        for kk in range(8):
            k.op("pe", lambda: nc.tensor.transpose(out=psB[:, kk, :], in_=h2[b][:, kk, :], identity=self.ident_bf[:]), reads=[f"pr_h2{b}", "ident_bf"], writes=["pr_psB"])
        k.op("act", lambda: nc.scalar.copy(out=h2tm[b][:], in_=psB[:].rearrange("p k c -> p (k c)")), writes=["pr_psB", f"pr_h2tm{b}"])

    def topk1(i, hp):
        b = i % 2
        sa, st_, it_ = s_all[b], s_top[b], i_top[b]
        k.op("dve", lambda: nc.vector.max(out=st_[:, hp, 0:8], in_=sa[:, hp, :]), reads=[f"pr_s{b}"], writes=[f"pr_stop{b}"])
        k.op("dve", lambda: nc.vector.max_index(out=it_[:, hp, 0:8], in_max=st_[:, hp, 0:8], in_values=sa[:, hp, :]), reads=[f"pr_s{b}", f"pr_stop{b}"], writes=[f"pr_itop{b}"])
        k.op("dve", lambda: nc.vector.match_replace(out=work[:, hp, :], in_to_replace=st_[:, hp, 0:8], in_values=sa[:, hp, :], imm_value=-1e30),
             reads=[f"pr_s{b}", f"pr_stop{b}"], writes=["pr_work"])
        k.op("dve", lambda: nc.vector.max(out=st_[:, hp, 8:16], in_=work[:, hp, :]), reads=["pr_work"], writes=[f"pr_stop{b}"])
        k.op("dve", lambda: nc.vector.max_index(out=it_[:, hp, 8:16], in_max=st_[:, hp, 8:16], in_values=work[:, hp, :]), reads=["pr_work", f"pr_stop{b}"], writes=[f"pr_itop{b}"])

    def front_b(i):
        b = i % 2
        k.op("dve", lambda: nc.vector.tensor_copy(out=i_f[:], in_=i_top[b][:]), reads=[f"pr_itop{b}"], writes=["pr_if"])
        st4 = s_top[b][:].rearrange("p (h two) a -> p h two a", two=2)
        if4 = i_f[:].rearrange("p (h two) a -> p h two a", two=2)
        cand4 = cand[:].rearrange("p h (a b) -> p h a b", a=16)
        k.op("dve", lambda: nc.vector.tensor_tensor(out=cand4, in0=bc(st4[:, :, 0, :].unsqueeze(3), [128, 8, 16, 16]), in1=bc(st4[:, :, 1, :].unsqueeze(2), [128, 8, 16, 16]), op=ALU.add),
             reads=[f"pr_stop{b}"], writes=["pr_cand"])
        for h in range(8):
            k.op("dve", lambda: nc.vector.max(out=best[:, h, 0:8], in_=cand[:, h, :]), reads=["pr_cand"], writes=["pr_best"])
            k.op("dve", lambda: nc.vector.max_index(out=pos[:, h, 0:8], in_max=best[:, h, 0:8], in_values=cand[:, h, :]), reads=["pr_cand", "pr_best"], writes=["pr_pos"])
            k.op("dve", lambda: nc.vector.match_replace(out=cwork[:, h, :], in_to_replace=best[:, h, 0:8], in_values=cand[:, h, :], imm_value=-1e30),
                 reads=["pr_cand", "pr_best"], writes=["pr_work"])
            k.op("dve", lambda: nc.vector.max(out=best[:, h, 8:16], in_=cwork[:, h, :]), reads=["pr_work"], writes=["pr_best"])
            k.op("dve", lambda: nc.vector.max_index(out=pos[:, h, 8:16], in_max=best[:, h, 8:16], in_values=cwork[:, h, :]), reads=["pr_work", "pr_best"], writes=["pr_pos"])
        k.op("dve", lambda: nc.vector.tensor_single_scalar(out=pa_u[:], in_=pos[:], scalar=4, op=ALU.logical_shift_right), reads=["pr_pos"], writes=["pr_pau"])
        k.op("dve", lambda: nc.vector.tensor_single_scalar(out=pb_u[:], in_=pos[:], scalar=15, op=ALU.bitwise_and), reads=["pr_pos"], writes=["pr_pbu"])
        k.op("dve", lambda: nc.vector.tensor_copy(out=pa_f[:], in_=pa_u[:]), reads=["pr_pau"], writes=["pr_paf"])
        k.op("dve", lambda: nc.vector.tensor_copy(out=pb_f[:], in_=pb_u[:]), reads=["pr_pbu"], writes=["pr_pbf"])
        io4 = bc(iota16[:].unsqueeze(1).unsqueeze(1), [128, 8, 16, 16])
        for (pf, pfk, half, sel, selk) in ((pa_f, "pr_paf", 0, i1sel, "pr_i1sel"), (pb_f, "pr_pbf", 1, i2sel, "pr_i2sel")):
            k.op("dve", lambda: nc.vector.tensor_tensor(out=oh[:], in0=bc(pf[:].unsqueeze(3), [128, 8, 16, 16]), in1=io4, op=ALU.is_equal),
                 reads=[pfk, "pr_iota"], writes=["pr_oh"])
            k.op("dve", lambda: nc.vector.tensor_tensor(out=oh2[:], in0=oh[:], in1=bc(if4[:, :, half, :].unsqueeze(2), [128, 8, 16, 16]), op=ALU.mult),
                 reads=["pr_oh", "pr_if"], writes=["pr_oh2"])
            k.op("dve", lambda: nc.vector.tensor_reduce(out=sel[:], in_=oh2[:], axis=AX.X, op=ALU.add), reads=["pr_oh2"], writes=[selk])
        k.op("dve", lambda: nc.vector.scalar_tensor_tensor(out=e_f[:], in0=i1sel[:].rearrange("p h r -> p (h r)"), scalar=128.0, in1=i2sel[:].rearrange("p h r -> p (h r)"),
                                                           op0=ALU.mult, op1=ALU.add), reads=["pr_i1sel", "pr_i2sel"], writes=["pr_ef"])
        k.op("dve", lambda: nc.vector.tensor_copy(out=e_i[:], in_=e_f[:]), reads=["pr_ef"], writes=["pr_ei"])
        k.op("dve", lambda: nc.vector.tensor_tensor(out=ex[:], in0=best[:], in1=bc(best[:, :, 0:1], [128, 8, 16]), op=ALU.subtract), reads=["pr_best"], writes=["pr_ex"])
        k.op("act", lambda: nc.scalar.activation(out=ex[:], in_=ex[:], func=AF.Exp), reads=["pr_ex"], writes=["pr_ex"])
        k.op("dve", lambda: nc.vector.tensor_reduce(out=zz[:], in_=ex[:], axis=AX.X, op=ALU.add), reads=["pr_ex"], writes=["pr_zz"])
        k.op("dve", lambda: nc.vector.reciprocal(out=zz[:], in_=zz[:]), reads=["pr_zz"], writes=["pr_zz"])
        k.op("dve", lambda: nc.vector.tensor_tensor(out=wgt[:].rearrange("p (h r) -> p h r", h=8), in0=ex[:], in1=bc(zz[:].unsqueeze(2), [128, 8, 16]), op=ALU.mult),
             reads=["pr_ex", "pr_zz"], writes=["pr_wgt"])

    def gathers(j):
        a = j % 2
        for si in range(BS):
            s_ = j * BS + si
            k.gather(gb[a][si][:], uvt, e_i[:, s_:s_ + 1], reads=["uv_bf", "pr_ei"], writes=[f"pr_g{a}_{si}"])

    def loop(i):
        b = i % 2
        gathers(0)
        for j in range(NBLK):
            a = j % 2
            if j + 1 < NBLK:
                gathers(j + 1)
            for si in range(BS):
                s_ = j * BS + si
                k.op("dve", lambda: nc.vector.scalar_tensor_tensor(out=junk[:], in0=gb[a][si][:, 0:1024], scalar=1.0, in1=h2tm[b][:], op0=ALU.mult, op1=ALU.mult, accum_out=act_[:, s_:s_ + 1]),
                     reads=[f"pr_g{a}_{si}", f"pr_h2tm{b}"], writes=["pr_junk", f"pr_act{a}"])
            sl = slice(j * BS, (j + 1) * BS)
            k.op("act", lambda: nc.scalar.activation(out=cg[:, sl], in_=act_[:, sl], func=AF.Gelu_apprx_tanh), reads=[f"pr_act{a}"], writes=[f"pr_cg{a}"])
            k.op("dve", lambda: nc.vector.tensor_tensor(out=coef[:, sl], in0=cg[:, sl], in1=wgt[:, sl], op=ALU.mult), reads=[f"pr_cg{a}", "pr_wgt"], writes=[f"pr_coef{a}"])
            if i + 1 < NTL and j < 16:
                topk1(i + 1, j)
            for si in range(BS):
                s_ = j * BS + si
                dgi = s_ % 4
                k.op("act", lambda: nc.scalar.activation(out=dg[dgi][:], in_=self.ident_bf[:], func=AF.Copy, scale=coef[:, s_:s_ + 1]),
                     reads=["ident_bf", f"pr_coef{a}"], writes=[f"pr_dg{dgi}"])
                for hf in range(2):
                    k.op("pe", lambda: nc.tensor.matmul(psO[hf][:], lhsT=dg[dgi][:], rhs=gb[a][si][:, 1024 + hf * 512:1024 + (hf + 1) * 512], start=(s_ == 0), stop=(s_ == 127)),
                         reads=[f"pr_dg{dgi}", f"pr_g{a}_{si}"], writes=[f"pr_psO{hf}"])

    def back(i):
        t0 = t_tiles[i]
        b = i % 2
        j = 1 if t0 < NCTX else 0
        for hf in range(2):
            k.op("act", lambda: nc.scalar.copy(out=po_sb[:, hf * 512:(hf + 1) * 512], in_=psO[hf][:]), writes=[f"pr_psO{hf}", "pr_posb"])
        for dc in range(8):
            pa = psA[dc // 4]
            pk = f"pr_psA{dc // 4}"
            k.op("pe", lambda: nc.tensor.transpose(out=pa[:, dc % 4, :], in_=po_sb[:, dc * 128:(dc + 1) * 128], identity=self.ident[:]), reads=["pr_posb", "ident"], writes=[pk])
            k.op("dve", lambda: nc.vector.scalar_tensor_tensor(out=xt[b][:, dc, :], in0=pa[:, dc % 4, :], scalar=self.modT[:, 40 + dc, j:j + 1], in1=xt[b][:, dc, :],
                                                               op0=ALU.mult, op1=ALU.add), reads=["modT"], writes=[pk, f"pr_x{b}"])
        k.dma("sp", xo[:, :, t0:t0 + 128], xt[b][:], reads=[f"pr_x{b}"], writes=["xres"])

    front_a(0)
    for hp in range(16):
        topk1(0, hp)
    front_b(0)
    for i in range(NTL):
        if i + 1 < NTL:
            front_a(i + 1)
        loop(i)
        back(i)
        if i + 1 < NTL:
            front_b(i + 1)
    k.end_phase()


Prog.declare_peer = declare_peer
Prog.stage_peer_prep = stage_peer_prep
Prog.stage_peer = stage_peer


def prep_peer(inp):
    sh = {}
    sh["peer_wq"] = np.ascontiguousarray(inp["peer_wq"], np.float32)
    ks = np.asarray(inp["peer_keys"], np.float32).reshape(DEPTH, 16, 128, 128)
    sh["peer_keysT"] = np.ascontiguousarray(ks.transpose(3, 0, 1, 2))
    sh["peer_u"] = np.ascontiguousarray(inp["peer_u"], np.float32)
    sh["peer_v"] = np.ascontiguousarray(inp["peer_v"], np.float32)
    sh["c_iota16"] = np.tile(np.arange(16, dtype=np.float32)[None, :], (128, 1))
    return sh


HY_W = 512
MAGIC = 12582912.0
TWO_PI_LO = 6.283185


def declare_hyena(self):
    k = self.k
    self.hy_cwT = self.inp("hy_cwT", [128, DEPTH, 12, 3])
    self.hy_cbT = self.inp("hy_cbT", [128, DEPTH, 12])
    self.hy_w1 = self.inp("hy_w1", [33, DEPTH, 64])
    self.hy_w2 = self.inp("hy_w2", [64, DEPTH, 64])
    self.hy_w3 = self.inp("hy_w3", [64, DEPTH, 2048])
    self.hy_b1 = self.inp("hy_b1T", [64, DEPTH]); self.hy_b2 = self.inp("hy_b2T", [64, DEPTH]); self.hy_fr = self.inp("hy_frT", [64, DEPTH])
    self.hy_skipb = self.inp("hy_skipb", [128, DEPTH, 2, 512])
    self.c_F1 = self.inp("c_F1", [128, 512], BF16)
    self.c_TT = self.inp("c_TT", [128, 2, 512])
    self.c_BD = self.inp("c_BD", [128, 4, 128], BF16)
    self.c_R = self.inp("c_R", [128, 2, 256], BF16)
    self.c_TAB = self.inp("c_TAB", [128, 2, 512])
    self.c_ICS = self.inp("c_ICS", [128, 2, 2, 128], BF16)
    self.c_feat_lat = self.inp("c_feat_lat", [33, NLAT])
    self.c_feat_ctx = self.inp("c_feat_ctx", [33, NCTX])
    self.c_win_lat = self.inp("c_win_lat", [4, 128, 128, 64])
    self.c_win_ctx = self.inp("c_win_ctx", [4, 4, 128, 64])
    self.c_F1c = self.inp("c_F1c", [4, 16], BF16)
    self.c_TTc = self.inp("c_TTc", [128, 2, 512])
    self.c_TABc = self.inp("c_TABc", [8, 2, 512])
    self.c_ICSc = self.inp("c_ICSc", [8, 2, 4], BF16)
    self.spec = [k.dram("spec0", [2, 4, 64, 128, 2, 512]),
                 k.dram("spec1", [2, 4, 2, 128, 2, 512])]


def swap_view(ap2d, half):
    a = ap2d
    st = a.ap[-1][0]
    return bass.AP(tensor=a.tensor, offset=a.offset + half * st, ap=[list(a.ap[0]), [-half * st, 2], [st, half]])


def swap_view_ri(ap2d):
    a = ap2d
    st = a.ap[-1][0]
    return bass.AP(tensor=a.tensor, offset=a.offset + 128 * st, ap=[list(a.ap[0]), [256 * st, 2], [-128 * st, 2], [st, 128]])


class HyCtx:
    pass


def hy_setup(self, l, nbanks=7, bankb=True, NT=6, NA=8, src=0, conv=True):
    k, nc = self.k, self.nc
    h = HyCtx()
    h.BD = k.sb("hy_BD", [128, 4, 128], BF16); k.dma("sp", h.BD[:], self.c_BD, writes=["hy_BD"])
    if src == 0:
        h.F1 = k.sb("hy_F1", [128, 512], BF16); k.dma("sp", h.F1[:], self.c_F1, writes=["hy_F1"])
        h.TT = k.sb("hy_TT", [128, 2, 512]); k.dma("sp", h.TT[:], self.c_TT, writes=["hy_TT"])
    else:
        h.F1c = k.sb("hy_F1c", [4, 16], BF16); k.dma("sp", h.F1c[:], self.c_F1c, writes=["hy_F1c"])
        h.TTc = k.sb("hy_TTc", [128, 2, 512]); k.dma("sp", h.TTc[:], self.c_TTc, writes=["hy_TTc"])
    if conv:
        h.R = k.sb("hy_R", [128, 2, 256], BF16); k.dma("sp", h.R[:], self.c_R, writes=["hy_R"])
        if src == 0:
            h.TAB = k.sb("hy_TAB", [128, 2, 512]); k.dma("sp", h.TAB[:], self.c_TAB, writes=["hy_TAB"])
            h.ICS = k.sb("hy_ICS", [128, 2, 2, 128], BF16); k.dma("sp", h.ICS[:], self.c_ICS, writes=["hy_ICS"])
        else:
            h.TABc = k.sb("hy_TABc", [8, 2, 512]); k.dma("sp", h.TABc[:], self.c_TABc, writes=["hy_TABc"])
            h.ICSc = k.sb("hy_ICSc", [8, 2, 4], BF16); k.dma("sp", h.ICSc[:], self.c_ICSc, writes=["hy_ICSc"])
    h.bank = [k.ps(f"hy_bank{i}", [128, 512]) for i in range(nbanks)]
    if bankb:
        h.bankb = k.ps("hy_bankb", [128, 1024], BF16)
    h.NT = NT
    h.NA = NA
    h.tt = [k.sb(f"hy_t{i}", [128, 512]) for i in range(h.NT)]
    h.uu = [k.sb(f"hy_u{i}", [128, 512]) for i in range(h.NT)]
    h.Ap = [k.sb(f"hy_Ap{i}", [128, 512], BF16) for i in range(h.NA)]
    h.cnt = 0
    h.acnt = 0
    return h


def hy_cmul(self, h, src_ps, src_key, tabA, tabB, tab_reads, out_ap, out_key, ri_layout=False, npart=128):
    k, nc = self.k, self.nc
    i = h.cnt % h.NT
    h.cnt += 1
    t, u = h.tt[i][:npart], h.uu[i][:npart]
    if ri_layout:
        sv = swap_view_ri(src_ps)
        k.op("dve", lambda: nc.vector.tensor_tensor(out=t[:], in0=src_ps, in1=tabA, op=ALU.mult), reads=tab_reads, writes=[src_key, f"hy_t{i}"])
        k.op("dve", lambda: nc.vector.tensor_tensor(out=u[:].rearrange("p (j r c) -> p j r c", j=2, r=2), in0=sv, in1=tabB.rearrange("p (j r c) -> p j r c", j=2, r=2), op=ALU.mult),
             reads=tab_reads, writes=[src_key, f"hy_u{i}"])
    else:
        sv = swap_view(src_ps, 256)
        k.op("dve", lambda: nc.vector.tensor_tensor(out=t[:], in0=src_ps, in1=tabA, op=ALU.mult), reads=tab_reads, writes=[src_key, f"hy_t{i}"])
        k.op("dve", lambda: nc.vector.tensor_tensor(out=u[:].rearrange("p (r c) -> p r c", r=2), in0=sv, in1=tabB.rearrange("p (r c) -> p r c", r=2), op=ALU.mult),
             reads=tab_reads, writes=[src_key, f"hy_u{i}"])
    k.op("pool", lambda: nc.gpsimd.tensor_tensor(out=out_ap, in0=t[:], in1=u[:], op=ALU.add), reads=[f"hy_t{i}", f"hy_u{i}"], writes=[out_key])


def hy_s12(self, h, lhsT_ap, lhsT_key, b1):
    k, nc = self.k, self.nc
    ps1 = h.bank[b1]
    k.op("pe", lambda: nc.tensor.matmul(ps1[:], lhsT=lhsT_ap, rhs=h.F1[:], start=True, stop=True), reads=[lhsT_key, "hy_F1"], writes=[f"hy_bank{b1}"])
    ai = h.acnt % h.NA
    h.acnt += 1
    hy_cmul(self, h, ps1[:], f"hy_bank{b1}", h.TT[:, 0, :], h.TT[:, 1, :], ["hy_TT"], h.Ap[ai][:], f"hy_Ap{ai}")
    return ai


def hy_s12_ctx(self, h, W2d, wkey, bt, b1):
    k, nc = self.k, self.nc
    ps1 = h.bank[b1]
    o4 = ps1[:].rearrange("p (r pr kb) -> p r pr kb", r=2, pr=32)
    f1 = h.F1c[:].rearrange("p (r kb) -> p r kb", r=2)
    for p in range(32):
        pr = bt * 32 + p
        k.op("pe", lambda: nc.tensor.matmul(o4[:, :, p, :], lhsT=W2d[0:4, pr * 128:(pr + 1) * 128], rhs=f1, start=True, stop=True),
             reads=[wkey(pr), "hy_F1c"], writes=[f"hy_bank{b1}"])
    ai = h.acnt % h.NA
    h.acnt += 1
    hy_cmul(self, h, ps1[:], f"hy_bank{b1}", h.TTc[:, 0, :], h.TTc[:, 1, :], ["hy_TTc"], h.Ap[ai][:], f"hy_Ap{ai}")
    return ai


def hy_s3(self, h, ai, b3):
    k, nc = self.k, self.nc
    ps3 = h.bank[b3]
    Ap = h.Ap[ai]
    ak = f"hy_Ap{ai}"
    k.op("pe", lambda: nc.tensor.matmul(ps3[:, 0:256], lhsT=h.BD[:, 0, :], rhs=Ap[:, 0:256], start=True, stop=False), reads=[ak, "hy_BD"], writes=[f"hy_bank{b3}"])
    k.op("pe", lambda: nc.tensor.matmul(ps3[:, 0:256], lhsT=h.BD[:, 1, :], rhs=Ap[:, 256:512], start=False, stop=True), reads=[ak, "hy_BD"], writes=[f"hy_bank{b3}"])
    k.op("pe", lambda: nc.tensor.matmul(ps3[:, 256:512], lhsT=h.BD[:, 0, :], rhs=Ap[:, 256:512], start=True, stop=False), reads=[ak, "hy_BD"], writes=[f"hy_bank{b3}"])
    k.op("pe", lambda: nc.tensor.matmul(ps3[:, 256:512], lhsT=h.BD[:, 2, :], rhs=Ap[:, 0:256], start=False, stop=True), reads=[ak, "hy_BD"], writes=[f"hy_bank{b3}"])


def hy_s3_fb(self, h, af, ab, b3):
    k, nc = self.k, self.nc
    ps3 = h.bank[b3]
    Af, Ab = h.Ap[af], h.Ap[ab]
    rd = [f"hy_Ap{af}", f"hy_Ap{ab}", "hy_BD"]
    wk = [f"hy_bank{b3}"]
    k.op("pe", lambda: nc.tensor.matmul(ps3[:, 0:256], lhsT=h.BD[:, 0, :], rhs=Af[:, 0:256], start=True, stop=False), reads=rd, writes=wk)
    k.op("pe", lambda: nc.tensor.matmul(ps3[:, 0:256], lhsT=h.BD[:, 1, :], rhs=Af[:, 256:512], start=False, stop=False), reads=rd, writes=wk)
    k.op("pe", lambda: nc.tensor.matmul(ps3[:, 0:256], lhsT=h.BD[:, 0, :], rhs=Ab[:, 0:256], start=False, stop=False), reads=rd, writes=wk)
    k.op("pe", lambda: nc.tensor.matmul(ps3[:, 0:256], lhsT=h.BD[:, 1, :], rhs=Ab[:, 256:512], start=False, stop=True), reads=rd, writes=wk)
    k.op("pe", lambda: nc.tensor.matmul(ps3[:, 256:512], lhsT=h.BD[:, 0, :], rhs=Af[:, 256:512], start=True, stop=False), reads=rd, writes=wk)
    k.op("pe", lambda: nc.tensor.matmul(ps3[:, 256:512], lhsT=h.BD[:, 2, :], rhs=Af[:, 0:256], start=False, stop=False), reads=rd, writes=wk)
    k.op("pe", lambda: nc.tensor.matmul(ps3[:, 256:512], lhsT=h.BD[:, 3, :], rhs=Ab[:, 256:512], start=False, stop=False), reads=rd, writes=wk)
    k.op("pe", lambda: nc.tensor.matmul(ps3[:, 256:512], lhsT=h.BD[:, 1, :], rhs=Ab[:, 0:256], start=False, stop=True), reads=rd, writes=wk)


def hy_gtab(self, h, b3, g, gk):
    k, nc = self.k, self.nc
    ps3 = h.bank[b3]
    bk = f"hy_bank{b3}"
    k.op("act", lambda: nc.scalar.copy(out=g[:, 0, :].rearrange("p (r c) -> p r c", r=2), in_=bc(ps3[:, 0:256].unsqueeze(1), [128, 2, 256])), writes=[bk, gk])
    k.op("act", lambda: nc.scalar.mul(out=g[:, 1, 0:256], in_=ps3[:, 256:512], mul=-1.0), writes=[bk, gk])
    k.op("act", lambda: nc.scalar.copy(out=g[:, 1, 256:512], in_=ps3[:, 256:512]), writes=[bk, gk])


def stage_hyena_filters(self, l, src):
    k, nc = self.k, self.nc
    n = NLAT if src == 0 else NCTX
    NP = n // 64
    k.begin_phase()
    h = hy_setup(self, l, nbanks=8, bankb=False, NT=4, NA=6, src=src, conv=False)
    w3b = k.sb("hf_w3b", [64, 2048], BF16)
    h2b = k.sb("hf_h2b", [64, n], BF16)
    k.begin_phase()
    w1 = k.sb("hf_w1", [33, 64]); w2 = k.sb("hf_w2", [64, 64]); w3 = k.sb("hf_w3", [64, 2048])
    pv = k.sb("hf_pv", [64, 3, DEPTH])
    k.dma("sp", w1[:], self.hy_w1[:, l], writes=["hf_w1"]); k.dma("sp", w2[:], self.hy_w2[:, l], writes=["hf_w2"]); k.dma("sp", w3[:], self.hy_w3[:, l], writes=["hf_w3"])
    k.dma("sp", pv[:, 0, :], self.hy_b1, writes=["hf_pv"]); k.dma("sp", pv[:, 1, :], self.hy_b2, writes=["hf_pv"]); k.dma("sp", pv[:, 2, :], self.hy_fr, writes=["hf_pv"])
    k.op("pool", lambda: nc.gpsimd.tensor_copy(out=w3b[:], in_=w3[:]), reads=["hf_w3"], writes=["hf_w3b"])
    sc = k.sb("hf_sc", [64, 3])
    k.op("dve", lambda: nc.vector.tensor_scalar(out=sc[:, 0:1], in0=pv[:, 2, l:l + 1], scalar1=1.0 / (2 * math.pi), scalar2=None, op0=ALU.mult), reads=["hf_pv"], writes=["hf_sc"])
    for i in range(2):
        k.op("dve", lambda: nc.vector.tensor_tensor(out=sc[:, 1 + i:2 + i], in0=pv[:, i, l:l + 1], in1=sc[:, 0:1], op=ALU.mult), reads=["hf_pv", "hf_sc"], writes=["hf_sc"])
    ft = [k.sb(f"hf_ft{i}", [33, 512]) for i in range(2)]
    yy = [k.sb(f"hf_y{i}", [64, 512]) for i in range(2)]
    rr = [k.sb(f"hf_r{i}", [64, 512]) for i in range(2)]
    h1 = [k.sb(f"hf_h1{i}", [64, 512]) for i in range(2)]
    feat = self.c_feat_lat if src == 0 else self.c_feat_ctx
    TW = min(512, n)
    for ti in range(n // TW):
        b = ti % 2
        s0 = ti * TW
        k.dma("sp", ft[b][:, :TW], feat[:, s0:s0 + TW], writes=[f"hf_ft{b}"])
        cur_in, cur_key, wmat, wkey = ft[b][:, :TW], f"hf_ft{b}", w1, "hf_w1"
        for layer in range(2):
            pb = h.bank[layer]
            k.op("pe", lambda: nc.tensor.matmul(pb[:64, :TW], lhsT=wmat[:], rhs=cur_in, start=True, stop=True), reads=[cur_key, wkey], writes=[f"hy_bank{layer}"])
            k.op("act", lambda: nc.scalar.activation(out=yy[b][:, :TW], in_=pb[:64, :TW], func=AF.Identity, scale=sc[:, 0:1], bias=sc[:, 1 + layer:2 + layer]),
                 reads=["hf_sc"], writes=[f"hy_bank{layer}", f"hf_y{b}"])
            k.op("dve", lambda: nc.vector.tensor_scalar(out=rr[b][:, :TW], in0=yy[b][:, :TW], scalar1=MAGIC, scalar2=None, op0=ALU.add), reads=[f"hf_y{b}"], writes=[f"hf_r{b}"])
            k.op("dve", lambda: nc.vector.tensor_scalar(out=rr[b][:, :TW], in0=rr[b][:, :TW], scalar1=MAGIC, scalar2=None, op0=ALU.subtract), reads=[f"hf_r{b}"], writes=[f"hf_r{b}"])
            k.op("dve", lambda: nc.vector.tensor_tensor(out=yy[b][:, :TW], in0=yy[b][:, :TW], in1=rr[b][:, :TW], op=ALU.subtract), reads=[f"hf_y{b}", f"hf_r{b}"], writes=[f"hf_y{b}"])
            if layer == 0:
                k.op("act", lambda: nc.scalar.activation(out=h1[b][:, :TW], in_=yy[b][:, :TW], func=AF.Sin, scale=TWO_PI_LO), reads=[f"hf_y{b}"], writes=[f"hf_h1{b}"])
                cur_in, cur_key, wmat, wkey = h1[b][:, :TW], f"hf_h1{b}", w2, "hf_w2"
            else:
                k.op("act", lambda: nc.scalar.activation(out=h2b[:, s0:s0 + TW], in_=yy[b][:, :TW], func=AF.Sin, scale=TWO_PI_LO), reads=[f"hf_y{b}"], writes=["hf_h2b"])
    k.end_phase()
    Wh = [k.sb(f"hf_Wh{i}", [128, 128, 64]) for i in range(2)]
    Whb = [k.sb(f"hf_Whb{i}", [128, 128 * 64], BF16) for i in range(2)]
    win = k.sb("hf_win", [128, 128, 64])
    asum = k.sb("hf_asum", [128, 2, 128]); rn = k.sb("hf_rn", [128, 128])
    sb3 = [k.sb(f"hf_sb3{i}", [128, 512]) for i in range(2)]
    gab = [k.sb(f"hf_gab{i}", [128, 2, 512]) for i in range(3)]
    onesf = k.sb("hf_ones", [128, 128]); k.op("pool", lambda: nc.gpsimd.memset(onesf[:], 1.0), writes=["hf_ones"])
    if NP < 128:
        for i in range(2):
            k.op("pool", lambda: nc.gpsimd.memset(Whb[i][:], 0.0), writes=[f"hf_Whb{i}"])
            k.op("pool", lambda: nc.gpsimd.memset(Wh[i][:], 0.0), writes=[f"hf_Wh{i}"])
    gcount = 0
    for cc in range(4):
        if src == 0:
            k.dma("sp", win[:], self.c_win_lat[cc], writes=["hf_win"])
        else:
            k.dma("sp", win[:NP], self.c_win_ctx[cc], writes=["hf_win"])
        for o in range(2):
            for dd in range(2):
                col0 = (o * 2 + dd) * 512 + cc * 128
                for q0 in range(0, 64, 4):
                    pb = h.bank[(q0 // 4) % 2]
                    pk = f"hy_bank{(q0 // 4) % 2}"
                    for qi in range(4):
                        q = q0 + qi
                        k.op("pe", lambda: nc.tensor.matmul(pb[:NP, qi * 128:(qi + 1) * 128], lhsT=h2b[:, q:q + (NP - 1) * 64 + 1:64], rhs=w3b[:, col0:col0 + 128], start=True, stop=True),
                             reads=["hf_h2b", "hf_w3b"], writes=[pk])
                    k.op("dve", lambda: nc.vector.tensor_tensor(out=Wh[dd][:NP].rearrange("p c q -> p q c")[:, q0:q0 + 4, :], in0=pb[:NP, :].rearrange("p (q c) -> p q c", q=4),
                                                                in1=win[:NP].rearrange("p c q -> p q c")[:, q0:q0 + 4, :], op=ALU.mult),
                         reads=["hf_win"], writes=[pk, f"hf_Wh{dd}"])
                k.op("dve", lambda: nc.vector.tensor_reduce(out=asum[:, dd, :], in_=Wh[dd][:], axis=AX.X, op=ALU.add, apply_absolute_value=True),
                     reads=[f"hf_Wh{dd}"], writes=["hf_asum"])
            k.op("dve", lambda: nc.vector.tensor_tensor(out=asum[:, 0, :], in0=asum[:, 0, :], in1=asum[:, 1, :], op=ALU.add), reads=["hf_asum"], writes=["hf_asum"])
            pb = h.bank[2]
            k.op("pe", lambda: nc.tensor.matmul(pb[:, 0:128], lhsT=onesf[:], rhs=asum[:, 0, :], start=True, stop=True), reads=["hf_ones", "hf_asum"], writes=["hy_bank2"])
            k.op("dve", lambda: nc.vector.tensor_scalar(out=rn[:], in0=pb[:, 0:128], scalar1=1e-6, scalar2=None, op0=ALU.add), writes=["hy_bank2", "hf_rn"])
            k.op("dve", lambda: nc.vector.reciprocal(out=rn[:], in_=rn[:]), reads=["hf_rn"], writes=["hf_rn"])
            for dd in range(2):
                k.op("dve", lambda: nc.vector.tensor_tensor(out=Whb[dd][:NP].rearrange("p (c q) -> p c q", q=64), in0=Wh[dd][:NP], in1=bc(rn[:NP].unsqueeze(2), [NP, 128, 64]), op=ALU.mult),
                     reads=[f"hf_Wh{dd}", "hf_rn"], writes=[f"hf_Whb{dd}"])
            if src == 1:
                for bt in range(2):
                    a0 = hy_s12_ctx(self, h, Whb[0], lambda pr: "hf_Whb0", bt, 0)
                    a1 = hy_s12_ctx(self, h, Whb[1], lambda pr: "hf_Whb1", bt, 1)
                    hy_s3_fb(self, h, a0, a1, 4)
                    g = gab[gcount % 3]
                    gk = f"hf_gab{gcount % 3}"
                    gcount += 1
                    hy_gtab(self, h, 4, g, gk)
                    k.dma("sp", self.spec[1][o, cc, bt], g[:], reads=[gk], writes=["spec1"])
                continue
            SK = 2
            ais = {}
            for it in range(64 + 2 * SK):
                if it < 64:
                    pr = it
                    ais[(pr, 0)] = hy_s12(self, h, Whb[0][:, pr * 128:(pr + 1) * 128], "hf_Whb0", pr % 2)
                    ais[(pr, 1)] = hy_s12(self, h, Whb[1][:, pr * 128:(pr + 1) * 128], "hf_Whb1", 2 + pr % 2)
                pr = it - SK
                if 0 <= pr < 64:
                    b3 = 4 + pr % 4
                    hy_s3_fb(self, h, ais.pop((pr, 0)), ais.pop((pr, 1)), b3)
                    g = gab[gcount % 3]
                    gk = f"hf_gab{gcount % 3}"
                    gcount += 1
                    hy_gtab(self, h, b3, g, gk)
                    k.dma("sp", self.spec[src][o, cc, pr], g[:], reads=[gk], writes=[f"spec{src}"])
    k.end_phase()


def stage_hyena_conv(self, l, src):
    k, nc = self.k, self.nc
    n = NLAT if src == 0 else NCTX
    NP = n // 64
    t0 = NCTX if src == 0 else 0
    k.begin_phase()
    h = hy_setup(self, l, NT=5, NA=4, src=src, conv=True)
    cw = k.sb("hc_cw", [128, 12, 3]); cb = k.sb("hc_cb", [128, 12]); skb = k.sb("hc_skb", [128, 2, 512])
    k.dma("sp", cw[:], self.hy_cwT[:, l], writes=["hc_cw"]); k.dma("sp", cb[:], self.hy_cbT[:, l], writes=["hc_cb"]); k.dma("sp", skb[:], self.hy_skipb[:, l], writes=["hc_skb"])
    fa = k.sb("hc_fa", [128, n]); fb = k.sb("hc_fb", [128, n])
    Wt = {nm: k.sb(f"hc_W{nm}", [128, 128 * 64], BF16) for nm in ("v", "x1", "x2")}
    yfm = k.sb("hc_yfm", [128, n], BF16)
    NYH, NCP, NGAB = 4, 2, 4
    Yh = [k.sb(f"hc_Yh{i}", [128, 512], BF16) for i in range(NYH)]
    Cp = [k.sb(f"hc_Cp{i}", [128, 4, 512], BF16) for i in range(NCP)]
    gab = [k.sb(f"hc_gab{i}", [128, 2, 512]) for i in range(NGAB)]
    WVK = [f"hc_Wv_g{g}" for g in range(16)]
    ea = [k.sb(f"hc_ea{i}", [128, 512]) for i in range(2)]
    eb = [k.sb(f"hc_eb{i}", [128, 512]) for i in range(2)]
    if NP < 128:
        for nm in Wt:
            k.op("pool", lambda: nc.gpsimd.memset(Wt[nm][:], 0.0), writes=(WVK if nm == "v" else [f"hc_W{nm}"]))
    gcount = 0
    for cc in range(4):
        for ui, nm in enumerate(("v", "x1", "x2")):
            ch = ui * 4 + cc
            k.dma("sp", fa[:], self.p_hy[ch, :, t0:t0 + n], reads=["p_hy"], writes=["hc_fa"])
            k.op("dve", lambda: nc.vector.tensor_scalar(out=fb[:], in0=fa[:], scalar1=cw[:, ch, 1:2], scalar2=cb[:, ch:ch + 1], op0=ALU.mult, op1=ALU.add),
                 reads=["hc_fa", "hc_cw", "hc_cb"], writes=["hc_fb"])
            k.op("dve", lambda: nc.vector.scalar_tensor_tensor(out=fb[:, 1:n], in0=fa[:, 0:n - 1], scalar=cw[:, ch, 0:1], in1=fb[:, 1:n], op0=ALU.mult, op1=ALU.add),
                 reads=["hc_fa", "hc_cw", "hc_fb"], writes=["hc_fb"])
            k.op("dve", lambda: nc.vector.scalar_tensor_tensor(out=fb[:, 0:n - 1], in0=fa[:, 1:n], scalar=cw[:, ch, 2:3], in1=fb[:, 0:n - 1], op0=ALU.mult, op1=ALU.add),
                 reads=["hc_fa", "hc_cw", "hc_fb"], writes=["hc_fb"])
            Wd = Wt[nm][:].rearrange("p (c q) -> p q c", q=64)
            for q0 in range(0, 64, 4):
                pb = h.bank[(q0 // 4) % 2]
                pk = f"hy_bank{(q0 // 4) % 2}"
                for qi in range(4):
                    q = q0 + qi
                    k.op("pe", lambda: nc.tensor.transpose(out=pb[:NP, qi * 128:(qi + 1) * 128], in_=fb[:, q:q + (NP - 1) * 64 + 1:64], identity=self.ident[:]),
                         reads=["hc_fb", "ident"], writes=[pk])
                k.op("act", lambda: nc.scalar.copy(out=Wd[:NP, q0:q0 + 4, :], in_=pb[:NP, :].rearrange("p (q c) -> p q c", q=4)), writes=[pk] + (WVK if nm == "v" else [f"hc_W{nm}"]))
        SK = 2
        for o in range(2):
            sig, gate, outn = (("v", "x1", "v") if o == 0 else ("v", "x2", "v"))
            Wsig, Wg, Wo = Wt[sig], Wt[gate], Wt[outn]
            if src == 1:
                for bt in range(2):
                    gi = gcount % NGAB
                    gcount += 1
                    k.dma("sp", gab[gi][:], self.spec[1][o, cc, bt], reads=["spec1"], writes=[f"hc_gab{gi}"])
                    ai = hy_s12_ctx(self, h, Wsig, lambda pr: f"hc_Wv_g{pr // 4}", bt, 0)
                    hy_s3(self, h, ai, 2)
                    yh = Yh[bt % NYH]
                    yk = f"hc_Yh{bt % NYH}"
                    hy_cmul(self, h, h.bank[2][:], "hy_bank2", gab[gi][:, 0, :], gab[gi][:, 1, :], [f"hc_gab{gi}"], yh[:], yk)
                    for duo in range(16):
                        b5 = 4 + duo % 2
                        ps5 = h.bank[b5]
                        for s2 in range(2):
                            p = duo * 2 + s2
                            k.op("pe", lambda: nc.tensor.matmul(ps5[0:8, s2 * 256:(s2 + 1) * 256], lhsT=yh[:, p * 8:p * 8 + 8], rhs=h.R[:, 0, :], start=True, stop=False),
                                 reads=[yk, "hy_R"], writes=[f"hy_bank{b5}"])
                            k.op("pe", lambda: nc.tensor.matmul(ps5[0:8, s2 * 256:(s2 + 1) * 256], lhsT=yh[:, 256 + p * 8:256 + p * 8 + 8], rhs=h.R[:, 1, :], start=False, stop=True),
                                 reads=[yk, "hy_R"], writes=[f"hy_bank{b5}"])
                        grp = bt * 8 + duo // 2
                        cpi = grp % NCP
                        hy_cmul(self, h, ps5[0:8, :], f"hy_bank{b5}", h.TABc[:, 0, :], h.TABc[:, 1, :], ["hy_TABc"], Cp[cpi][0:8, duo % 2, :], f"hc_Cp{cpi}", ri_layout=True, npart=8)
                        if duo % 2 == 1:
                            ps6 = h.bank[6]
                            c5 = Cp[cpi][0:8, 0:2, :].rearrange("p d (s r c) -> p (d s) r c", s=2, r=2)
                            for ri in range(2):
                                k.op("pe", lambda: nc.tensor.matmul(ps6[:NP, :].rearrange("p (g c) -> p g c", g=4), lhsT=h.ICSc[:, ri, :], rhs=c5[:, :, ri, :], start=(ri == 0), stop=(ri == 1)),
                                     reads=[f"hc_Cp{cpi}", "hy_ICSc"], writes=["hy_bank6"])
                            c0 = grp * 8 * 64
                            ei = grp % 2
                            wk = f"hc_Wv_g{grp}"
                            skv = bc(skb[:NP, o, cc * 128 + grp * 8:cc * 128 + grp * 8 + 8].unsqueeze(2), [NP, 8, 64])
                            k.op("pool", lambda: nc.gpsimd.tensor_tensor(out=ea[ei][:NP].rearrange("p (c q) -> p c q", q=64), in0=Wsig[:NP, c0:c0 + 512].rearrange("p (c q) -> p c q", q=64), in1=skv, op=ALU.mult),
                                 reads=[wk, "hc_skb"], writes=[f"hc_ea{ei}"])
                            k.op("dve", lambda: nc.vector.tensor_tensor(out=eb[ei][:NP], in0=ps6[:NP, :], in1=ea[ei][:NP], op=ALU.add), reads=[f"hc_ea{ei}"], writes=["hy_bank6", f"hc_eb{ei}"])
                            k.op("pool", lambda: nc.gpsimd.tensor_tensor(out=Wo[:NP, c0:c0 + 512], in0=eb[ei][:NP], in1=Wg[:NP, c0:c0 + 512], op=ALU.mult),
                                 reads=[f"hc_eb{ei}", f"hc_W{gate}"], writes=[wk])
                continue
            st = {}
            for it in range(64 + 3 * SK):
                if it < 64:
                    pr = it
                    gi = gcount % NGAB
                    gcount += 1
                    k.dma("sp", gab[gi][:], self.spec[src][o, cc, pr], reads=[f"spec{src}"], writes=[f"hc_gab{gi}"])
                    ai = hy_s12(self, h, Wsig[:, pr * 128:(pr + 1) * 128], f"hc_Wv_g{pr // 4}", pr % 2)
                    st[pr] = [gi, ai, None]
                pr = it - SK
                if 0 <= pr < 64:
                    gi, ai, _ = st[pr]
                    b3 = 2 + pr % 2
                    hy_s3(self, h, ai, b3)
                    yi = pr % NYH
                    hy_cmul(self, h, h.bank[b3][:], f"hy_bank{b3}", gab[gi][:, 0, :], gab[gi][:, 1, :], [f"hc_gab{gi}"], Yh[yi][:], f"hc_Yh{yi}")
                    st[pr][2] = yi
                pr = it - 2 * SK
                if 0 <= pr < 64:
                    yi = st[pr][2]
                    yh = Yh[yi]
                    b5 = 4 + pr % 2
                    ps5 = h.bank[b5]
                    for j in range(2):
                        k.op("pe", lambda: nc.tensor.matmul(ps5[:, j * 256:(j + 1) * 256], lhsT=yh[:, j * 128:(j + 1) * 128], rhs=h.R[:, 0, :], start=True, stop=False),
                             reads=[f"hc_Yh{yi}", "hy_R"], writes=[f"hy_bank{b5}"])
                        k.op("pe", lambda: nc.tensor.matmul(ps5[:, j * 256:(j + 1) * 256], lhsT=yh[:, 256 + j * 128:256 + (j + 1) * 128], rhs=h.R[:, 1, :], start=False, stop=True),
                             reads=[f"hc_Yh{yi}", "hy_R"], writes=[f"hy_bank{b5}"])
                    grp = pr // 4
                    cpi = grp % NCP
                    hy_cmul(self, h, ps5[:], f"hy_bank{b5}", h.TAB[:, 0, :], h.TAB[:, 1, :], ["hy_TAB"], Cp[cpi][:, pr % 4, :], f"hc_Cp{cpi}", ri_layout=True)
                    del st[pr]
                pr = it - 3 * SK
                if 0 <= pr < 64 and pr % 4 == 3:
                    grp = pr // 4
                    cpi = grp % NCP
                    ps6 = h.bank[6]
                    c5 = Cp[cpi][:].rearrange("p g (j r c) -> p g j r c", j=2, r=2)
                    n_mm = 0
                    for j in range(2):
                        for ri in range(2):
                            k.op("pe", lambda: nc.tensor.matmul(ps6[:NP, :].rearrange("p (g c) -> p g c", g=4), lhsT=h.ICS[:, ri, j, 0:NP], rhs=c5[:, :, j, ri, :], start=(n_mm == 0), stop=(n_mm == 3)),
                                 reads=[f"hc_Cp{cpi}", "hy_ICS"], writes=["hy_bank6"])
                            n_mm += 1
                    c0 = grp * 8 * 64
                    ei = grp % 2
                    wk = f"hc_Wv_g{grp}"
                    skv = bc(skb[:NP, o, cc * 128 + grp * 8:cc * 128 + grp * 8 + 8].unsqueeze(2), [NP, 8, 64])
                    k.op("pool", lambda: nc.gpsimd.tensor_tensor(out=ea[ei][:NP].rearrange("p (c q) -> p c q", q=64), in0=Wsig[:NP, c0:c0 + 512].rearrange("p (c q) -> p c q", q=64), in1=skv, op=ALU.mult),
                         reads=[wk, "hc_skb"], writes=[f"hc_ea{ei}"])
                    k.op("dve", lambda: nc.vector.tensor_tensor(out=eb[ei][:NP], in0=ps6[:NP, :], in1=ea[ei][:NP], op=ALU.add), reads=[f"hc_ea{ei}"], writes=["hy_bank6", f"hc_eb{ei}"])
                    k.op("pool", lambda: nc.gpsimd.tensor_tensor(out=Wo[:NP, c0:c0 + 512], in0=eb[ei][:NP], in1=Wg[:NP, c0:c0 + 512], op=ALU.mult),
                         reads=[f"hc_eb{ei}", f"hc_W{gate}"], writes=[wk])
        Wy3 = Wt["v"][:].rearrange("p (c q) -> p c q", q=64)
        for q0 in range(0, 64, 8):
            for qi in range(8):
                q = q0 + qi
                k.op("pe", lambda: nc.tensor.transpose(out=h.bankb[:, qi * 128:qi * 128 + NP], in_=Wy3[:NP, :, q], identity=self.ident_bf[:NP, :NP]),
                     reads=WVK + ["ident_bf"], writes=["hy_bankb"])
            ov = bass.AP(tensor=yfm[:].tensor, offset=yfm[:].offset + q0, ap=[list(yfm[:].ap[0]), [1, 8], [64, NP]])
            k.op("act", lambda: nc.scalar.copy(out=ov, in_=h.bankb[:].rearrange("p (q c) -> p q c", q=8)[:, :, 0:NP]), writes=["hy_bankb", "hc_yfm"])
        k.dma("sp", self.y_hy[cc, :, t0:t0 + n], yfm[:], reads=["hc_yfm"], writes=["y_hy"])
    k.end_phase()


Prog.declare_hyena = declare_hyena
Prog.stage_hyena_filters = stage_hyena_filters
Prog.stage_hyena_conv = stage_hyena_conv


def prep_hyena(inp):
    sh = {}
    L = DEPTH
    cw = np.asarray(inp["hy_conv_w"], np.float32)
    sh["hy_cwT"] = np.ascontiguousarray(cw.reshape(L, 3, 12, 128).transpose(3, 0, 2, 1))
    sh["hy_cbT"] = chunkT(inp["hy_conv_b"])
    sh["hy_w1"] = np.ascontiguousarray(np.asarray(inp["hy_w1"], np.float32).transpose(1, 0, 2))
    sh["hy_w2"] = np.ascontiguousarray(np.asarray(inp["hy_w2"], np.float32).transpose(1, 0, 2))
    sh["hy_w3"] = np.ascontiguousarray(np.asarray(inp["hy_w3"], np.float32).transpose(1, 0, 2))
    sh["hy_b1T"] = np.ascontiguousarray(np.asarray(inp["hy_b1"], np.float32).T)
    sh["hy_b2T"] = np.ascontiguousarray(np.asarray(inp["hy_b2"], np.float32).T)
    sh["hy_frT"] = np.ascontiguousarray(np.asarray(inp["hy_freq"], np.float32).T)
    sk = np.asarray(inp["hy_skip"], np.float32)
    sh["hy_skipb"] = np.ascontiguousarray(np.broadcast_to(sk[None], (128, L, 2, 512)))
    bf = ml_dtypes.bfloat16
    P_ = np.arange(128, dtype=np.float64)[:, None]
    kb = np.arange(256, dtype=np.float64)[None, :]
    a = 2 * np.pi * P_ * kb / 256.0
    sh["c_F1"] = np.concatenate([np.cos(a), -np.sin(a)], 1).astype(bf)
    q = (np.arange(128) % 64).astype(np.float64)[:, None]
    a = 2 * np.pi * q * kb / 16384.0
    sh["c_TT"] = np.stack([np.concatenate([np.cos(a), np.cos(a)], 1), np.concatenate([np.sin(a), -np.sin(a)], 1)], 1).astype(np.float32)
    qq = np.arange(64, dtype=np.float64)
    a = 2 * np.pi * np.outer(qq, qq) / 64.0
    def bd(m):
        z = np.zeros((128, 128)); z[:64, :64] = m; z[64:, 64:] = m
        return z
    BDC, BDS = bd(np.cos(a)), bd(np.sin(a))
    sh["c_BD"] = np.stack([BDC, BDS, -BDS, -BDC], 1).astype(bf)
    sh["c_R"] = np.stack([np.concatenate([BDC, BDS], 1), np.concatenate([-BDS, BDC], 1)], 1).astype(bf)
    p_ = np.arange(128, dtype=np.float64)
    TA = np.zeros((128, 2, 2, 2, 64)); TB = np.zeros((128, 2, 2, 2, 64))
    for j in range(2):
        ang = 2 * np.pi * np.outer(j * 128 + p_, qq) / 16384.0
        TA[:, j, :, :, :] = np.cos(ang)[:, None, None, :]
        TB[:, j, 0, :, :] = -np.sin(ang)[:, None, :]
        TB[:, j, 1, :, :] = np.sin(ang)[:, None, :]
    sh["c_TAB"] = np.stack([TA.reshape(128, 512), TB.reshape(128, 512)], 1).astype(np.float32)
    ICS = np.zeros((128, 2, 2, 128))
    Pn = np.arange(128, dtype=np.float64)
    for j in range(2):
        ang = 2 * np.pi * np.outer(j * 128 + p_, Pn) / 256.0
        ICS[:, 0, j, :] = np.cos(ang) / 16384.0
        ICS[:, 1, j, :] = -np.sin(ang) / 16384.0
    sh["c_ICS"] = ICS.astype(bf)
    P4 = np.arange(4, dtype=np.float64)[:, None]; m8 = np.arange(8, dtype=np.float64)[None, :]
    a = 2 * np.pi * P4 * m8 / 8.0
    sh["c_F1c"] = np.concatenate([np.cos(a), -np.sin(a)], 1).astype(bf)
    a = 2 * np.pi * q * m8 / 512.0
    ct = np.tile(np.cos(a)[:, None, :], (1, 32, 1)).reshape(128, 256); st_ = np.tile(np.sin(a)[:, None, :], (1, 32, 1)).reshape(128, 256)
    sh["c_TTc"] = np.stack([np.concatenate([ct, ct], 1), np.concatenate([st_, -st_], 1)], 1).astype(np.float32)
    TAc = np.zeros((8, 2, 2, 2, 64)); TBc = np.zeros((8, 2, 2, 2, 64))
    ang = 2 * np.pi * np.outer(np.arange(8, dtype=np.float64), qq) / 512.0
    TAc[:] = np.cos(ang)[:, None, None, None, :]
    TBc[:, :, 0, :, :] = -np.sin(ang)[:, None, None, :]
    TBc[:, :, 1, :, :] = np.sin(ang)[:, None, None, :]
    sh["c_TABc"] = np.stack([TAc.reshape(8, 512), TBc.reshape(8, 512)], 1).astype(np.float32)
    ang = 2 * np.pi * np.outer(np.arange(8, dtype=np.float64), np.arange(4, dtype=np.float64)) / 8.0
    sh["c_ICSc"] = np.stack([np.cos(ang) / 512.0, -np.sin(ang) / 512.0], 1).astype(bf)
    bands = np.linspace(1e-4, 15, 16)
    deltas = np.abs(np.linspace(math.log(1e-2) / 0.3, math.log(1e-2) / 1.5, 512))
    for nm, n in (("lat", NLAT), ("ctx", NCTX)):
        idx = np.arange(n, dtype=np.float64)
        tn = idx / (n - 1)
        ang = (2 * np.pi / n) * idx[:, None] * bands[None, :]
        feats = np.concatenate([tn[:, None], np.cos(ang), -np.sin(ang)], -1)
        sh["c_feat_" + nm] = np.ascontiguousarray(feats.T).astype(np.float32)
        win = np.exp(-tn[:, None] * deltas[None, :])
        NPn = n // 64
        w = win.reshape(NPn, 64, 4, 128).transpose(2, 0, 3, 1)
        sh["c_win_" + nm] = np.ascontiguousarray(w).astype(np.float32)
    return sh


def stage_final(self):
    k, nc = self.k, self.nc
    self.out = self.outp("out", [8, 128, NLAT])
    k.begin_phase()
    NB = 2
    xt = [k.sb(f"f_xt{i}", [128, 8, 512]) for i in range(NB)]
    sq = [k.sb(f"f_sq{i}", [128, 8, 512], BF16) for i in range(NB)]
    ss = [k.ps(f"f_ss{i}", [128, 512]) for i in range(NB)]
    rstd = [k.sb(f"f_rstd{i}", [128, 512]) for i in range(NB)]
    ot = [k.sb(f"f_o{i}", [128, 8, 512]) for i in range(NB)]
    xv = self.xres.rearrange("k p t -> p k t")
    ov = self.out.rearrange("k p t -> p k t")
    for ti, (s0, W) in enumerate(TILES[1:]):
        b = ti % NB
        k.dma("sp", xt[b][:], xv[:, :, s0:s0 + W], reads=["xres"], writes=[f"f_xt{b}"])
        k.op("act", lambda: nc.scalar.activation(out=sq[b][:], in_=xt[b][:], func=AF.Square), reads=[f"f_xt{b}"], writes=[f"f_sq{b}"])
        for kk in range(8):
            k.op("pe", lambda: nc.tensor.matmul(ss[b][:], lhsT=self.ones_bf[:], rhs=sq[b][:, kk, :], start=(kk == 0), stop=(kk == 7)),
                 reads=[f"f_sq{b}", "ones_bf"], writes=[f"f_ss{b}"])
        k.op("act", lambda: nc.scalar.activation(out=rstd[b][:], in_=ss[b][:], func=AF.Sqrt, scale=1.0 / D, bias=self.epsc[:, 0:1]),
             reads=["epsc"], writes=[f"f_ss{b}", f"f_rstd{b}"])
        k.op("dve", lambda: nc.vector.reciprocal(out=rstd[b][:], in_=rstd[b][:]), reads=[f"f_rstd{b}"], writes=[f"f_rstd{b}"])
        for kk in range(8):
            k.op("dve", lambda: nc.vector.scalar_tensor_tensor(out=ot[b][:, kk, :], in0=xt[b][:, kk, :], scalar=self.gfin[:, kk:kk + 1], in1=rstd[b][:], op0=ALU.mult, op1=ALU.mult),
                 reads=[f"f_xt{b}", "gfin", f"f_rstd{b}"], writes=[f"f_o{b}"])
        k.dma("pool", ov[:, :, s0 - NCTX:s0 - NCTX + W], ot[b][:], reads=[f"f_o{b}"], writes=["out"])
    k.end_phase()


Prog.stage_final = stage_final


def build_full(layers=DEPTH):
    P = Prog(layers=layers)
    P.declare_rg(); P.declare_gla(); P.declare_merge(); P.declare_hyena(); P.declare_peer()
    for l in range(layers):
        need_ctx = l < DEPTH - 1
        xsrc = P.xT if l == 0 else P.xres
        tiles = TILES if need_ctx else TILES[1:]
        P.stage0(l)
        P.stage_norm(l, 1, xsrc)
        P.stage_inproj(l)
        P.stage_rglru(l)
        P.stage_gla(l)
        P.stage_hyena_filters(l, 0)
        P.stage_hyena_conv(l, 0)
        if need_ctx:
            P.stage_hyena_filters(l, 1)
            P.stage_hyena_conv(l, 1)
        P.stage_merge(l, xsrc, tiles)
        P.stage_norm(l, 2, P.xres, tiles)
        P.stage_peer_prep(l)
        P.stage_peer(l, list(range(0 if need_ctx else NCTX, T, 128)))
    P.stage_final()
    P.k.finish()
    return P


def prep_all_shared(inp):
    sh = prep_shared(inp)
    sh.update(prep_rg(inp)); sh.update(prep_gla(inp)); sh.update(prep_merge(inp)); sh.update(prep_hyena(inp)); sh.update(prep_peer(inp))
    return sh


N_CORES = 4


def kernel(**inputs):
    inp = {k_: np.asarray(v) for k_, v in inputs.items()}
    P = build_full()
    sh = prep_all_shared(inp)
    in_maps = []
    for b in range(N_CORES):
        m = dict(sh)
        m.update(prep_core(inp, b))
        in_maps.append(m)
    res = run_bass_kernel_spmd(P.nc, in_maps, core_ids=list(range(N_CORES)))
    outs = []
    for b in range(N_CORES):
        o = np.asarray(res.results[b]["out"], np.float32).reshape(D, NLAT)
        outs.append(np.ascontiguousarray(o.T))
    return np.stack(outs, 0).astype(np.float32)
```
